# Optimizing a Trainium2 kernel written in Bass

```python
import jax, jax.numpy as jnp
from jax import lax
import numpy as np

D_MODEL = 1024
BATCH = 2
SEQ = 8192
DEPTH = 1

N_ATTN_HEADS = 8
ATTN_HEAD_DIM = 64
KV_LATENT = 256
N_IDX_HEADS = 8
IDX_HEAD_DIM = 32
TOPK_MAX = 256
Q_BLOCK = 128
N_RWKV_HEADS = 8
RWKV_HEAD_DIM = 64
DECAY_LORA = 64
AAA_LORA = 64
GATE_LORA = 160
N_GROUPS = 4
EXPERTS_PER_GROUP = 8
N_EXPERTS = N_GROUPS * EXPERTS_PER_GROUP
D_EXPERT = 512
TOP_K_EXPERTS = 2
NORM_EPS = 1e-6
GN_EPS = 64e-5
ATTN_W = N_ATTN_HEADS * ATTN_HEAD_DIM
RWKV_W = N_RWKV_HEADS * RWKV_HEAD_DIM
IDX_Q_W = N_IDX_HEADS * IDX_HEAD_DIM
SHIFT_W = 3 * RWKV_W + DECAY_LORA + AAA_LORA + GATE_LORA
IN_SPLITS = (ATTN_W, KV_LATENT, IDX_Q_W, IDX_HEAD_DIM, N_IDX_HEADS, SHIFT_W, D_MODEL, D_MODEL)
IN_W = ATTN_W + KV_LATENT + IDX_Q_W + IDX_HEAD_DIM + N_IDX_HEADS + SHIFT_W + 2 * D_MODEL

kernel_name = "hybrid_dsa_rwkv7_hmoe_block"


def _split(x, sizes):
    idx = np.cumsum(sizes)[:-1].tolist()
    return jnp.split(x, idx, axis=-1)


def _rmsnorm(x, g):
    xf = x.astype(jnp.float32)
    y = xf * lax.rsqrt(jnp.mean(xf * xf, axis=-1, keepdims=True) + NORM_EPS)
    return (y * g.astype(jnp.float32)).astype(x.dtype)


def _dsa_attention(q, k, v, q_idx, k_idx, w_idx):
    B, S = q.shape[0], q.shape[1]
    top_k = min(TOPK_MAX, S // 4)
    nb = S // Q_BLOCK
    f32 = jnp.float32
    slopes = 2.0 ** (-8.0 * jnp.arange(1, N_ATTN_HEADS + 1, dtype=f32) / N_ATTN_HEADS)
    key_pos = jnp.arange(S)
    k_idx_f = k_idx.astype(f32)
    idx_scale = (IDX_HEAD_DIM ** -0.5) * (N_IDX_HEADS ** -0.5)

    def blocks(a):
        return jnp.moveaxis(a.reshape((B, nb, Q_BLOCK) + a.shape[2:]), 1, 0)

    def one_block(args):
        qb, qib, wib, start = args
        t = start + jnp.arange(Q_BLOCK)
        dots = jnp.einsum('bthd,bsd->bths', qib.astype(f32), k_idx_f)
        score = jnp.einsum('bths,bth->bts', jax.nn.relu(dots), wib.astype(f32)) * idx_scale
        causal = key_pos[None, :] <= t[:, None]
        score = jnp.where(causal[None], score, -jnp.inf)
        _, sel = lax.top_k(score, top_k)
        k_sel = jax.vmap(lambda kb, ib: kb[ib])(k, sel)
        v_sel = jax.vmap(lambda vb, ib: vb[ib])(v, sel)
        logits = jnp.einsum('bthd,btkhd->bthk', qb.astype(f32), k_sel.astype(f32)) * (ATTN_HEAD_DIM ** -0.5)
        dist = (t[None, :, None] - sel).astype(f32)
        logits = logits - slopes[None, None, :, None] * dist[:, :, None, :]
        valid = sel <= t[None, :, None]
        logits = jnp.where(valid[:, :, None, :], logits, -jnp.inf)
        p = jax.nn.softmax(logits, axis=-1)
        o = jnp.einsum('bthk,btkhd->bthd', p, v_sel.astype(f32))
        return o.astype(q.dtype)

    starts = jnp.arange(nb) * Q_BLOCK
    out = lax.map(one_block, (blocks(q), blocks(q_idx), blocks(w_idx), starts))
    return jnp.moveaxis(out, 0, 1).reshape(B, S, ATTN_W)


def _rwkv7_time_mix(p_shift, mu, w0, w2, a0, a2, g2, k_k, k_a, r_k, ln_w, ln_b):
    B, S = p_shift.shape[0], p_shift.shape[1]
    H, N = N_RWKV_HEADS, RWKV_HEAD_DIM
    f32 = jnp.float32
    prev = jnp.pad(p_shift[:, :-1], ((0, 0), (1, 0), (0, 0)))
    mixed = p_shift + (prev - p_shift) * mu
    r, k, v, wl, al, gl = _split(mixed, (RWKV_W, RWKV_W, RWKV_W, DECAY_LORA, AAA_LORA, GATE_LORA))
    w = -jax.nn.softplus(-(w0 + jnp.tanh(wl) @ w2)) - 0.5
    decay = jnp.exp(-jnp.exp(w.astype(f32)))
    a = jax.nn.sigmoid(a0 + al @ a2)
    g = jax.nn.sigmoid(gl) @ g2
    heads = lambda z: z.reshape(B, S, H, N).astype(f32)
    kk = heads(k * k_k)
    kk = kk / jnp.maximum(jnp.sqrt(jnp.sum(kk * kk, axis=-1, keepdims=True)), 1e-12)
    k = k * (1.0 + (a - 1.0) * k_a)
    r_h, k_h, v_h, a_h, d_h = heads(r), heads(k), heads(v), heads(a), heads(decay)

    def step(state, inp):
        r_t, d_t, k_t, v_t, kk_t, b_t = inp
        sa = jnp.einsum('bhij,bhj->bhi', state, -kk_t)
        state = state * d_t[:, :, None, :] + sa[..., None] * b_t[:, :, None, :] + v_t[..., None] * k_t[:, :, None, :]
        y = jnp.einsum('bhij,bhj->bhi', state, r_t)
        return state, y

    xs = tuple(jnp.moveaxis(z, 1, 0) for z in (r_h, d_h, k_h, v_h, kk, kk * a_h))
    _, y = lax.scan(step, jnp.zeros((B, H, N, N), f32), xs)
    y = jnp.moveaxis(y, 0, 1)
    mean = jnp.mean(y, axis=-1, keepdims=True)
    var = jnp.mean(jnp.square(y - mean), axis=-1, keepdims=True)
    y = ((y - mean) * lax.rsqrt(var + GN_EPS)).reshape(B, S, RWKV_W) * ln_w + ln_b
    bonus = (jnp.sum(r_h * k_h * r_k, axis=-1, keepdims=True) * v_h).reshape(B, S, RWKV_W)
    return ((y + bonus) * g).astype(p_shift.dtype)


def _hier_moe(xt, wg_router, we_router, e_bias, w_gate, w_up, w_down):
    T, D = xt.shape
    f32 = jnp.float32
    p_group = jax.nn.softmax((xt @ wg_router).astype(f32), axis=-1)
    g_sel = jnp.argmax(p_group, axis=-1)
    p_gsel = jnp.max(p_group, axis=-1)
    e_logits = (xt @ we_router).astype(f32).reshape(T, N_GROUPS, EXPERTS_PER_GROUP) + e_bias.astype(f32).reshape(N_GROUPS, EXPERTS_PER_GROUP)
    e_in = jnp.take_along_axis(e_logits, g_sel[:, None, None], axis=1)[:, 0]
    top_v, top_i = lax.top_k(e_in, TOP_K_EXPERTS)
    w_top = jax.nn.softmax(top_v, axis=-1) * p_gsel[:, None]
    expert_id = g_sel[:, None] * EXPERTS_PER_GROUP + top_i
    combine = jnp.sum(jax.nn.one_hot(expert_id, N_EXPERTS, dtype=f32) * w_top[..., None], axis=1)

    def body(acc, ew):
        wg, wu, wd, cw = ew
        hdn = jax.nn.silu(xt @ wg) * (xt @ wu)
        return acc + cw[:, None] * (hdn @ wd).astype(f32), None

    acc, _ = lax.scan(body, jnp.zeros((T, D), f32), (w_gate, w_up, w_down, combine.T))
    return acc.astype(xt.dtype)


def setup_inputs(seed: int = 0) -> dict:
    key = jax.random.key(seed)
    ks = iter(jax.random.split(key, 40))
    nrm = lambda shape, scale: jax.random.normal(next(ks), shape, jnp.float32) * scale
    L = DEPTH
    return {
        "x": nrm((BATCH, SEQ, D_MODEL), 1.0),
        "c": nrm((BATCH, D_MODEL), 1.0),
        "ada_w": nrm((L, D_MODEL, 6 * D_MODEL), 0.5 * D_MODEL ** -0.5),
        "ada_b": nrm((L, 6 * D_MODEL), 0.02),
        "mix_norm_g": 1.0 + nrm((L, D_MODEL), 0.02),
        "w_in": nrm((L, D_MODEL, IN_W), D_MODEL ** -0.5),
        "kv_norm_g": 1.0 + nrm((L, KV_LATENT), 0.02),
        "w_kv_up": nrm((L, KV_LATENT, 2 * ATTN_W), KV_LATENT ** -0.5),
        "q_norm_g": 1.0 + nrm((L, ATTN_HEAD_DIM), 0.02),
        "k_norm_g": 1.0 + nrm((L, ATTN_HEAD_DIM), 0.02),
        "rwkv_mu": jax.random.uniform(next(ks), (L, SHIFT_W), jnp.float32, 0.2, 0.8),
        "rwkv_w0": -1.0 + nrm((L, RWKV_W), 0.5),
        "rwkv_w2": nrm((L, DECAY_LORA, RWKV_W), 0.5 * DECAY_LORA ** -0.5),
        "rwkv_a0": nrm((L, RWKV_W), 0.1),
        "rwkv_a2": nrm((L, AAA_LORA, RWKV_W), 0.5 * AAA_LORA ** -0.5),
        "rwkv_g2": nrm((L, GATE_LORA, RWKV_W), GATE_LORA ** -0.5),
        "rwkv_k_k": 0.85 + nrm((L, RWKV_W), 0.02),
        "rwkv_k_a": 1.0 + nrm((L, RWKV_W), 0.02),
        "rwkv_r_k": nrm((L, N_RWKV_HEADS, RWKV_HEAD_DIM), 0.1),
        "rwkv_ln_w": 1.0 + nrm((L, RWKV_W), 0.02),
        "rwkv_ln_b": nrm((L, RWKV_W), 0.02),
        "w_branch_attn": nrm((L, ATTN_W, D_MODEL), ATTN_W ** -0.5),
        "w_branch_rwkv": nrm((L, RWKV_W, D_MODEL), RWKV_W ** -0.5),
        "w_out": nrm((L, D_MODEL, D_MODEL), D_MODEL ** -0.5),
        "moe_norm_g": 1.0 + nrm((L, D_MODEL), 0.02),
        "router_group_w": nrm((L, D_MODEL, N_GROUPS), D_MODEL ** -0.5),
        "router_expert_w": nrm((L, D_MODEL, N_EXPERTS), D_MODEL ** -0.5),
        "router_expert_bias": nrm((L, N_EXPERTS), 0.01),
        "expert_w_gate": nrm((L, N_EXPERTS, D_MODEL, D_EXPERT), D_MODEL ** -0.5),
        "expert_w_up": nrm((L, N_EXPERTS, D_MODEL, D_EXPERT), D_MODEL ** -0.5),
        "expert_w_down": nrm((L, N_EXPERTS, D_EXPERT, D_MODEL), D_EXPERT ** -0.5),
    }


def reference(x, c, ada_w, ada_b, mix_norm_g, w_in, kv_norm_g, w_kv_up, q_norm_g, k_norm_g,
              rwkv_mu, rwkv_w0, rwkv_w2, rwkv_a0, rwkv_a2, rwkv_g2, rwkv_k_k, rwkv_k_a, rwkv_r_k,
              rwkv_ln_w, rwkv_ln_b, w_branch_attn, w_branch_rwkv, w_out, moe_norm_g,
              router_group_w, router_expert_w, router_expert_bias, expert_w_gate, expert_w_up,
              expert_w_down):
    B, S, D = x.shape
    h = x
    for l in range(DEPTH):
        mod = jax.nn.silu(c) @ ada_w[l] + ada_b[l]
        sh1, sc1, gt1, sh2, sc2, gt2 = jnp.split(mod[:, None, :], 6, axis=-1)

        xn = _rmsnorm(h, mix_norm_g[l]) * (1.0 + sc1) + sh1
        proj = xn @ w_in[l]
        q_a, kv_lat, q_i, k_i, w_i, p_shift, gate_a, gate_r = _split(proj, IN_SPLITS)
        q = _rmsnorm(q_a.reshape(B, S, N_ATTN_HEADS, ATTN_HEAD_DIM), q_norm_g[l])
        kv = _rmsnorm(kv_lat, kv_norm_g[l]) @ w_kv_up[l]
        k_lin, v_lin = jnp.split(kv, 2, axis=-1)
        k = _rmsnorm(k_lin.reshape(B, S, N_ATTN_HEADS, ATTN_HEAD_DIM), k_norm_g[l])
        v = v_lin.reshape(B, S, N_ATTN_HEADS, ATTN_HEAD_DIM)
        o_attn = _dsa_attention(q, k, v, q_i.reshape(B, S, N_IDX_HEADS, IDX_HEAD_DIM), k_i, w_i)
        o_rwkv = _rwkv7_time_mix(p_shift, rwkv_mu[l], rwkv_w0[l], rwkv_w2[l], rwkv_a0[l], rwkv_a2[l],
                                 rwkv_g2[l], rwkv_k_k[l], rwkv_k_a[l], rwkv_r_k[l], rwkv_ln_w[l], rwkv_ln_b[l])
        merged = jax.nn.sigmoid(gate_a) * (o_attn @ w_branch_attn[l]) + jax.nn.sigmoid(gate_r) * (o_rwkv @ w_branch_rwkv[l])
        h = h + gt1 * (merged @ w_out[l])

        xn2 = _rmsnorm(h, moe_norm_g[l]) * (1.0 + sc2) + sh2
        moe = _hier_moe(xn2.reshape(B * S, D), router_group_w[l], router_expert_w[l], router_expert_bias[l],
                        expert_w_gate[l], expert_w_up[l], expert_w_down[l])
        h = h + gt2 * moe.reshape(B, S, D)
    return h
```

```python
from contextlib import ExitStack
import numpy as np
import concourse.bass as bass
import concourse.mybir as mybir
from concourse.bass_utils import run_bass_kernel_spmd

F32 = mybir.dt.float32
BF16 = mybir.dt.bfloat16
AF = mybir.ActivationFunctionType
ALU = mybir.AluOpType
AX = mybir.AxisListType

D = 1024
NBIS = 22
NORM_EPS = 1e-6
GN_EPS = 64e-5
SLOPES = [2.0 ** (-(h + 1)) for h in range(8)]
BIG = 262144.0


class T:
    def __init__(self, h, name):
        self.h = h
        self.name = name
        self.w = None
        self.r = {}

    def __getitem__(self, idx):
        return self.h[idx]


class Ctx:
    NDMA = 32

    def __init__(self, nc):
        self.nc = nc
        self.eng = {'pe': nc.tensor, 'act': nc.scalar, 'dve': nc.vector, 'pool': nc.gpsimd, 'sp': nc.sync}
        self.sem = {}
        self.cnt = {}
        self.known = {}
        for k in self.eng:
            self.sem[k] = nc.alloc_semaphore("s_" + k)
            self.cnt[k] = 0
            self.known[k] = {}
        self.dma_n = 0
        for i in range(self.NDMA):
            self.sem['dma%d' % i] = nc.alloc_semaphore("s_dma%d" % i)
        self.sem['cc'] = nc.alloc_semaphore("s_cc")
        self.uid = 0
        self.ninst = 0
        self.nw = {k: 0 for k in self.eng}
        self.snaps = {}
        self.snapq = []
        self.nd = {k: 0 for k in self.eng}

    def sb(self, stack, shape, dt=F32, name=None):
        self.uid += 1
        name = name or ("t%d" % self.uid)
        h = stack.enter_context(self.nc.sbuf_tensor(name, list(shape), dt))
        return T(h, name)

    def _learn(self, e, key, val):
        kn = self.known[e]
        if kn.get(key, 0) < val:
            kn[key] = val
        sn = self.snaps.get((key, val))
        if sn is not None:
            for k2, v2 in sn.items():
                if kn.get(k2, 0) < v2:
                    kn[k2] = v2

    def _wait(self, e, key, val):
        if key == e and e in ('pe', 'sp'):
            return
        kn = self.known[e]
        if kn.get(key, 0) >= val:
            return
        self.eng[e].wait_ge(self.sem[key], val)
        self._learn(e, key, val)
        self.ninst += 1
        self.nw[e] += 1

    def _snap(self, e, ev):
        self.snaps[ev] = dict(self.known[e])
        self.snapq.append(ev)
        if len(self.snapq) > 20000:
            old = self.snapq.pop(0)
            self.snaps.pop(old, None)

    def _deps(self, e, outs, ins):
        for t in ins:
            if t is not None and t.w is not None:
                self._wait(e, *t.w)
        for t in outs:
            if t.w is not None:
                self._wait(e, *t.w)
            for k, v in t.r.items():
                if k != e:
                    self._wait(e, k, v)

    def _done(self, ev, outs, ins):
        for t in ins:
            if t is not None:
                t.r[ev[0]] = max(ev[1], t.r.get(ev[0], 0))
        for t in outs:
            t.w = ev
            t.r = {}

    def op(self, e, fn, outs, ins):
        self._deps(e, outs, ins)
        inst = fn()
        self.cnt[e] += 1
        inst.then_inc(self.sem[e], 1)
        self.ninst += 1
        self._snap(e, (e, self.cnt[e]))
        self._done((e, self.cnt[e]), outs, ins)

    def dma(self, e, out_ap, in_ap, outs, ins, **kw):
        n = self.dma_n
        self.dma_n += 1
        key = 'dma%d' % (n % self.NDMA)
        rnd = n // self.NDMA
        if rnd > 0:
            self._wait(e, key, 16 * rnd)
        self._deps(e, outs, ins)
        inst = self.eng[e].dma_start(out=out_ap, in_=in_ap, **kw)
        self.nd[e] += 1
        inst.then_inc(self.sem[key], 16)
        self.ninst += 1
        ev = (key, 16 * (rnd + 1))
        self._snap(e, ev)
        self._done(ev, outs, ins)
        return ev

    def barrier(self):
        cur = {k: self.cnt[k] for k in ('pe', 'act', 'dve', 'pool')}
        for i in range(self.NDMA):
            total = (self.dma_n - i + self.NDMA - 1) // self.NDMA
            if total > 0:
                cur['dma%d' % i] = 16 * total
        for e in ('pe', 'act', 'dve', 'pool', 'sp'):
            for k, v in cur.items():
                if v > 0 and k != e:
                    self._wait(e, k, v)
            if e in cur and cur[e] > 0 and e not in ('pe',):
                self._wait(e, e, cur[e])


def build_program(NG=16, dbg=False, stop=99, NE=32):
    NT = 4 * NG; NO = NG; S = 512 * NG
    nc = bass.Bass("TRN2", target_bir_lowering=False)
    c = Ctx(nc)
    V, A, P, G, PE = nc.vector, nc.scalar, nc.gpsimd, nc.gpsimd, nc.tensor

    def din(name, shape, dt=F32):
        return nc.dram_tensor(name, list(shape), dt, kind="ExternalInput").ap()

    NXB = (NT + 31) // 32
    xbs = [din("xb%d" % i, [min(32, NT - 32 * i) * 128, D]) for i in range(NXB)]
    xo = din("xo", [NO * 128, D]); cb = din("cb", [128, 8])
    qrel = din("qrel", [128, 1]); sel = din("sel", [128, 4])
    ada_w = din("ada_w", [D, 6 * D]); ada_b = din("ada_b", [1, 6 * D])
    mixg = din("mixg", [128, 8]); moeg = din("moeg", [128, 8])
    w_rw = din("w_rw", [D, 672]); mu_rw = din("mu_rw", [128, 10])
    w_kv = din("w_kv", [D, 256]); w_ki = din("w_ki", [D, 128]); w_q = din("w_q", [D, 776])
    kvg = din("kvg", [128, 2]); w_kvup = din("w_kvup", [256, 1024])
    qg = din("qg", [128, 1]); kg = din("kg", [128, 1])
    rwv = din("rwv", [64, 2, 8])
    w2 = din("w2", [64, 128]); a2 = din("a2", [64, 128]); g2 = din("g2", [160, 128])
    w_ga = din("w_ga", [D, D]); w_gr = din("w_gr", [D, D])
    w_ba = din("w_ba", [512, D]); w_br = din("w_br", [512, D]); w_out = din("w_out", [D, D])
    w_rt = din("w_rt", [D, 36]); e_bias = din("e_bias", [1, 32])
    ew_g = [din("ew_g%d" % e, [D, 512]) for e in range(NE)]
    ew_u = [din("ew_u%d" % e, [D, 512]) for e in range(NE)]
    ew_d = [din("ew_d%d" % e, [512, D]) for e in range(NE)]
    out = nc.dram_tensor("out", [NO * 128, D], F32, kind="ExternalOutput")
    outT = T(out, "out")
    KTd = nc.dram_tensor("KTd", [NG, 128, 4, 512], BF16, kind="ExternalOutput"); KTdT = T(KTd, "KTd")
    V1d = nc.dram_tensor("V1d", [NT, 128, 8, 65], BF16, kind="ExternalOutput"); V1dT = T(V1d, "V1d")
    CG = min(NG, 4); NCH = NG // CG
    RSrcs = [nc.dram_tensor("RSrc%d" % k, [CG * 4 * 128, 128], BF16, kind="Internal") for k in range(NCH)]
    RSrcT = T(None, "RSrc")
    OATd = nc.dram_tensor("OATd", [NO, 128, 4, 128], BF16, kind="ExternalOutput"); OATdT = T(OATd, "OATd")
    RDsts = [nc.dram_tensor("RDst%d" % k, [4 * CG * 4 * 128, 128], BF16, kind="Internal") for k in range(NCH)]
    RDstT = T(None, "RDst")

    glob = ExitStack()
    PB = []
    for i in range(8):
        h = glob.enter_context(nc.psum_tensor("pb%d" % i, [128, 512], F32))
        PB.append(T(h, "pb%d" % i))

    sb = lambda shape, dt=F32, st=glob: c.sb(st, shape, dt)

    ident = sb([128, 128]); identb = sb([128, 128], BF16); ibig = sb([128, 128], BF16)
    ones64 = sb([64, 64]); onesr = sb([1, 128]); one11 = sb([1, 1])
    idrep = sb([128, 32], BF16)
    c.op('pool', lambda: G.memset(ident[:], 1.0), [ident], [])
    c.op('pool', lambda: G.affine_select(out=ident[:], in_=ident[:], pattern=[[-1, 128]], compare_op=ALU.is_equal,
                                         fill=0.0, base=0, channel_multiplier=1), [ident], [ident])
    c.op('dve', lambda: V.tensor_copy(out=identb[:], in_=ident[:]), [identb], [ident])
    c.op('dve', lambda: V.tensor_scalar(out=ibig[:], in0=ident[:], scalar1=BIG, scalar2=None, op0=ALU.mult), [ibig], [ident])
    c.op('dve', lambda: V.memset(ones64[:], 1.0 / 64.0), [ones64], [])
    c.op('dve', lambda: V.memset(onesr[:], 1.0), [onesr], [])
    c.op('dve', lambda: V.memset(one11[:], 1.0), [one11], [])
    idr32 = sb([128, 32])
    c.op('dve', lambda: V.tensor_tensor(out=idr32[:], in0=ident[:, 0:32], in1=ident[:, 32:64], op=ALU.add), [idr32], [ident])
    c.op('dve', lambda: V.tensor_tensor(out=idr32[:], in0=idr32[:], in1=ident[:, 64:96], op=ALU.add), [idr32], [idr32, ident])
    c.op('dve', lambda: V.tensor_tensor(out=idrep[:], in0=idr32[:], in1=ident[:, 96:128], op=ALU.add), [idrep], [idr32, ident])

    def load_small(ap, shape):
        t = sb(shape)
        c.dma('sp', t[:], ap, [t], [])
        return t
    cbt = load_small(cb, [128, 8]); qrel_t = load_small(qrel, [128, 1]); sel_t = load_small(sel, [128, 4])
    mixg_t = load_small(mixg, [128, 8]); moeg_t = load_small(moeg, [128, 8]); mu_t = load_small(mu_rw, [128, 10])
    kvg_t = load_small(kvg, [128, 2]); qg_t = load_small(qg, [128, 1]); kg_t = load_small(kg, [128, 1])
    rwv_t = load_small(rwv, [64, 2, 8])
    ebias_t = sb([128, 32])
    c.dma('sp', ebias_t[:], e_bias[0:1, :].broadcast_to([128, 32]), [ebias_t], [])

    stg = [sb([128, 1024]) for _ in range(2)]
    stg_i = [0]

    def load_w(dst, dst_ap_fn, src, nk, ncols, eng_cast=('pool', 'act')):
        cbk = max(1, min(ncols, 1024 // nk))
        for c0 in range(0, ncols, cbk):
            c1 = min(ncols, c0 + cbk)
            st = stg[stg_i[0] % 2]; stg_i[0] += 1
            view = st[:, 0:nk * (c1 - c0)].rearrange("p (k f) -> p k f", k=nk)
            c.dma('sp', view, src[:, c0:c1].rearrange("(k p) f -> p k f", p=128), [st], [])
            e = eng_cast[stg_i[0] % len(eng_cast)]
            if e == 'act':
                c.op('act', lambda: A.copy(out=dst_ap_fn(c0, c1), in_=view), [dst], [st])
            else:
                c.op('pool', lambda: G.tensor_copy(out=dst_ap_fn(c0, c1), in_=view), [dst], [st])

    silu_c = sb([128, 8])
    c.op('act', lambda: A.activation(out=silu_c[:], in_=cbt[:], func=AF.Silu), [silu_c], [cbt])
    gmod1 = sb([128, 8]); sh1 = sb([128, 8]); gmod2 = sb([128, 8]); sh2 = sb([128, 8])

    def ada_phase(js, stk, gts=None, silu_bc=None):
        blk = [c.sb(stk, [128, 8, 1024]) for _ in range(2)]
        adab_t = c.sb(stk, [1, 6 * D])
        c.dma('sp', adab_t[:], ada_b, [adab_t], [])
        for jj, j in enumerate(js):
            bt = blk[jj % 2]
            for kk in range(2):
                c.dma('sp', bt[:, 4 * kk:4 * kk + 4, :],
                      ada_w[512 * kk:512 * kk + 512, j * 1024:(j + 1) * 1024].rearrange("(k p) f -> p k f", p=128), [bt], [])
            if j in (0, 1, 3, 4):
                pm = PB[j % 2]
                for m in range(8):
                    for k in range(8):
                        c.op('pe', lambda: PE.matmul(pm[:, m:m + 1], lhsT=bt[:, k, m * 128:(m + 1) * 128], rhs=silu_c[:, k:k + 1],
                                                     start=(k == 0), stop=False), [pm], [bt, silu_c])
                    c.op('pe', lambda: PE.matmul(pm[:, m:m + 1], lhsT=adab_t[0:1, j * 1024 + m * 128:j * 1024 + (m + 1) * 128],
                                                 rhs=one11[0:1, 0:1], start=False, stop=True), [pm], [adab_t, one11])
                if j == 0:
                    c.op('dve', lambda: V.tensor_copy(out=sh1[:], in_=pm[:, 0:8]), [sh1], [pm])
                elif j == 3:
                    c.op('dve', lambda: V.tensor_copy(out=sh2[:], in_=pm[:, 0:8]), [sh2], [pm])
                else:
                    gm, gg = (gmod1, mixg_t) if j == 1 else (gmod2, moeg_t)
                    c.op('dve', lambda: V.scalar_tensor_tensor(out=gm[:], in0=pm[:, 0:8], scalar=1.0, in1=gg[:], op0=ALU.add, op1=ALU.mult),
                         [gm], [pm, gg])
            else:
                gt = gts[0] if j == 2 else gts[1]
                for half in range(2):
                    pm = PB[2 + half]
                    for k in range(8):
                        c.op('pe', lambda: PE.matmul(pm[:, :], lhsT=silu_bc[:, k, :], rhs=bt[:, k, half * 512:(half + 1) * 512],
                                                     start=(k == 0), stop=False), [pm], [bt, silu_bc])
                    c.op('pe', lambda: PE.matmul(pm[:, :], lhsT=onesr[0:1, :], rhs=adab_t[0:1, j * 1024 + half * 512:j * 1024 + (half + 1) * 512],
                                                 start=False, stop=True), [pm], [adab_t, onesr])
                    c.op('act', lambda: A.copy(out=gt[:, half * 512:(half + 1) * 512], in_=pm[:, :]), [gt], [pm])

    with ExitStack() as st0:
        ada_phase((0, 1, 3, 4), st0)
        c.barrier()
    if stop == 0:
        return nc, c

    xt_buf = [sb([128, D]) for _ in range(2)]
    xs_buf = sb([128, D]); st4 = sb([128, 4])
    xcnt = [0]
    dbg_outs = {}

    def dbg_dump(name, t, ap, shape, dt=F32):
        if not dbg:
            return
        d = nc.dram_tensor(name, list(shape), dt, kind="ExternalOutput")
        c.dma('sp', d.ap(), ap, [], [t])

    def norm_T(src_dram_ap, gm, sh, dst, dst_ap_fn, keep_x=None):
        xt = keep_x if keep_x is not None else xt_buf[xcnt[0] % 2]
        xcnt[0] += 1
        if src_dram_ap is not None:
            c.dma('sp', xt[:], src_dram_ap, [xt], [])
        c.op('act', lambda: A.activation(out=xs_buf[:], in_=xt[:], func=AF.Square, accum_out=st4[:, 0:1]), [xs_buf, st4], [xt])
        c.op('dve', lambda: V.tensor_scalar(out=st4[:, 1:2], in0=st4[:, 0:1], scalar1=1.0 / D, scalar2=NORM_EPS, op0=ALU.mult, op1=ALU.add), [st4], [st4])
        c.op('act', lambda: A.activation(out=st4[:, 2:3], in_=st4[:, 1:2], func=AF.Sqrt), [st4], [st4])
        c.op('dve', lambda: V.reciprocal(out=st4[:, 3:4], in_=st4[:, 2:3]), [st4], [st4])
        c.op('act', lambda: A.activation(out=xs_buf[:], in_=xt[:], func=AF.Copy, scale=st4[:, 3:4]), [xs_buf], [xt, st4])
        for half in range(2):
            pm = PB[half]
            for kq in range(4):
                k = half * 4 + kq
                c.op('pe', lambda: PE.transpose(out=pm[:, kq * 128:(kq + 1) * 128], in_=xs_buf[:, k * 128:(k + 1) * 128], identity=ident[:]),
                     [pm], [xs_buf, ident])
            for kq in range(4):
                k = half * 4 + kq
                if kq % 2 == 0:
                    c.op('dve', lambda: V.tensor_scalar(out=dst_ap_fn(k), in0=pm[:, kq * 128:(kq + 1) * 128], scalar1=gm[:, k:k + 1],
                                                        scalar2=sh[:, k:k + 1], op0=ALU.mult, op1=ALU.add), [dst], [pm, gm, sh])
                else:
                    c.op('act', lambda: A.activation(out=dst_ap_fn(k), in_=pm[:, kq * 128:(kq + 1) * 128], func=AF.Identity,
                                                     scale=gm[:, k:k + 1], bias=sh[:, k:k + 1]), [dst], [pm, gm, sh])

    def head_rstd(ss, tmp, rs, scale, eps):
        c.op('dve', lambda: V.tensor_scalar(out=tmp[:, 0:8], in0=ss[:], scalar1=scale, scalar2=eps, op0=ALU.mult, op1=ALU.add), [tmp], [ss])
        c.op('act', lambda: A.activation(out=tmp[:, 8:16], in_=tmp[:, 0:8], func=AF.Sqrt), [tmp], [tmp])
        c.op('dve', lambda: V.reciprocal(out=rs[:], in_=tmp[:, 8:16]), [rs], [tmp])

    dbg_dump("d_gmod1", gmod1, gmod1[:], [128, 8]); dbg_dump("d_sh1", sh1, sh1[:], [128, 8])
    dbg_dump("d_gmod2", gmod2, gmod2[:], [128, 8]); dbg_dump("d_sh2", sh2, sh2[:], [128, 8])

    stK = ExitStack()
    kiT = c.sb(stK, [128, S], BF16)
    with ExitStack() as st1:
        s1 = lambda shape, dt=F32: c.sb(st1, shape, dt)
        Wrw = s1([128, 8, 672], BF16); Wkv = s1([128, 8, 256], BF16); Wki = s1([128, 8, 128], BF16)
        Wup = s1([128, 2, 1024], BF16)
        load_w(Wrw, lambda c0, c1: Wrw[:, :, c0:c1], w_rw, 8, 672)
        load_w(Wkv, lambda c0, c1: Wkv[:, :, c0:c1], w_kv, 8, 256)
        load_w(Wki, lambda c0, c1: Wki[:, :, c0:c1], w_ki, 8, 128)
        for k in range(2):
            st = stg[stg_i[0] % 2]; stg_i[0] += 1
            c.dma('sp', st[:, 0:1024], w_kvup[k * 128:(k + 1) * 128, :], [st], [])
            c.op('dve', lambda: V.tensor_scalar(out=Wup[:, k, :], in0=st[:, 0:1024], scalar1=kvg_t[:, k:k + 1], scalar2=None, op0=ALU.mult),
                 [Wup], [st, kvg_t])
        w2b = s1([64, 128], BF16); a2b = s1([64, 128], BF16); g2b0 = s1([128, 128], BF16); g2b1 = s1([32, 128], BF16)
        for (bb, src, np_) in ((w2b, w2, 64), (a2b, a2, 64), (g2b0, g2[0:128, :], 128), (g2b1, g2[128:160, :], 32)):
            st = stg[stg_i[0] % 2]; stg_i[0] += 1
            c.dma('sp', st[0:np_, 0:128], src, [st], [])
            c.op('dve', lambda: V.tensor_copy(out=bb[:], in_=st[0:np_, 0:128]), [bb], [st])

        xnT = s1([128, 8, 512], BF16)
        Sst = [[s1([64, 64], BF16) for _ in range(2)] for _ in range(2)]
        for h in range(2):
            for j_ in range(2):
                c.op('dve', lambda: V.memset(Sst[h][j_][:], 0.0), [Sst[h][j_]], [])
        pend = [None]
        qspec = [("r0", 0, 64, 0), ("r1", 64, 64, 1), ("k0", 128, 64, 2), ("k1", 192, 64, 3), ("v0", 256, 64, 4), ("v1", 320, 64, 5),
                 ("wl", 384, 64, 6), ("al", 448, 64, 7), ("gl0", 512, 128, 8), ("gl1", 640, 32, 9)]
        SC = [s1([128, 512]) for _ in range(4)]
        praw = [s1([128, 513]) for _ in range(2)]
        lastc = s1([128, 10])
        c.op('dve', lambda: V.memset(lastc[:], 0.0), [lastc], [])
        mixd = {}
        for nm in ("r0", "r1", "k0", "k1", "v0", "v1"):
            mixd[nm] = s1([64, 512])
        mixd["wl"] = SC[0]; mixd["al"] = SC[1]; mixd["gl0"] = SC[2]; mixd["gl1"] = SC[3]
        twb = s1([64, 512], BF16); alb = s1([64, 512], BF16); sg0 = s1([128, 512], BF16); sg1 = s1([32, 512], BF16)
        hd = []
        for h in range(2):
            dct = {}
            for nm in ("dT", "b", "nkk", "kp"):
                dct[nm] = s1([64, 512])
            for nm in ("gT", "bv"):
                dct[nm] = s1([64, 512], BF16)
            dct["rT"] = s1([64, 512], BF16); dct["o"] = s1([64, 512], BF16)
            dct["tokE"] = [s1([64, 4, 64], BF16) for _ in range(2)]
            dct["tokO"] = [s1([64, 4, 64], BF16) for _ in range(2)]
            dct["BX"] = s1([64, 32, 64], BF16); dct["VX"] = s1([64, 32, 64], BF16)
            dct["tokF"] = [s1([64, 4, 64], BF16) for _ in range(2)]
            dct["A"] = [s1([64, 32, 64], BF16) for _ in range(2)]
            hd.append(dct)
        kvn = s1([128, 256]); kvs = s1([128, 4]); kvnT = s1([128, 2, 128], BF16)
        kss = s1([128, 8]); krs = s1([128, 8]); ktmp = s1([128, 16])
        KTt = [s1([128, 4, 128], BF16) for _ in range(2)]; V1t = [s1([128, 8, 65], BF16) for _ in range(2)]
        for v1 in V1t:
            c.op('dve', lambda: V.memset(v1[:], 1.0), [v1], [])

        maskE = s1([64, 1]); maskO = s1([64, 1])
        c.op('dve', lambda: V.tensor_reduce(out=maskE[:], in_=ident[0:64, 0:32], axis=AX.X, op=ALU.add), [maskE], [ident])
        c.op('dve', lambda: V.tensor_reduce(out=maskO[:], in_=ident[0:64, 32:64], axis=AX.X, op=ALU.add), [maskO], [ident])
        import os as _os
        acnt = [0]
        for g in range(int(_os.environ.get('START_G', '0')), NG):
            xg = xnT
            for tt in range(4):
                tile = 4 * g + tt
                norm_T(xbs[0 if _os.environ.get('XB0') else tile // 32][(tile % 32) * 128:(tile % 32 + 1) * 128, :], gmod1, sh1, xg, lambda k: xg[:, k, tt * 128:(tt + 1) * 128])
            if g == 0:
                dbg_dump("d_xnT", xg, xg[:], [128, 8, 512], BF16)
            if stop == 10 and g == int(_os.environ.get('STOP_G', '0')):
                c.barrier()
                return nc, c
            pm = PB[2]
            for k in range(0 if _os.environ.get('SKIP_KI') else 8):
                c.op('pe', lambda: PE.matmul(pm[:, :], lhsT=Wki[:, k, :], rhs=xg[:, k, :], start=(k == 0), stop=(k == 7)), [pm], [Wki, xg])
            gk = 0 if _os.environ.get('KI0') else g
            c.op('act', lambda: A.copy(out=kiT[:, gk * 512:(gk + 1) * 512], in_=pm[:, :]), [kiT], [pm])
            if stop == 11 and g == int(_os.environ.get('STOP_G', '0')):
                c.barrier()
                return nc, c
            for tt in range(0 if _os.environ.get('SKIP_KV') else 4):
                tile = 4 * g + tt
                pm = PB[3]
                for k in range(8):
                    c.op('pe', lambda: PE.matmul(pm[:, 0:256], lhsT=xg[:, k, tt * 128:(tt + 1) * 128], rhs=Wkv[:, k, :], start=(k == 0), stop=(k == 7)),
                         [pm], [Wkv, xg])
                c.op('act', lambda: A.activation(out=kvn[:], in_=pm[:, 0:256], func=AF.Square, accum_out=kvs[:, 0:1]), [kvn, kvs], [pm])
                c.op('dve', lambda: V.tensor_scalar(out=kvs[:, 1:2], in0=kvs[:, 0:1], scalar1=1.0 / 256, scalar2=NORM_EPS, op0=ALU.mult, op1=ALU.add), [kvs], [kvs])
                c.op('act', lambda: A.activation(out=kvs[:, 2:3], in_=kvs[:, 1:2], func=AF.Sqrt), [kvs], [kvs])
                c.op('dve', lambda: V.reciprocal(out=kvs[:, 3:4], in_=kvs[:, 2:3]), [kvs], [kvs])
                c.op('act', lambda: A.activation(out=kvn[:], in_=pm[:, 0:256], func=AF.Copy, scale=kvs[:, 3:4]), [kvn], [pm, kvs])
                pt = PB[4]
                for k in range(2):
                    c.op('pe', lambda: PE.transpose(out=pt[:, k * 128:(k + 1) * 128], in_=kvn[:, k * 128:(k + 1) * 128], identity=ident[:]), [pt], [kvn, ident])
                c.op('dve', lambda: V.tensor_copy(out=kvnT[:].rearrange("p k t -> p (k t)"), in_=pt[:, 0:256]), [kvnT], [pt])
                pk = PB[5]; pv = PB[6]
                for k in range(2):
                    c.op('pe', lambda: PE.matmul(pk[:, :], lhsT=kvnT[:, k, :], rhs=Wup[:, k, 0:512], start=(k == 0), stop=(k == 1)), [pk], [kvnT, Wup])
                for k in range(2):
                    c.op('pe', lambda: PE.matmul(pv[:, :], lhsT=kvnT[:, k, :], rhs=Wup[:, k, 512:1024], start=(k == 0), stop=(k == 1)), [pv], [kvnT, Wup])
                v1 = V1t[tile % 2]
                c.op('act', lambda: A.copy(out=v1[:, :, 0:64], in_=pv[:, :].rearrange("p (h e) -> p h e", h=8)), [v1], [pv])
                c.dma('sp', V1d.ap()[tile], v1[:], [V1dT], [v1])
                ksq = SC[0]; Kn = SC[1]
                c.op('act', lambda: A.activation(out=ksq[:], in_=pk[:, :], func=AF.Square), [ksq], [pk])
                c.op('dve', lambda: V.tensor_reduce(out=kss[:], in_=ksq[:].rearrange("p (h e) -> p h e", h=8), axis=AX.X, op=ALU.add), [kss], [ksq])
                head_rstd(kss, ktmp, krs, 1.0 / 64, NORM_EPS)
                c.op('dve', lambda: V.tensor_tensor(out=Kn[:].rearrange("p (h e) -> p h e", h=8), in0=pk[:, :].rearrange("p (h e) -> p h e", h=8),
                                                    in1=krs[:, :].unsqueeze(2).broadcast_to([128, 8, 64]), op=ALU.mult), [Kn], [pk, krs])
                ptk = PB[7]
                for pr in range(4):
                    c.op('pe', lambda: PE.transpose(out=ptk[:, pr * 128:(pr + 1) * 128], in_=Kn[:, pr * 128:(pr + 1) * 128], identity=ident[:]), [ptk], [Kn, ident])
                kt = KTt[tile % 2]
                c.op('dve', lambda: V.tensor_scalar(out=kt[:].rearrange("p a t -> p (a t)"), in0=ptk[:, :], scalar1=kg_t[:, 0:1], scalar2=None, op0=ALU.mult),
                     [kt], [ptk, kg_t])
                c.dma('sp', KTd.ap()[g, :, :, tt * 128:(tt + 1) * 128], kt[:], [KTdT], [kt])

            if stop == 12:
                c.barrier()
                return nc, c
            if _os.environ.get('SKIP_RW'):
                continue
            for qi, (nm, c0, M, mc) in enumerate(qspec):
                pm = PB[2 + (qi % 2)]
                pr_ = praw[qi % 2]; mx = mixd[nm]
                c.op('dve', lambda: V.tensor_copy(out=pr_[0:M, 0:1], in_=lastc[0:M, qi:qi + 1]), [pr_], [lastc])
                for k in range(8):
                    c.op('pe', lambda: PE.matmul(pm[0:M, :], lhsT=Wrw[:, k, c0:c0 + M], rhs=xg[:, k, :], start=(k == 0), stop=(k == 7)), [pm], [Wrw, xg])
                c.op('act', lambda: A.copy(out=pr_[0:M, 1:513], in_=pm[0:M, :]), [pr_], [pm])
                c.op('dve', lambda: V.tensor_copy(out=lastc[0:M, qi:qi + 1], in_=pr_[0:M, 512:513]), [lastc], [pr_])
                c.op('dve', lambda: V.tensor_tensor(out=mx[0:M, :], in0=pr_[0:M, 0:512], in1=pr_[0:M, 1:513], op=ALU.subtract), [mx], [pr_])
                c.op('dve', lambda: V.scalar_tensor_tensor(out=mx[0:M, :], in0=mx[0:M, :], scalar=mu_t[0:M, mc:mc + 1], in1=pr_[0:M, 1:513],
                                                           op0=ALU.mult, op1=ALU.add), [mx], [mx, pr_, mu_t])
            if stop == 13:
                c.barrier()
                return nc, c
            c.op('act', lambda: A.activation(out=twb[:], in_=mixd["wl"][0:64, :], func=AF.Tanh), [twb], [mixd["wl"]])
            c.op('dve', lambda: V.tensor_copy(out=alb[:], in_=mixd["al"][0:64, :]), [alb], [mixd["al"]])
            c.op('act', lambda: A.activation(out=sg0[:], in_=mixd["gl0"][:, :], func=AF.Sigmoid), [sg0], [mixd["gl0"]])
            c.op('act', lambda: A.activation(out=sg1[:], in_=mixd["gl1"][0:32, :], func=AF.Sigmoid), [sg1], [mixd["gl1"]])
            for h in range(2):
                dct = hd[h]
                rv = lambda j: rwv_t[:, h, j:j + 1]
                mr, mk, mv = mixd["r%d" % h], mixd["k%d" % h], mixd["v%d" % h]
                s0, s1_, s2_, s3_ = [SC[j] for j in range(4)]
                pm = PB[2]
                c.op('pe', lambda: PE.matmul(pm[0:64, :], lhsT=w2b[:, h * 64:(h + 1) * 64], rhs=twb[:], start=True, stop=True), [pm], [w2b, twb])
                c.op('act', lambda: A.activation(out=s0[0:64, :], in_=pm[0:64, :], func=AF.Sigmoid, bias=rv(0), scale=1.0), [s0], [pm, rwv_t])
                c.op('act', lambda: A.activation(out=dct["dT"][:], in_=s0[0:64, :], func=AF.Exp, scale=-0.6065306597126334), [dct["dT"]], [s0])
                pm = PB[3]
                c.op('pe', lambda: PE.matmul(pm[0:64, :], lhsT=a2b[:, h * 64:(h + 1) * 64], rhs=alb[:], start=True, stop=True), [pm], [a2b, alb])
                c.op('act', lambda: A.activation(out=s1_[0:64, :], in_=pm[0:64, :], func=AF.Sigmoid, bias=rv(1), scale=1.0), [s1_], [pm, rwv_t])
                pm = PB[2]
                c.op('pe', lambda: PE.matmul(pm[0:64, :], lhsT=g2b0[:, h * 64:(h + 1) * 64], rhs=sg0[:], start=True, stop=False), [pm], [g2b0, sg0])
                c.op('pe', lambda: PE.matmul(pm[0:64, :], lhsT=g2b1[:, h * 64:(h + 1) * 64], rhs=sg1[:], start=False, stop=True), [pm], [g2b1, sg1])
                c.op('act', lambda: A.copy(out=dct["gT"][:], in_=pm[0:64, :]), [dct["gT"]], [pm])
                c.op('dve', lambda: V.tensor_scalar(out=s2_[0:64, :], in0=mk[:], scalar1=rv(2), scalar2=None, op0=ALU.mult), [s2_], [mk, rwv_t])
                c.op('act', lambda: A.activation(out=s3_[0:64, :], in_=s2_[0:64, :], func=AF.Square), [s3_], [s2_])
                pm = PB[3]
                c.op('pe', lambda: PE.matmul(pm[0:64, :], lhsT=ones64[:], rhs=s3_[0:64, :], start=True, stop=True), [pm], [ones64, s3_])
                c.op('act', lambda: A.activation(out=s3_[0:64, :], in_=pm[0:64, :], func=AF.Sqrt, scale=64.0), [s3_], [pm])
                c.op('dve', lambda: V.tensor_scalar(out=s3_[0:64, :], in0=s3_[0:64, :], scalar1=1e-12, scalar2=None, op0=ALU.max), [s3_], [s3_])
                c.op('dve', lambda: V.reciprocal(out=s0[0:64, :], in_=s3_[0:64, :]), [s0], [s3_])
                c.op('dve', lambda: V.tensor_tensor(out=s2_[0:64, :], in0=s2_[0:64, :], in1=s0[0:64, :], op=ALU.mult), [s2_], [s2_, s0])
                c.op('dve', lambda: V.tensor_tensor(out=dct["b"][:], in0=s2_[0:64, :], in1=s1_[0:64, :], op=ALU.mult), [dct["b"]], [s2_, s1_])
                c.op('dve', lambda: V.tensor_scalar(out=dct["nkk"][:], in0=s2_[0:64, :], scalar1=-1.0, scalar2=None, op0=ALU.mult), [dct["nkk"]], [s2_])
                c.op('dve', lambda: V.tensor_scalar(out=s0[0:64, :], in0=s1_[0:64, :], scalar1=1.0, scalar2=rv(3), op0=ALU.subtract, op1=ALU.mult),
                     [s0], [s1_, rwv_t])
                c.op('dve', lambda: V.scalar_tensor_tensor(out=dct["kp"][:], in0=s0[0:64, :], scalar=1.0, in1=mk[:], op0=ALU.add, op1=ALU.mult),
                     [dct["kp"]], [s0, mk])
                c.op('dve', lambda: V.scalar_tensor_tensor(out=s3_[0:64, :], in0=mr[:], scalar=rv(4), in1=dct["kp"][:], op0=ALU.mult, op1=ALU.mult),
                     [s3_], [mr, dct["kp"], rwv_t])
                pm = PB[2]
                c.op('pe', lambda: PE.matmul(pm[0:64, :], lhsT=ones64[:], rhs=s3_[0:64, :], start=True, stop=True), [pm], [ones64, s3_])
                c.op('dve', lambda: V.scalar_tensor_tensor(out=dct["bv"][:], in0=pm[0:64, :], scalar=64.0, in1=mv[:], op0=ALU.mult, op1=ALU.mult),
                     [dct["bv"]], [pm, mv])
                c.op('act', lambda: A.copy(out=dct["rT"][:], in_=mr[:]), [dct["rT"]], [mr])
                if g == 0 and dbg:
                    for nm in ("dT", "gT", "b", "nkk", "kp", "bv"):
                        dbg_dump("d_%s%d" % (nm, h), dct[nm], dct[nm][:], [64, 512], F32 if nm in ("dT", "b", "nkk", "kp") else BF16)
            if stop == 14:
                c.barrier()
                return nc, c
            PY = [PB[6], PB[7]]
            PS = [PB[4], PB[5]]
            for ht in range(0 if _os.environ.get('SKIP_CHAIN') else 8):
                for h in range(2):
                    dct = hd[h]
                    tokE = dct["tokE"][ht % 2]; tokO = dct["tokO"][ht % 2]
                    pm = PB[2 + h]
                    srcs = [dct["nkk"], dct["b"], dct["kp"], mixd["v%d" % h]]
                    for si, sT in enumerate(srcs):
                        c.op('pe', lambda: PE.transpose(out=pm[0:64, si * 64:(si + 1) * 64], in_=sT[:, ht * 64:(ht + 1) * 64], identity=ident[0:64, 0:64]),
                             [pm], [sT, ident])
                    c.op('act', lambda: A.activation(out=tokE[:].rearrange("p a j -> p (a j)"), in_=pm[0:64, 0:256], func=AF.Copy, scale=maskE[:, 0:1]),
                         [tokE], [pm, maskE])
                    c.op('act', lambda: A.activation(out=tokO[:].rearrange("p a j -> p (a j)"), in_=pm[0:64, 0:256], func=AF.Copy, scale=maskO[:, 0:1]),
                         [tokO], [pm, maskO])
                    tokF = dct["tokF"][ht % 2]
                    c.op('act', lambda: A.copy(out=tokF[:].rearrange("p a j -> p (a j)"), in_=pm[0:64, 0:256]), [tokF], [pm])
                    c.op('pool', lambda: G.tensor_tensor(out=dct["BX"][:], in0=idrep[0:64, :].unsqueeze(2).broadcast_to([64, 32, 64]),
                                                         in1=tokF[:, 1, :].unsqueeze(1).broadcast_to([64, 32, 64]), op=ALU.mult), [dct["BX"]], [idrep, tokF])
                    c.op('pool', lambda: G.tensor_tensor(out=dct["VX"][:], in0=idrep[0:64, :].unsqueeze(2).broadcast_to([64, 32, 64]),
                                                         in1=tokF[:, 3, :].unsqueeze(1).broadcast_to([64, 32, 64]), op=ALU.mult), [dct["VX"]], [idrep, tokF])
                if stop == 15:
                    c.barrier()
                    return nc, c
                for mb in range(2):
                    Ablk = []
                    for h in range(2):
                        dct = hd[h]
                        tk = (dct["tokE"] if mb == 0 else dct["tokO"])[ht % 2]
                        bx = dct["BX"]
                        Ab = dct["A"][acnt[0] % 2]
                        Ablk.append(Ab)
                        for qq in range(4):
                            pm = PB[2 + (qq % 2)]
                            c.op('pe', lambda: PE.matmul(pm[0:64, :], lhsT=tk[:, 0, :], rhs=bx[:, 8 * qq:8 * qq + 8, :].rearrange("p a j -> p (a j)"),
                                                         start=True, stop=True), [pm], [tk, bx])
                            c.op('act', lambda: A.copy(out=Ab[:, 8 * qq:8 * qq + 8, :].rearrange("p a j -> p (a j)"), in_=pm[0:64, :]), [Ab], [pm])
                    acnt[0] += 1
                    if stop == 16:
                        c.barrier()
                        return nc, c
                    for tl in range(32):
                        tcol = ht * 64 + mb * 32 + tl
                        for h in range(2):
                            dct = hd[h]
                            tk = (dct["tokE"] if mb == 0 else dct["tokO"])[ht % 2]
                            vx = dct["VX"]
                            ps = PS[h]
                            src = Sst[h][tcol % 2]; dstS = Sst[h][(tcol + 1) % 2]
                            c.op('pe', lambda: PE.matmul(ps[0:64, 0:64], lhsT=Ablk[h][:, tl, :], rhs=src[:], start=True, stop=False), [ps], [Ablk[h], src])
                            c.op('pe', lambda: PE.matmul(ps[0:64, 0:64], lhsT=tk[:, 2, :], rhs=vx[:, tl, :], start=False, stop=True), [ps], [tk, vx])
                            c.op('dve', lambda: V.scalar_tensor_tensor(out=dstS[:], in0=src[:], scalar=dct["dT"][:, tcol:tcol + 1], in1=ps[0:64, 0:64],
                                                                       op0=ALU.mult, op1=ALU.add), [dstS], [src, ps, dct["dT"]])
                        if pend[0] is not None:
                            pc = pend[0]
                            for h in range(2):
                                dct = hd[h]
                                sy = Sst[h][(pc + 1) % 2]
                                c.op('pe', lambda: PE.matmul(PY[h][0:64, pc:pc + 1], lhsT=sy[:], rhs=dct["rT"][:, pc:pc + 1], start=True, stop=True),
                                     [PY[h]], [sy, dct["rT"]])
                        pend[0] = tcol
                        if stop == 18:
                            c.barrier()
                            return nc, c
            if pend[0] is not None:
                pc = pend[0]
                for h in range(2):
                    dct = hd[h]
                    sy = Sst[h][(pc + 1) % 2]
                    c.op('pe', lambda: PE.matmul(PY[h][0:64, pc:pc + 1], lhsT=sy[:], rhs=dct["rT"][:, pc:pc + 1], start=True, stop=True),
                         [PY[h]], [sy, dct["rT"]])
                pend[0] = None
            if stop == 17:
                c.barrier()
                return nc, c
            for h in range(2):
                dct = hd[h]
                rv = lambda j: rwv_t[:, h, j:j + 1]
                Y, yc, ysq, tmp = [SC[j] for j in range(4)]
                c.op('act', lambda: A.copy(out=Y[0:64, :], in_=PY[h][0:64, :]), [Y], [PY[h]])
                if g == 0:
                    dbg_dump("d_Y%d" % h, Y, Y[0:64, :], [64, 512])
                pm = PB[2]
                c.op('pe', lambda: PE.matmul(pm[0:64, :], lhsT=ones64[:], rhs=Y[0:64, :], start=True, stop=True), [pm], [ones64, Y])
                c.op('dve', lambda: V.tensor_tensor(out=yc[0:64, :], in0=Y[0:64, :], in1=pm[0:64, :], op=ALU.subtract), [yc], [Y, pm])
                c.op('act', lambda: A.activation(out=ysq[0:64, :], in_=yc[0:64, :], func=AF.Square), [ysq], [yc])
                pm = PB[3]
                c.op('pe', lambda: PE.matmul(pm[0:64, :], lhsT=ones64[:], rhs=ysq[0:64, :], start=True, stop=True), [pm], [ones64, ysq])
                c.op('dve', lambda: V.tensor_scalar(out=tmp[0:64, :], in0=pm[0:64, :], scalar1=GN_EPS, scalar2=None, op0=ALU.add), [tmp], [pm])
                c.op('act', lambda: A.activation(out=ysq[0:64, :], in_=tmp[0:64, :], func=AF.Sqrt), [ysq], [tmp])
                c.op('dve', lambda: V.reciprocal(out=tmp[0:64, :], in_=ysq[0:64, :]), [tmp], [ysq])
                c.op('dve', lambda: V.tensor_tensor(out=yc[0:64, :], in0=yc[0:64, :], in1=tmp[0:64, :], op=ALU.mult), [yc], [yc, tmp])
                c.op('dve', lambda: V.tensor_scalar(out=yc[0:64, :], in0=yc[0:64, :], scalar1=rv(5), scalar2=rv(6), op0=ALU.mult, op1=ALU.add),
                     [yc], [yc, rwv_t])
                c.op('dve', lambda: V.tensor_tensor(out=yc[0:64, :], in0=yc[0:64, :], in1=dct["bv"][:], op=ALU.add), [yc], [yc, dct["bv"]])
                c.op('dve', lambda: V.tensor_tensor(out=dct["o"][:], in0=yc[0:64, :], in1=dct["gT"][:], op=ALU.mult), [dct["o"]], [yc, dct["gT"]])
                dst = RSrcs[g // CG].ap().rearrange("(g t c) x -> g c t x", g=CG, t=4, c=128)[g % CG, h * 64:(h + 1) * 64, :, :]
                c.dma('sp', dst, dct["o"][:].rearrange("p (t x) -> p t x", t=4), [RSrcT], [dct["o"]])
        c.barrier()
    if dbg:
        dk = nc.dram_tensor("d_ki", [128, S], BF16, kind="ExternalOutput")
        c.dma('sp', dk.ap(), kiT[:], [], [kiT])
        drs = nc.dram_tensor("d_rsrc", [NG * 4 * 128, 128], BF16, kind="ExternalOutput")
        for k_ in range(NCH):
            c.dma('sp', drs.ap()[k_ * CG * 512:(k_ + 1) * CG * 512, :], RSrcs[k_].ap(), [], [RSrcT])
        dkt = nc.dram_tensor("d_kt", [NG, 128, 4, 512], BF16, kind="ExternalOutput")
        c.dma('sp', dkt.ap(), KTd.ap(), [], [KTdT])
        dv1 = nc.dram_tensor("d_v1", [NT, 128, 8, 65], BF16, kind="ExternalOutput")
        c.dma('sp', dv1.ap(), V1d.ap(), [], [V1dT])

    if stop == 1:
        c.barrier()
        return nc, c
    c._deps('pool', [RDstT], [RSrcT])
    for k_ in range(NCH):
        inst = nc.gpsimd.collective_compute("AllGather", ALU.bypass, replica_groups=[[0, 1, 2, 3], [4, 5, 6, 7]],
                                            ins=[RSrcs[k_].ap()], outs=[RDsts[k_].ap()])
        inst.then_inc(c.sem['cc'], 1)
    c._done(('cc', NCH), [RDstT], [RSrcT])

    if stop == 2:
        c.barrier()
        return nc, c
    with ExitStack() as st2:
        s2 = lambda shape, dt=F32: c.sb(st2, shape, dt)
        Wq = s2([128, 8, 776], BF16)
        load_w(Wq, lambda c0, c1: Wq[:, :, c0:c1], w_q, 8, 776)
        sc = s2([128, S]); Mall = s2([128, S], BF16)
        pen = s2([128, 512])
        iota0 = s2([128, 512]); iota1 = s2([128, 512]); iotai = s2([128, 512], mybir.dt.int32)
        c.op('pool', lambda: G.iota(iotai[:], pattern=[[1, 512]], base=0, channel_multiplier=0), [iotai], [])
        c.op('dve', lambda: V.tensor_copy(out=iota0[:], in_=iotai[:]), [iota0], [iotai])
        c.op('dve', lambda: V.tensor_scalar(out=iota1[:], in0=iota0[:], scalar1=1.0, scalar2=None, op0=ALU.add), [iota1], [iota0])
        c.op('dve', lambda: V.tensor_scalar(out=pen[:], in0=iota0[:], scalar1=qrel_t[:, 0:1], scalar2=-1e30, op0=ALU.is_gt, op1=ALU.mult), [pen], [iota0, qrel_t])
        KPf = s2([3, 512]); KPl = s2([3, 512], BF16); lo_f = s2([1, 512])
        c.op('dve', lambda: V.memset(KPf[:], 1.0), [KPf], [])
        kpi = s2([1, 512], mybir.dt.int32); kpi2 = s2([1, 512], mybir.dt.int32)
        c.op('pool', lambda: G.iota(kpi[:], pattern=[[64, 8], [0, 64]], base=0, channel_multiplier=0), [kpi], [])
        c.op('pool', lambda: G.iota(kpi2[:], pattern=[[0, 8], [1, 64]], base=0, channel_multiplier=0), [kpi2], [])
        c.op('dve', lambda: V.tensor_copy(out=KPf[0:1, :], in_=kpi[:]), [KPf], [kpi, KPf])
        c.op('dve', lambda: V.tensor_copy(out=lo_f[:], in_=kpi2[:]), [lo_f], [kpi2])
        c.dma('sp', KPf[1:2, :], lo_f[:], [KPf], [lo_f])
        c.op('dve', lambda: V.tensor_copy(out=KPl[:], in_=KPf[:]), [KPl], [KPf])
        sl3 = s2([3, 8])
        for h in range(8):
            c.op('dve', lambda: V.memset(sl3[:, h:h + 1], 8.0 * SLOPES[h]), [sl3], [sl3])
        xq_t = s2([128, 8, 128], BF16)
        qsq = s2([128, 512]); qss = s2([128, 8]); qrs = s2([128, 8]); qtmp = s2([128, 16]); Qn = s2([128, 512])
        QT = s2([128, 4, 128], BF16); qiT = s2([128, 3, 128], BF16); widx = s2([128, 8])
        Rl = [s2([128, 512], BF16) for _ in range(2)]
        bs = s2([128, 8])
        sm = s2([128, 8]); smtmp = s2([128, 512])
        base3 = s2([128, 8]); QPb = s2([3, 128]); QP = s2([3, 8, 128], BF16)
        KTg = [s2([128, 4, 512], BF16) for _ in range(2)]; V1g = [s2([128, 4, 8, 65], BF16) for _ in range(2)]
        PT = [s2([128, 4, 128], BF16) for _ in range(2)]
        acc = s2([128, 8, 65]); rec = s2([128, 8]); oat = s2([128, 512])
        OATb = [s2([128, 4, 128], BF16) for _ in range(2)]
        pcnt = [0]
        for i in range(NO):
            L = 512 * (i + 1)
            norm_T(xo[i * 128:(i + 1) * 128, :], gmod1, sh1, xq_t, lambda k: xq_t[:, k, :])
            xq = lambda k: xq_t[:, k, :]
            pm = PB[2]
            for k in range(8):
                c.op('pe', lambda: PE.matmul(pm[:, :], lhsT=xq(k), rhs=Wq[:, k, 0:512], start=(k == 0), stop=(k == 7)), [pm], [xq_t, Wq])
            c.op('act', lambda: A.activation(out=qsq[:], in_=pm[:, :], func=AF.Square), [qsq], [pm])
            c.op('dve', lambda: V.tensor_reduce(out=qss[:], in_=qsq[:].rearrange("p (h e) -> p h e", h=8), axis=AX.X, op=ALU.add), [qss], [qsq])
            head_rstd(qss, qtmp, qrs, 1.0 / 64, NORM_EPS)
            c.op('dve', lambda: V.tensor_tensor(out=Qn[:].rearrange("p (h e) -> p h e", h=8), in0=pm[:, :].rearrange("p (h e) -> p h e", h=8),
                                                in1=qrs[:, :].unsqueeze(2).broadcast_to([128, 8, 64]), op=ALU.mult), [Qn], [pm, qrs])
            pt = PB[3]
            for pr in range(4):
                c.op('pe', lambda: PE.transpose(out=pt[:, pr * 128:(pr + 1) * 128], in_=Qn[:, pr * 128:(pr + 1) * 128], identity=ident[:]), [pt], [Qn, ident])
            c.op('dve', lambda: V.tensor_scalar(out=QT[:].rearrange("p a t -> p (a t)"), in0=pt[:, :], scalar1=qg_t[:, 0:1], scalar2=None, op0=ALU.mult),
                 [QT], [pt, qg_t])
            pm = PB[2]
            for a in range(3):
                M = 96 if a < 2 else 64
                for k in range(8):
                    c.op('pe', lambda: PE.matmul(pm[0:M, a * 128:(a + 1) * 128], lhsT=Wq[:, k, 512 + a * 96:512 + a * 96 + M], rhs=xq(k),
                                                 start=(k == 0), stop=(k == 7)), [pm], [xq_t, Wq])
                c.op('act', lambda: A.copy(out=qiT[0:M, a, :], in_=pm[0:M, a * 128:(a + 1) * 128]), [qiT], [pm])
            pm = PB[3]
            for k in range(8):
                c.op('pe', lambda: PE.matmul(pm[:, 0:8], lhsT=xq(k), rhs=Wq[:, k, 768:776], start=(k == 0), stop=(k == 7)), [pm], [xq_t, Wq])
            c.op('dve', lambda: V.tensor_copy(out=widx[:], in_=pm[:, 0:8]), [widx], [pm])
            for cg in range(i + 1):
                for h in range(8):
                    a, r = h // 3, h % 3
                    pm = PB[4 + (h % 2)]
                    c.op('pe', lambda: PE.matmul(pm[:, :], lhsT=qiT[32 * r:32 * r + 32, a, :], rhs=kiT[32 * r:32 * r + 32, cg * 512:(cg + 1) * 512],
                                                 start=True, stop=True), [pm], [qiT, kiT])
                    rl = Rl[h % 2]
                    c.op('act', lambda: A.activation(out=rl[:], in_=pm[:, :], func=AF.Relu), [rl], [pm])
                    if h == 0:
                        c.op('dve', lambda: V.tensor_scalar(out=sc[:, cg * 512:(cg + 1) * 512], in0=rl[:], scalar1=widx[:, 0:1], scalar2=None, op0=ALU.mult),
                             [sc], [rl, widx])
                    else:
                        c.op('dve', lambda: V.scalar_tensor_tensor(out=sc[:, cg * 512:(cg + 1) * 512], in0=rl[:], scalar=widx[:, h:h + 1],
                                                                   in1=sc[:, cg * 512:(cg + 1) * 512], op0=ALU.mult, op1=ALU.add), [sc], [rl, widx, sc])
            c.op('dve', lambda: V.tensor_reduce(out=bs[:, 0:1], in_=sc[:, 0:L], axis=AX.X, op=ALU.max, apply_absolute_value=True), [bs], [sc])
            c.op('dve', lambda: V.tensor_tensor(out=sc[:, L - 512:L], in0=sc[:, L - 512:L], in1=pen[:], op=ALU.add), [sc], [sc, pen])
            c.op('dve', lambda: V.tensor_scalar(out=bs[:, 2:3], in0=bs[:, 0:1], scalar1=-1.0, scalar2=-1.0, op0=ALU.mult, op1=ALU.add), [bs], [bs])
            c.op('dve', lambda: V.tensor_scalar(out=bs[:, 1:2], in0=bs[:, 0:1], scalar1=2.0, scalar2=2.0, op0=ALU.mult, op1=ALU.add), [bs], [bs])
            for it in range(NBIS):
                f = 2.0 ** (-(it + 1))
                c.op('dve', lambda: V.scalar_tensor_tensor(out=bs[:, 3:4], in0=bs[:, 1:2], scalar=f, in1=bs[:, 2:3], op0=ALU.mult, op1=ALU.add), [bs], [bs])
                c.op('dve', lambda: V.tensor_scalar(out=Mall[:, 0:L], in0=sc[:, 0:L], scalar1=bs[:, 3:4], scalar2=None, op0=ALU.is_ge, op1=ALU.add,
                                                    accum_out=bs[:, 4:5]), [Mall, bs], [sc, bs])
                c.op('dve', lambda: V.tensor_scalar(out=bs[:, 5:6], in0=bs[:, 4:5], scalar1=255.5, scalar2=f, op0=ALU.is_ge, op1=ALU.mult), [bs], [bs])
                c.op('dve', lambda: V.scalar_tensor_tensor(out=bs[:, 2:3], in0=bs[:, 5:6], scalar=bs[:, 1:2], in1=bs[:, 2:3], op0=ALU.mult, op1=ALU.add), [bs], [bs])
            c.op('dve', lambda: V.memset(bs[:, 7:8], 0.0), [bs], [bs])
            for cg in range(i + 1):
                c.op('dve', lambda: V.tensor_scalar(out=Mall[:, cg * 512:(cg + 1) * 512], in0=sc[:, cg * 512:(cg + 1) * 512], scalar1=bs[:, 2:3], scalar2=None,
                                                    op0=ALU.is_ge), [Mall], [sc, bs])
                c.op('dve', lambda: V.tensor_tensor(out=smtmp[:], in0=Mall[:, cg * 512:(cg + 1) * 512], in1=iota1[:], op=ALU.mult), [smtmp], [Mall, iota1])
                c.op('dve', lambda: V.tensor_reduce(out=sm[:, 0:1], in_=smtmp[:], axis=AX.X, op=ALU.max), [sm], [smtmp])
                c.op('dve', lambda: V.tensor_scalar(out=sm[:, 1:2], in0=sm[:, 0:1], scalar1=1.0, scalar2=512.0 * cg, op0=ALU.min, op1=ALU.mult), [sm], [sm])
                c.op('dve', lambda: V.tensor_tensor(out=sm[:, 2:3], in0=sm[:, 0:1], in1=sm[:, 1:2], op=ALU.add), [sm], [sm])
                c.op('dve', lambda: V.tensor_tensor(out=bs[:, 7:8], in0=bs[:, 7:8], in1=sm[:, 2:3], op=ALU.max), [bs], [bs, sm])
            if i == min(1, NO - 1):
                dbg_dump("d_bs", bs, bs[:], [128, 8]); dbg_dump("d_mall", Mall, Mall[:, 0:L], [128, L], BF16)
                dbg_dump("d_sc", sc, sc[:, 0:L], [128, L])
            c.op('dve', lambda: V.memset(base3[:], 1.0), [base3], [])
            c.op('dve', lambda: V.tensor_scalar(out=base3[:, 2:3], in0=bs[:, 7:8], scalar1=-1.0, scalar2=1.0, op0=ALU.mult, op1=ALU.add), [base3], [bs, base3])
            pm = PB[2]
            c.op('pe', lambda: PE.transpose(out=pm[0:8, 0:128], in_=base3[:, 0:8], identity=ident[:]), [pm], [base3, ident])
            c.op('dve', lambda: V.tensor_copy(out=QPb[:], in_=pm[0:3, 0:128]), [QPb], [pm])
            c.op('dve', lambda: V.tensor_tensor(out=QP[:], in0=QPb[:, :].unsqueeze(1).broadcast_to([3, 8, 128]),
                                                in1=sl3[:, :].unsqueeze(2).broadcast_to([3, 8, 128]), op=ALU.mult), [QP], [QPb, sl3])
            for cg in range(i + 1):
                ktg = KTg[pcnt[0] % 2]; v1g = V1g[pcnt[0] % 2]
                c.dma('sp', ktg[:], KTd.ap()[cg], [ktg], [KTdT])
                c.dma('sp', v1g[:], V1d.ap()[4 * cg:4 * cg + 4].rearrange("t p h e -> p t h e"), [v1g], [V1dT])
                for h in range(8):
                    pr_, hh = h // 2, h % 2
                    pq = PB[4 + (h % 2)]
                    for sb_ in range(4):
                        o_ = pq[:, sb_ * 128:(sb_ + 1) * 128]
                        c.op('pe', lambda: PE.matmul(o_, lhsT=ktg[hh * 64:(hh + 1) * 64, pr_, sb_ * 128:(sb_ + 1) * 128], rhs=QT[hh * 64:(hh + 1) * 64, pr_, :],
                                                     start=True, stop=False), [pq], [ktg, QT])
                        c.op('pe', lambda: PE.matmul(o_, lhsT=KPl[0:3, sb_ * 128:(sb_ + 1) * 128], rhs=QP[0:3, h, :],
                                                     start=False, stop=False), [pq], [KPl, QP])
                        c.op('pe', lambda: PE.matmul(o_, lhsT=Mall[:, cg * 512 + sb_ * 128:cg * 512 + (sb_ + 1) * 128], rhs=ibig[:],
                                                     start=False, stop=True), [pq], [Mall, ibig])
                    ptt = PT[h % 2]
                    bias_h = SLOPES[h] * 512.0 * cg - BIG / 8.0
                    c.op('act', lambda: A.activation(out=ptt[:].rearrange("p a t -> p (a t)"), in_=pq[:, :], func=AF.Exp, scale=0.125, bias=bias_h),
                         [ptt], [pq])
                    po = PB[6 + (h // 4)]
                    for sb_ in range(4):
                        c.op('pe', lambda: PE.matmul(po[:, (h % 4) * 65:(h % 4) * 65 + 65], lhsT=ptt[:, sb_, :], rhs=v1g[:, sb_, h, :],
                                                     start=(sb_ == 0), stop=(sb_ == 3)), [po], [ptt, v1g])
                    if h % 4 == 3:
                        av = acc[:, (h // 4) * 4:(h // 4) * 4 + 4, :].rearrange("p a e -> p (a e)")
                        if cg == 0:
                            c.op('dve', lambda: V.tensor_copy(out=av, in_=po[:, 0:260]), [acc], [po])
                        else:
                            c.op('dve', lambda: V.tensor_tensor(out=av, in0=av, in1=po[:, 0:260], op=ALU.add), [acc], [acc, po])
                pcnt[0] += 1
            c.op('dve', lambda: V.reciprocal(out=rec[:], in_=acc[:, :, 64]), [rec], [acc])
            c.op('dve', lambda: V.tensor_tensor(out=oat[:].rearrange("p (h e) -> p h e", h=8), in0=acc[:, :, 0:64],
                                                in1=rec[:, :].unsqueeze(2).broadcast_to([128, 8, 64]), op=ALU.mult), [oat], [acc, rec])
            if i == min(1, NO - 1):
                dbg_dump("d_oat", oat, oat[:], [128, 512])
            pt = PB[2]
            for k in range(4):
                c.op('pe', lambda: PE.transpose(out=pt[:, k * 128:(k + 1) * 128], in_=oat[:, k * 128:(k + 1) * 128], identity=ident[:]), [pt], [oat, ident])
            oatb = OATb[i % 2]
            c.op('act', lambda: A.copy(out=oatb[:].rearrange("p k t -> p (k t)"), in_=pt[:, :]), [oatb], [pt])
            c.dma('sp', OATd.ap()[i], oatb[:], [OATdT], [oatb])
        c.barrier()
    stK.close()
    if stop == 3:
        c.barrier()
        return nc, c
    with ExitStack() as st3:
        s3 = lambda shape, dt=F32: c.sb(st3, shape, dt)
        gt1_bc = s3([128, D]); gt2_bc = s3([128, D])
        with ExitStack() as stg_:
            silu_bc = c.sb(stg_, [128, 8, 128])
            c.op('dve', lambda: V.tensor_copy(out=silu_bc[:], in_=silu_c[:, :].unsqueeze(2).broadcast_to([128, 8, 128])), [silu_bc], [silu_c])
            ada_phase((2, 5), stg_, gts=(gt1_bc, gt2_bc), silu_bc=silu_bc)
            c.barrier()
        dbg_dump("d_gt1", gt1_bc, gt1_bc[:], [128, D]); dbg_dump("d_gt2", gt2_bc, gt2_bc[:], [128, D])
        xnTo = s3([128, 8, NO * 128], BF16)
        cw = s3([128, NO, 32])
        stM = ExitStack()
        mergedT = c.sb(stM, [128, 8, NO * 128], BF16)
        with ExitStack() as st3a:
            s3a = lambda shape, dt=F32: c.sb(st3a, shape, dt)
            for i in range(NO):
                norm_T(xo[i * 128:(i + 1) * 128, :], gmod1, sh1, xnTo, lambda k: xnTo[:, k, i * 128:(i + 1) * 128])
            OAT = s3a([128, 4, NO * 128], BF16)
            for i in range(NO):
                c.dma('sp', OAT[:, :, i * 128:(i + 1) * 128], OATd.ap()[i], [OAT], [OATdT])
            ORT = s3a([128, 4, NO * 128], BF16)
            GH = max(1, NG // 2)
            Gsb = s3a([128, GH, 4, 128], BF16)
            rds = [RDsts[k_].ap().rearrange("(p g t c) x -> p c g t x", p=4, g=CG, t=4, c=128) for k_ in range(NCH)]
            for p in range(4):
                for g0 in range(0, NG, GH):
                    for gq in range(g0, g0 + GH, 2):
                        g1 = min(gq + 2, g0 + GH)
                        c.dma('sp', Gsb[:, gq - g0:g1 - g0, :, :], rds[gq // CG][p, :, gq % CG:gq % CG + (g1 - gq), :, :], [Gsb], [RDstT])
                    dstv = ORT[:, p, g0 * 128:(g0 + GH) * 128].rearrange("c (g x) -> c g x", g=GH)
                    c.op('dve', lambda: V.tensor_scalar(out=dstv, in0=Gsb[:, :, 0, :], scalar1=sel_t[:, 0:1], scalar2=None, op0=ALU.mult), [ORT], [Gsb, sel_t])
                    for t in range(1, 4):
                        c.op('dve', lambda: V.scalar_tensor_tensor(out=dstv, in0=Gsb[:, :, t, :], scalar=sel_t[:, t:t + 1], in1=dstv, op0=ALU.mult, op1=ALU.add),
                             [ORT], [Gsb, sel_t, ORT])
            if stop == 30:
                c.barrier()
                return nc, c
            Wba = s3a([128, 4, D], BF16); Wbr = s3a([128, 4, D], BF16)
            load_w(Wba, lambda c0, c1: Wba[:, :, c0:c1], w_ba, 4, D)
            load_w(Wbr, lambda c0, c1: Wbr[:, :, c0:c1], w_br, 4, D)
            Wga = s3a([128, 8, 128], BF16); Wgr = s3a([128, 8, 128], BF16)
            sga = s3a([128, 512], BF16); sgr = s3a([128, 512], BF16); t1 = s3a([128, 512]); t2 = s3a([128, 512])
            NTG = (NO * 128 + 511) // 512
            for m in range(8):
                load_w(Wga, lambda c0, c1: Wga[:, :, c0:c1], w_ga[:, m * 128:(m + 1) * 128], 8, 128)
                load_w(Wgr, lambda c0, c1: Wgr[:, :, c0:c1], w_gr[:, m * 128:(m + 1) * 128], 8, 128)
                for tg in range(NTG):
                    t0 = tg * 512; t1e = min(NO * 128, t0 + 512); n = t1e - t0
                    pa, pr, pba, pbr = PB[2], PB[3], PB[4], PB[5]
                    for k in range(8):
                        c.op('pe', lambda: PE.matmul(pa[:, 0:n], lhsT=Wga[:, k, :], rhs=xnTo[:, k, t0:t1e], start=(k == 0), stop=(k == 7)), [pa], [Wga, xnTo])
                    for k in range(8):
                        c.op('pe', lambda: PE.matmul(pr[:, 0:n], lhsT=Wgr[:, k, :], rhs=xnTo[:, k, t0:t1e], start=(k == 0), stop=(k == 7)), [pr], [Wgr, xnTo])
                    for k in range(4):
                        c.op('pe', lambda: PE.matmul(pba[:, 0:n], lhsT=Wba[:, k, m * 128:(m + 1) * 128], rhs=OAT[:, k, t0:t1e], start=(k == 0), stop=(k == 3)),
                             [pba], [Wba, OAT])
                    for k in range(4):
                        c.op('pe', lambda: PE.matmul(pbr[:, 0:n], lhsT=Wbr[:, k, m * 128:(m + 1) * 128], rhs=ORT[:, k, t0:t1e], start=(k == 0), stop=(k == 3)),
                             [pbr], [Wbr, ORT])
                    c.op('act', lambda: A.activation(out=sga[:, 0:n], in_=pa[:, 0:n], func=AF.Sigmoid), [sga], [pa])
                    c.op('act', lambda: A.activation(out=sgr[:, 0:n], in_=pr[:, 0:n], func=AF.Sigmoid), [sgr], [pr])
                    c.op('dve', lambda: V.tensor_tensor(out=t1[:, 0:n], in0=sga[:, 0:n], in1=pba[:, 0:n], op=ALU.mult), [t1], [sga, pba])
                    c.op('dve', lambda: V.tensor_tensor(out=t2[:, 0:n], in0=sgr[:, 0:n], in1=pbr[:, 0:n], op=ALU.mult), [t2], [sgr, pbr])
                    c.op('pool', lambda: G.tensor_tensor(out=mergedT[:, m, t0:t1e], in0=t1[:, 0:n], in1=t2[:, 0:n], op=ALU.add), [mergedT], [t1, t2])
            c.barrier()
        if stop == 31:
            c.barrier()
            return nc, c
        dbg_dump("d_merged", mergedT, mergedT[:], [128, 8, NO * 128], BF16)
        with ExitStack() as st3b:
            s3b = lambda shape, dt=F32: c.sb(st3b, shape, dt)
            Wout = s3b([128, 8, D], BF16); Wrt = s3b([128, 8, 36], BF16)
            load_w(Wout, lambda c0, c1: Wout[:, :, c0:c1], w_out, 8, D)
            load_w(Wrt, lambda c0, c1: Wrt[:, :, c0:c1], w_rt, 8, 36)
            h1t = s3b([128, D]); xot = s3b([128, D]); tmpo = s3b([128, 512])
            lg = s3b([128, 36]); r8 = s3b([128, 16]); ohg = s3b([128, 4]); peng = s3b([128, 4]); el = s3b([128, 32]); el2 = s3b([128, 32])
            oh1 = s3b([128, 32]); oh2 = s3b([128, 32]); ge4 = s3b([128, 4])
            for i in range(NO):
                c.dma('sp', xot[:], xo[i * 128:(i + 1) * 128, :], [xot], [])
                for half in range(2):
                    po = PB[6 + half]
                    for k in range(8):
                        c.op('pe', lambda: PE.matmul(po[:, :], lhsT=mergedT[:, k, i * 128:(i + 1) * 128], rhs=Wout[:, k, half * 512:(half + 1) * 512],
                                                     start=(k == 0), stop=(k == 7)), [po], [mergedT, Wout])
                    c.op('dve', lambda: V.tensor_tensor(out=tmpo[:], in0=po[:, :], in1=gt1_bc[:, half * 512:(half + 1) * 512], op=ALU.mult), [tmpo], [po, gt1_bc])
                    c.op('dve', lambda: V.tensor_tensor(out=h1t[:, half * 512:(half + 1) * 512], in0=tmpo[:], in1=xot[:, half * 512:(half + 1) * 512], op=ALU.add),
                         [h1t], [tmpo, xot])
                c.dma('sp', out.ap()[i * 128:(i + 1) * 128, :], h1t[:], [outT], [h1t])
                norm_T(None, gmod2, sh2, xnTo, lambda k: xnTo[:, k, i * 128:(i + 1) * 128], keep_x=h1t)
                pm = PB[2]
                for k in range(8):
                    c.op('pe', lambda: PE.matmul(pm[:, 0:36], lhsT=xnTo[:, k, i * 128:(i + 1) * 128], rhs=Wrt[:, k, :], start=(k == 0), stop=(k == 7)), [pm], [xnTo, Wrt])
                c.op('dve', lambda: V.tensor_copy(out=lg[:], in_=pm[:, 0:36]), [lg], [pm])
                c.op('dve', lambda: V.tensor_reduce(out=r8[:, 0:1], in_=lg[:, 0:4], axis=AX.X, op=ALU.max), [r8], [lg])
                c.op('dve', lambda: V.tensor_scalar(out=r8[:, 1:2], in0=r8[:, 0:1], scalar1=-1.0, scalar2=None, op0=ALU.mult), [r8], [r8])
                c.op('act', lambda: A.activation(out=ge4[:], in_=lg[:, 0:4], func=AF.Exp, bias=r8[:, 1:2], scale=1.0, accum_out=r8[:, 2:3]), [ge4, r8], [lg, r8])
                c.op('dve', lambda: V.reciprocal(out=r8[:, 3:4], in_=r8[:, 2:3]), [r8], [r8])
                c.op('dve', lambda: V.tensor_scalar(out=ohg[:], in0=lg[:, 0:4], scalar1=r8[:, 0:1], scalar2=None, op0=ALU.is_equal), [ohg], [lg, r8])
                c.op('dve', lambda: V.tensor_scalar(out=peng[:], in0=ohg[:], scalar1=1.0, scalar2=1e30, op0=ALU.subtract, op1=ALU.mult), [peng], [ohg])
                c.op('dve', lambda: V.tensor_tensor(out=el[:], in0=lg[:, 4:36], in1=ebias_t[:], op=ALU.add), [el], [lg, ebias_t])
                c.op('dve', lambda: V.tensor_tensor(out=el[:].rearrange("p (g e) -> p g e", g=4), in0=el[:].rearrange("p (g e) -> p g e", g=4),
                                                    in1=peng[:, :].unsqueeze(2).broadcast_to([128, 4, 8]), op=ALU.add), [el], [el, peng])
                c.op('dve', lambda: V.tensor_reduce(out=r8[:, 4:5], in_=el[:], axis=AX.X, op=ALU.max), [r8], [el])
                c.op('dve', lambda: V.tensor_scalar(out=oh1[:], in0=el[:], scalar1=r8[:, 4:5], scalar2=None, op0=ALU.is_equal), [oh1], [el, r8])
                c.op('dve', lambda: V.scalar_tensor_tensor(out=el2[:], in0=oh1[:], scalar=-1e30, in1=el[:], op0=ALU.mult, op1=ALU.add), [el2], [oh1, el])
                c.op('dve', lambda: V.tensor_reduce(out=r8[:, 5:6], in_=el2[:], axis=AX.X, op=ALU.max), [r8], [el2])
                c.op('dve', lambda: V.tensor_scalar(out=oh2[:], in0=el2[:], scalar1=r8[:, 5:6], scalar2=None, op0=ALU.is_equal), [oh2], [el2, r8])
                c.op('dve', lambda: V.tensor_tensor(out=r8[:, 6:7], in0=r8[:, 4:5], in1=r8[:, 5:6], op=ALU.subtract), [r8], [r8])
                c.op('act', lambda: A.activation(out=r8[:, 7:8], in_=r8[:, 6:7], func=AF.Sigmoid), [r8], [r8])
                c.op('dve', lambda: V.tensor_scalar(out=r8[:, 8:9], in0=r8[:, 7:8], scalar1=-1.0, scalar2=1.0, op0=ALU.mult, op1=ALU.add), [r8], [r8])
                c.op('dve', lambda: V.tensor_tensor(out=r8[:, 9:10], in0=r8[:, 7:8], in1=r8[:, 3:4], op=ALU.mult), [r8], [r8])
                c.op('dve', lambda: V.tensor_tensor(out=r8[:, 10:11], in0=r8[:, 8:9], in1=r8[:, 3:4], op=ALU.mult), [r8], [r8])
                c.op('dve', lambda: V.tensor_scalar(out=cw[:, i, :], in0=oh1[:], scalar1=r8[:, 9:10], scalar2=None, op0=ALU.mult), [cw], [oh1, r8])
                c.op('dve', lambda: V.scalar_tensor_tensor(out=cw[:, i, :], in0=oh2[:], scalar=r8[:, 10:11], in1=cw[:, i, :], op0=ALU.mult, op1=ALU.add),
                     [cw], [oh2, r8, cw])
            c.barrier()
        if stop == 32:
            c.barrier()
            return nc, c
        dbg_dump("d_cw", cw, cw[:], [128, NO, 32])
        stM.close()
        with ExitStack() as st3c:
            s3c = lambda shape, dt=F32: c.sb(st3c, shape, dt)
            accm = s3c([128, NO, D], BF16)
            c.op('pool', lambda: G.memset(accm[:], 0.0), [accm], [])
            Wg = [s3c([128, 8, 512], BF16) for _ in range(2)]; Wu = [s3c([128, 8, 512], BF16) for _ in range(2)]
            Wd = [s3c([128, 4, D], BF16) for _ in range(2)]
            hdn = [s3c([128, 4, 512], BF16) for _ in range(2)]; sgb = [s3c([128, 512], BF16) for _ in range(2)]
            NTG = (NO * 128 + 511) // 512
            cnt = 0
            for e in range(NE):
                wg, wu, wd = Wg[e % 2], Wu[e % 2], Wd[e % 2]
                load_w(wg, lambda c0, c1: wg[:, :, c0:c1], ew_g[e], 8, 512)
                load_w(wu, lambda c0, c1: wu[:, :, c0:c1], ew_u[e], 8, 512)
                load_w(wd, lambda c0, c1: wd[:, :, c0:c1], ew_d[e], 4, D)
                for tg in range(NTG):
                    t0 = tg * 512; t1e = min(NO * 128, t0 + 512); n = t1e - t0
                    hb = hdn[tg % 2]
                    for f in range(4):
                        pg_, pu_ = PB[2 + (cnt % 2)], PB[4 + (cnt % 2)]
                        sg_ = sgb[cnt % 2]
                        cnt += 1
                        for k in range(8):
                            c.op('pe', lambda: PE.matmul(pg_[:, 0:n], lhsT=wg[:, k, f * 128:(f + 1) * 128], rhs=xnTo[:, k, t0:t1e], start=(k == 0), stop=(k == 7)),
                                 [pg_], [wg, xnTo])
                        for k in range(8):
                            c.op('pe', lambda: PE.matmul(pu_[:, 0:n], lhsT=wu[:, k, f * 128:(f + 1) * 128], rhs=xnTo[:, k, t0:t1e], start=(k == 0), stop=(k == 7)),
                                 [pu_], [wu, xnTo])
                        c.op('act', lambda: A.activation(out=sg_[:, 0:n], in_=pg_[:, 0:n], func=AF.Silu), [sg_], [pg_])
                        c.op('dve', lambda: V.tensor_tensor(out=hb[:, f, 0:n], in0=sg_[:, 0:n], in1=pu_[:, 0:n], op=ALU.mult), [hb], [sg_, pu_])
                    for tt in range(n // 128):
                        tile = tg * 4 + tt
                        for half in range(2):
                            pd = PB[6 + half]
                            for f in range(4):
                                c.op('pe', lambda: PE.matmul(pd[:, :], lhsT=hb[:, f, tt * 128:(tt + 1) * 128], rhs=wd[:, f, half * 512:(half + 1) * 512],
                                                             start=(f == 0), stop=(f == 3)), [pd], [hb, wd])
                            av = accm[:, tile, half * 512:(half + 1) * 512]
                            c.op('dve', lambda: V.scalar_tensor_tensor(out=av, in0=pd[:, :], scalar=cw[:, tile, e:e + 1], in1=av, op0=ALU.mult, op1=ALU.add),
                                 [accm], [pd, cw, accm])
            if stop == 33:
                c.barrier()
                return nc, c
            h1b = [s3c([128, D]) for _ in range(2)]; ob = [s3c([128, D]) for _ in range(2)]
            for i in range(NO):
                hb_, o_ = h1b[i % 2], ob[i % 2]
                c.dma('sp', hb_[:], out.ap()[i * 128:(i + 1) * 128, :], [hb_], [outT])
                c.op('dve', lambda: V.tensor_tensor(out=o_[:], in0=accm[:, i, :], in1=gt2_bc[:], op=ALU.mult), [o_], [accm, gt2_bc])
                c.op('pool', lambda: G.tensor_tensor(out=o_[:], in0=o_[:], in1=hb_[:], op=ALU.add), [o_], [o_, hb_])
                c.dma('sp', out.ap()[i * 128:(i + 1) * 128, :], o_[:], [outT], [o_])
            c.barrier()
    import os as _os
    for _ in range(int(_os.environ.get("PAD_PE", "0"))):
        c.op('pe', lambda: PE.matmul(PB[0][:, 0:1], lhsT=ident[:, 0:128], rhs=ident[:, 0:1], start=True, stop=True), [PB[0]], [ident])
    for _ in range(int(_os.environ.get("PAD_ACT", "0"))):
        c.op('act', lambda: A.copy(out=st4[:, 0:1], in_=st4[:, 1:2]), [st4], [])
    c.barrier()
    glob.close()
    return nc, c


_IN_W_OFF = dict(q_a=0, kv=512, q_i=768, k_i=1024, w_i=1056, r=1064, k=1576, v=2088, wl=2600, al=2664, gl=2728, ga=2888, gr=3912)


def _prep_inputs(inp, NG=16, NE=32):
    S = 512 * NG
    f = lambda a: np.ascontiguousarray(np.asarray(a, dtype=np.float32))
    x = f(inp["x"]); cvec = f(inp["c"])
    w_in = f(inp["w_in"])[0]
    O = _IN_W_OFF
    col = lambda a, n: w_in[:, a:a + n]
    r128 = lambda v: f(v.reshape(-1, 128).T)
    mu = f(inp["rwkv_mu"])[0]
    shared = {
        "ada_w": f(inp["ada_w"])[0], "ada_b": f(inp["ada_b"])[0].reshape(1, -1),
        "mixg": r128(f(inp["mix_norm_g"])[0]), "moeg": r128(f(inp["moe_norm_g"])[0]),
        "w_kv": f(col(O["kv"], 256)), "w_ki": f(np.tile(col(O["k_i"], 32), (1, 4))),
        "w_q": f(np.concatenate([col(O["q_a"], 512), col(O["q_i"], 256), col(O["w_i"], 8)], axis=1)),
        "kvg": r128(f(inp["kv_norm_g"])[0]), "w_kvup": f(inp["w_kv_up"])[0],
        "qg": f(np.tile(f(inp["q_norm_g"])[0], 2).reshape(128, 1)), "kg": f(np.tile(f(inp["k_norm_g"])[0], 2).reshape(128, 1)),
        "w_ga": f(col(O["ga"], 1024)), "w_gr": f(col(O["gr"], 1024)),
        "w_ba": f(inp["w_branch_attn"])[0], "w_br": f(inp["w_branch_rwkv"])[0], "w_out": f(inp["w_out"])[0],
        "w_rt": f(np.concatenate([f(inp["router_group_w"])[0], f(inp["router_expert_w"])[0]], axis=1)),
        "e_bias": f(inp["router_expert_bias"])[0].reshape(1, 32),
    }
    for e in range(NE):
        shared["ew_g%d" % e] = f(inp["expert_w_gate"][0, e]); shared["ew_u%d" % e] = f(inp["expert_w_up"][0, e])
        shared["ew_d%d" % e] = f(inp["expert_w_down"][0, e])
    maps = []
    for cid in range(8):
        b, q = cid // 4, cid % 4
        m = dict(shared)
        for i_ in range((4 * NG + 31) // 32):
            m["xb%d" % i_] = f(x[b, i_ * 4096:min(S, (i_ + 1) * 4096)])
        own = [q + 4 * i for i in range(NG)]
        m["xo"] = f(np.concatenate([x[b, t * 128:(t + 1) * 128] for t in own], axis=0))
        m["cb"] = r128(cvec[b])
        m["qrel"] = f((128 * q + np.arange(128)).reshape(128, 1))
        sel = np.zeros((128, 4), np.float32); sel[:, q] = 1.0
        m["sel"] = sel
        h0 = 2 * q
        rcols = [col(O["r"] + (h0 + h) * 64, 64) for h in range(2)]
        kcols = [col(O["k"] + (h0 + h) * 64, 64) for h in range(2)]
        vcols = [col(O["v"] + (h0 + h) * 64, 64) for h in range(2)]
        m["w_rw"] = f(np.concatenate(rcols + kcols + vcols + [col(O["wl"], 64), col(O["al"], 64), col(O["gl"], 160)], axis=1))
        mu_rw = np.zeros((128, 10), np.float32)
        for h in range(2):
            mu_rw[0:64, 0 + h] = mu[(h0 + h) * 64:(h0 + h + 1) * 64]
            mu_rw[0:64, 2 + h] = mu[512 + (h0 + h) * 64:512 + (h0 + h + 1) * 64]
            mu_rw[0:64, 4 + h] = mu[1024 + (h0 + h) * 64:1024 + (h0 + h + 1) * 64]
        mu_rw[0:64, 6] = mu[1536:1600]; mu_rw[0:64, 7] = mu[1600:1664]
        mu_rw[0:128, 8] = mu[1664:1792]; mu_rw[0:32, 9] = mu[1792:1824]
        m["mu_rw"] = mu_rw
        rwv = np.zeros((64, 2, 8), np.float32)
        for h in range(2):
            sl = slice((h0 + h) * 64, (h0 + h + 1) * 64)
            rwv[:, h, 0] = f(inp["rwkv_w0"])[0][sl]; rwv[:, h, 1] = f(inp["rwkv_a0"])[0][sl]
            rwv[:, h, 2] = f(inp["rwkv_k_k"])[0][sl]; rwv[:, h, 3] = f(inp["rwkv_k_a"])[0][sl]
            rwv[:, h, 4] = f(inp["rwkv_r_k"])[0][h0 + h]; rwv[:, h, 5] = f(inp["rwkv_ln_w"])[0][sl]
            rwv[:, h, 6] = f(inp["rwkv_ln_b"])[0][sl]
        m["rwv"] = rwv
        hs = slice(h0 * 64, h0 * 64 + 128)
        m["w2"] = f(f(inp["rwkv_w2"])[0][:, hs]); m["a2"] = f(f(inp["rwkv_a2"])[0][:, hs]); m["g2"] = f(f(inp["rwkv_g2"])[0][:, hs])
        maps.append(m)
    return maps


_CACHE = {}


def kernel(**inputs):
    NG = 16
    if NG not in _CACHE:
        _CACHE[NG] = build_program(NG)[0]
    nc = _CACHE[NG]
    maps = _prep_inputs(inputs, NG)
    res = run_bass_kernel_spmd(nc, maps, core_ids=list(range(8)))
    out = np.zeros((2, 512 * NG, D), np.float32)
    for cid in range(8):
        b, q = cid // 4, cid % 4
        o = np.asarray(res.results[cid]["out"], dtype=np.float32)
        for i in range(NG):
            t = q + 4 * i
            out[b, t * 128:(t + 1) * 128] = o[i * 128:(i + 1) * 128]
    return out
```

```python
from contextlib import ExitStack
import numpy as np
import concourse.bass as bass
import concourse.mybir as mybir
from concourse.bass_utils import run_bass_kernel_spmd

F32 = mybir.dt.float32
BF16 = mybir.dt.bfloat16
AF = mybir.ActivationFunctionType
ALU = mybir.AluOpType
AX = mybir.AxisListType

D = 1024
NBIS = 22
NORM_EPS = 1e-6
GN_EPS = 64e-5
SLOPES = [2.0 ** (-(h + 1)) for h in range(8)]
BIG = 262144.0


class T:
    def __init__(self, h, name):
        self.h = h
        self.name = name
        self.w = None
        self.r = {}

    def __getitem__(self, idx):
        return self.h[idx]


class Ctx:
    NDMA = 32

    def __init__(self, nc):
        self.nc = nc
        self.eng = {'pe': nc.tensor, 'act': nc.scalar, 'dve': nc.vector, 'pool': nc.gpsimd, 'sp': nc.sync}
        self.sem = {}
        self.cnt = {}
        self.known = {}
        for k in self.eng:
            self.sem[k] = nc.alloc_semaphore("s_" + k)
            self.cnt[k] = 0
            self.known[k] = {}
        self.dma_n = 0
        for i in range(self.NDMA):
            self.sem['dma%d' % i] = nc.alloc_semaphore("s_dma%d" % i)
        self.sem['cc'] = nc.alloc_semaphore("s_cc")
        self.uid = 0
        self.ninst = 0
        self.nw = {k: 0 for k in self.eng}
        self.snaps = {}
        self.snapq = []
        self.nd = {k: 0 for k in self.eng}

    def sb(self, stack, shape, dt=F32, name=None):
        self.uid += 1
        name = name or ("t%d" % self.uid)
        h = stack.enter_context(self.nc.sbuf_tensor(name, list(shape), dt))
        return T(h, name)

    def _learn(self, e, key, val):
        kn = self.known[e]
        if kn.get(key, 0) < val:
            kn[key] = val
        sn = self.snaps.get((key, val))
        if sn is not None:
            for k2, v2 in sn.items():
                if kn.get(k2, 0) < v2:
                    kn[k2] = v2

    def _wait(self, e, key, val):
        if key == e and e in ('pe', 'sp'):
            return
        kn = self.known[e]
        if kn.get(key, 0) >= val:
            return
        self.eng[e].wait_ge(self.sem[key], val)
        self._learn(e, key, val)
        self.ninst += 1
        self.nw[e] += 1

    def _snap(self, e, ev):
        self.snaps[ev] = dict(self.known[e])
        self.snapq.append(ev)
        if len(self.snapq) > 20000:
            old = self.snapq.pop(0)
            self.snaps.pop(old, None)

    def _deps(self, e, outs, ins):
        for t in ins:
            if t is not None and t.w is not None:
                self._wait(e, *t.w)
        for t in outs:
            if t.w is not None:
                self._wait(e, *t.w)
            for k, v in t.r.items():
                self._wait(e, k, v)

    def _done(self, ev, outs, ins):
        for t in ins:
            if t is not None:
                t.r[ev[0]] = max(ev[1], t.r.get(ev[0], 0))
        for t in outs:
            t.w = ev
            t.r = {}

    def op(self, e, fn, outs, ins):
        self._deps(e, outs, ins)
        inst = fn()
        self.cnt[e] += 1
        inst.then_inc(self.sem[e], 1)
        self.ninst += 1
        self._snap(e, (e, self.cnt[e]))
        self._done((e, self.cnt[e]), outs, ins)

    def dma(self, e, out_ap, in_ap, outs, ins, **kw):
        n = self.dma_n
        self.dma_n += 1
        key = 'dma%d' % (n % self.NDMA)
        rnd = n // self.NDMA
        if rnd > 0:
            self._wait(e, key, 16 * rnd)
        self._deps(e, outs, ins)
        inst = self.eng[e].dma_start(out=out_ap, in_=in_ap, **kw)
        self.nd[e] += 1
        inst.then_inc(self.sem[key], 16)
        self.ninst += 1
        ev = (key, 16 * (rnd + 1))
        self._snap(e, ev)
        self._done(ev, outs, ins)
        return ev

    def barrier(self):
        cur = {k: self.cnt[k] for k in ('pe', 'act', 'dve', 'pool')}
        for i in range(self.NDMA):
            total = (self.dma_n - i + self.NDMA - 1) // self.NDMA
            if total > 0:
                cur['dma%d' % i] = 16 * total
        for e in ('pe', 'act', 'dve', 'pool', 'sp'):
            for k, v in cur.items():
                if v > 0 and k != e:
                    self._wait(e, k, v)
            if e in cur and cur[e] > 0 and e not in ('pe',):
                self._wait(e, e, cur[e])


def build_program(NG=16, dbg=False, stop=99, NE=32):
    NT = 4 * NG; NO = NG; S = 512 * NG
    nc = bass.Bass("TRN2", target_bir_lowering=False)
    c = Ctx(nc)
    V, A, P, G, PE = nc.vector, nc.scalar, nc.gpsimd, nc.gpsimd, nc.tensor

    def din(name, shape, dt=F32):
        return nc.dram_tensor(name, list(shape), dt, kind="ExternalInput").ap()

    NXB = (NT + 31) // 32
    xbs = [din("xb%d" % i, [min(32, NT - 32 * i) * 128, D]) for i in range(NXB)]
    xo = din("xo", [NO * 128, D]); cb = din("cb", [128, 8])
    qrel = din("qrel", [128, 1]); sel = din("sel", [128, 4])
    ada_w = din("ada_w", [D, 6 * D]); ada_b = din("ada_b", [1, 6 * D])
    mixg = din("mixg", [128, 8]); moeg = din("moeg", [128, 8])
    w_rw = din("w_rw", [D, 672]); mu_rw = din("mu_rw", [128, 10])
    w_kv = din("w_kv", [D, 256]); w_ki = din("w_ki", [D, 128]); w_q = din("w_q", [D, 776])
    kvg = din("kvg", [128, 2]); w_kvup = din("w_kvup", [256, 1024])
    qg = din("qg", [128, 1]); kg = din("kg", [128, 1])
    rwv = din("rwv", [64, 2, 8])
    w2 = din("w2", [64, 128]); a2 = din("a2", [64, 128]); g2 = din("g2", [160, 128])
    w_ga = din("w_ga", [D, D]); w_gr = din("w_gr", [D, D])
    w_ba = din("w_ba", [512, D]); w_br = din("w_br", [512, D]); w_out = din("w_out", [D, D])
    w_rt = din("w_rt", [D, 36]); e_bias = din("e_bias", [1, 32])
    ew_g = [din("ew_g%d" % e, [D, 512]) for e in range(NE)]
    ew_u = [din("ew_u%d" % e, [D, 512]) for e in range(NE)]
    ew_d = [din("ew_d%d" % e, [512, D]) for e in range(NE)]
    out = nc.dram_tensor("out", [NO * 128, D], F32, kind="ExternalOutput")
    outT = T(out, "out")
    KTd = nc.dram_tensor("KTd", [NG, 128, 4, 512], BF16, kind="ExternalOutput"); KTdT = T(KTd, "KTd")
    V1d = nc.dram_tensor("V1d", [NT, 128, 8, 65], BF16, kind="ExternalOutput"); V1dT = T(V1d, "V1d")
    CG = min(NG, 4); NCH = NG // CG
    RSrcs = [nc.dram_tensor("RSrc%d" % k, [CG * 4 * 128, 128], BF16, kind="Internal") for k in range(NCH)]
    RSrcT = T(None, "RSrc")
    OATd = nc.dram_tensor("OATd", [NO, 128, 4, 128], BF16, kind="ExternalOutput"); OATdT = T(OATd, "OATd")
    RDsts = [nc.dram_tensor("RDst%d" % k, [4 * CG * 4 * 128, 128], BF16, kind="Internal") for k in range(NCH)]
    RDstT = T(None, "RDst")

    glob = ExitStack()
    PB = []
    for i in range(8):
        h = glob.enter_context(nc.psum_tensor("pb%d" % i, [128, 512], F32))
        PB.append(T(h, "pb%d" % i))

    sb = lambda shape, dt=F32, st=glob: c.sb(st, shape, dt)

    ident = sb([128, 128]); identb = sb([128, 128], BF16); ibig = sb([128, 128], BF16)
    ones64 = sb([64, 64]); onesr = sb([1, 128]); one11 = sb([1, 1])
    idrep = sb([128, 32], BF16)
    c.op('pool', lambda: G.memset(ident[:], 1.0), [ident], [])
    c.op('pool', lambda: G.affine_select(out=ident[:], in_=ident[:], pattern=[[-1, 128]], compare_op=ALU.is_equal,
                                         fill=0.0, base=0, channel_multiplier=1), [ident], [ident])
    c.op('dve', lambda: V.tensor_copy(out=identb[:], in_=ident[:]), [identb], [ident])
    c.op('dve', lambda: V.tensor_scalar(out=ibig[:], in0=ident[:], scalar1=BIG, scalar2=None, op0=ALU.mult), [ibig], [ident])
    c.op('dve', lambda: V.memset(ones64[:], 1.0 / 64.0), [ones64], [])
    c.op('dve', lambda: V.memset(onesr[:], 1.0), [onesr], [])
    c.op('dve', lambda: V.memset(one11[:], 1.0), [one11], [])
    idr32 = sb([128, 32])
    c.op('dve', lambda: V.tensor_tensor(out=idr32[:], in0=ident[:, 0:32], in1=ident[:, 32:64], op=ALU.add), [idr32], [ident])
    c.op('dve', lambda: V.tensor_tensor(out=idr32[:], in0=idr32[:], in1=ident[:, 64:96], op=ALU.add), [idr32], [idr32, ident])
    c.op('dve', lambda: V.tensor_tensor(out=idrep[:], in0=idr32[:], in1=ident[:, 96:128], op=ALU.add), [idrep], [idr32, ident])

    def load_small(ap, shape):
        t = sb(shape)
        c.dma('sp', t[:], ap, [t], [])
        return t
    cbt = load_small(cb, [128, 8]); qrel_t = load_small(qrel, [128, 1]); sel_t = load_small(sel, [128, 4])
    mixg_t = load_small(mixg, [128, 8]); moeg_t = load_small(moeg, [128, 8]); mu_t = load_small(mu_rw, [128, 10])
    kvg_t = load_small(kvg, [128, 2]); qg_t = load_small(qg, [128, 1]); kg_t = load_small(kg, [128, 1])
    rwv_t = load_small(rwv, [64, 2, 8])
    ebias_t = sb([128, 32])
    c.dma('sp', ebias_t[:], e_bias[0:1, :].broadcast_to([128, 32]), [ebias_t], [])

    stg = [sb([128, 1024]) for _ in range(2)]
    stg_i = [0]

    def load_w(dst, dst_ap_fn, src, nk, ncols, eng_cast=('pool', 'act')):
        cbk = max(1, min(ncols, 1024 // nk))
        for c0 in range(0, ncols, cbk):
            c1 = min(ncols, c0 + cbk)
            st = stg[stg_i[0] % 2]; stg_i[0] += 1
            view = st[:, 0:nk * (c1 - c0)].rearrange("p (k f) -> p k f", k=nk)
            c.dma('sp', view, src[:, c0:c1].rearrange("(k p) f -> p k f", p=128), [st], [])
            e = eng_cast[stg_i[0] % len(eng_cast)]
            if e == 'act':
                c.op('act', lambda: A.copy(out=dst_ap_fn(c0, c1), in_=view), [dst], [st])
            else:
                c.op('pool', lambda: G.tensor_copy(out=dst_ap_fn(c0, c1), in_=view), [dst], [st])

    silu_c = sb([128, 8])
    c.op('act', lambda: A.activation(out=silu_c[:], in_=cbt[:], func=AF.Silu), [silu_c], [cbt])
    gmod1 = sb([128, 8]); sh1 = sb([128, 8]); gmod2 = sb([128, 8]); sh2 = sb([128, 8])

    def ada_phase(js, stk, gts=None, silu_bc=None):
        blk = [c.sb(stk, [128, 8, 1024]) for _ in range(2)]
        adab_t = c.sb(stk, [1, 6 * D])
        c.dma('sp', adab_t[:], ada_b, [adab_t], [])
        for jj, j in enumerate(js):
            bt = blk[jj % 2]
            for kk in range(2):
                c.dma('sp', bt[:, 4 * kk:4 * kk + 4, :],
                      ada_w[512 * kk:512 * kk + 512, j * 1024:(j + 1) * 1024].rearrange("(k p) f -> p k f", p=128), [bt], [])
            if j in (0, 1, 3, 4):
                pm = PB[j % 2]
                for m in range(8):
                    for k in range(8):
                        c.op('pe', lambda: PE.matmul(pm[:, m:m + 1], lhsT=bt[:, k, m * 128:(m + 1) * 128], rhs=silu_c[:, k:k + 1],
                                                     start=(k == 0), stop=False), [pm], [bt, silu_c])
                    c.op('pe', lambda: PE.matmul(pm[:, m:m + 1], lhsT=adab_t[0:1, j * 1024 + m * 128:j * 1024 + (m + 1) * 128],
                                                 rhs=one11[0:1, 0:1], start=False, stop=True), [pm], [adab_t, one11])
                if j == 0:
                    c.op('dve', lambda: V.tensor_copy(out=sh1[:], in_=pm[:, 0:8]), [sh1], [pm])
                elif j == 3:
                    c.op('dve', lambda: V.tensor_copy(out=sh2[:], in_=pm[:, 0:8]), [sh2], [pm])
                else:
                    gm, gg = (gmod1, mixg_t) if j == 1 else (gmod2, moeg_t)
                    c.op('dve', lambda: V.scalar_tensor_tensor(out=gm[:], in0=pm[:, 0:8], scalar=1.0, in1=gg[:], op0=ALU.add, op1=ALU.mult),
                         [gm], [pm, gg])
            else:
                gt = gts[0] if j == 2 else gts[1]
                for half in range(2):
                    pm = PB[2 + half]
                    for k in range(8):
                        c.op('pe', lambda: PE.matmul(pm[:, :], lhsT=silu_bc[:, k, :], rhs=bt[:, k, half * 512:(half + 1) * 512],
                                                     start=(k == 0), stop=False), [pm], [bt, silu_bc])
                    c.op('pe', lambda: PE.matmul(pm[:, :], lhsT=onesr[0:1, :], rhs=adab_t[0:1, j * 1024 + half * 512:j * 1024 + (half + 1) * 512],
                                                 start=False, stop=True), [pm], [adab_t, onesr])
                    c.op('act', lambda: A.copy(out=gt[:, half * 512:(half + 1) * 512], in_=pm[:, :]), [gt], [pm])

    with ExitStack() as st0:
        ada_phase((0, 1, 3, 4), st0)
        c.barrier()
    if stop == 0:
        return nc, c

    xt_buf = [sb([128, D]) for _ in range(2)]
    xs_buf = sb([128, D]); st4 = sb([128, 4])
    xcnt = [0]
    dbg_outs = {}

    def dbg_dump(name, t, ap, shape, dt=F32):
        if not dbg:
            return
        d = nc.dram_tensor(name, list(shape), dt, kind="ExternalOutput")
        c.dma('sp', d.ap(), ap, [], [t])

    def norm_T(src_dram_ap, gm, sh, dst, dst_ap_fn, keep_x=None):
        xt = keep_x if keep_x is not None else xt_buf[xcnt[0] % 2]
        xcnt[0] += 1
        if src_dram_ap is not None:
            c.dma('sp', xt[:], src_dram_ap, [xt], [])
        c.op('act', lambda: A.activation(out=xs_buf[:], in_=xt[:], func=AF.Square, accum_out=st4[:, 0:1]), [xs_buf, st4], [xt])
        c.op('dve', lambda: V.tensor_scalar(out=st4[:, 1:2], in0=st4[:, 0:1], scalar1=1.0 / D, scalar2=NORM_EPS, op0=ALU.mult, op1=ALU.add), [st4], [st4])
        c.op('act', lambda: A.activation(out=st4[:, 2:3], in_=st4[:, 1:2], func=AF.Sqrt), [st4], [st4])
        c.op('dve', lambda: V.reciprocal(out=st4[:, 3:4], in_=st4[:, 2:3]), [st4], [st4])
        c.op('act', lambda: A.activation(out=xs_buf[:], in_=xt[:], func=AF.Copy, scale=st4[:, 3:4]), [xs_buf], [xt, st4])
        for half in range(2):
            pm = PB[half]
            for kq in range(4):
                k = half * 4 + kq
                c.op('pe', lambda: PE.transpose(out=pm[:, kq * 128:(kq + 1) * 128], in_=xs_buf[:, k * 128:(k + 1) * 128], identity=ident[:]),
                     [pm], [xs_buf, ident])
            for kq in range(4):
                k = half * 4 + kq
                if kq % 2 == 0:
                    c.op('dve', lambda: V.tensor_scalar(out=dst_ap_fn(k), in0=pm[:, kq * 128:(kq + 1) * 128], scalar1=gm[:, k:k + 1],
                                                        scalar2=sh[:, k:k + 1], op0=ALU.mult, op1=ALU.add), [dst], [pm, gm, sh])
                else:
                    c.op('act', lambda: A.activation(out=dst_ap_fn(k), in_=pm[:, kq * 128:(kq + 1) * 128], func=AF.Identity,
                                                     scale=gm[:, k:k + 1], bias=sh[:, k:k + 1]), [dst], [pm, gm, sh])

    def head_rstd(ss, tmp, rs, scale, eps):
        c.op('dve', lambda: V.tensor_scalar(out=tmp[:, 0:8], in0=ss[:], scalar1=scale, scalar2=eps, op0=ALU.mult, op1=ALU.add), [tmp], [ss])
        c.op('act', lambda: A.activation(out=tmp[:, 8:16], in_=tmp[:, 0:8], func=AF.Sqrt), [tmp], [tmp])
        c.op('dve', lambda: V.reciprocal(out=rs[:], in_=tmp[:, 8:16]), [rs], [tmp])

    dbg_dump("d_gmod1", gmod1, gmod1[:], [128, 8]); dbg_dump("d_sh1", sh1, sh1[:], [128, 8])
    dbg_dump("d_gmod2", gmod2, gmod2[:], [128, 8]); dbg_dump("d_sh2", sh2, sh2[:], [128, 8])

    stK = ExitStack()
    kiT = c.sb(stK, [128, S], BF16)
    with ExitStack() as st1:
        s1 = lambda shape, dt=F32: c.sb(st1, shape, dt)
        Wrw = s1([128, 8, 672], BF16); Wkv = s1([128, 8, 256], BF16); Wki = s1([128, 8, 128], BF16)
        Wup = s1([128, 2, 1024], BF16)
        load_w(Wrw, lambda c0, c1: Wrw[:, :, c0:c1], w_rw, 8, 672)
        load_w(Wkv, lambda c0, c1: Wkv[:, :, c0:c1], w_kv, 8, 256)
        load_w(Wki, lambda c0, c1: Wki[:, :, c0:c1], w_ki, 8, 128)
        for k in range(2):
            st = stg[stg_i[0] % 2]; stg_i[0] += 1
            c.dma('sp', st[:, 0:1024], w_kvup[k * 128:(k + 1) * 128, :], [st], [])
            c.op('dve', lambda: V.tensor_scalar(out=Wup[:, k, :], in0=st[:, 0:1024], scalar1=kvg_t[:, k:k + 1], scalar2=None, op0=ALU.mult),
                 [Wup], [st, kvg_t])
        w2b = s1([64, 128], BF16); a2b = s1([64, 128], BF16); g2b0 = s1([128, 128], BF16); g2b1 = s1([32, 128], BF16)
        for (bb, src, np_) in ((w2b, w2, 64), (a2b, a2, 64), (g2b0, g2[0:128, :], 128), (g2b1, g2[128:160, :], 32)):
            st = stg[stg_i[0] % 2]; stg_i[0] += 1
            c.dma('sp', st[0:np_, 0:128], src, [st], [])
            c.op('dve', lambda: V.tensor_copy(out=bb[:], in_=st[0:np_, 0:128]), [bb], [st])

        xnT = s1([128, 8, 512], BF16)
        Sst = [[s1([64, 64], BF16) for _ in range(2)] for _ in range(2)]
        for h in range(2):
            for j_ in range(2):
                c.op('dve', lambda: V.memset(Sst[h][j_][:], 0.0), [Sst[h][j_]], [])
        pend = [None]
        qspec = [("r0", 0, 64, 0), ("r1", 64, 64, 1), ("k0", 128, 64, 2), ("k1", 192, 64, 3), ("v0", 256, 64, 4), ("v1", 320, 64, 5),
                 ("wl", 384, 64, 6), ("al", 448, 64, 7), ("gl0", 512, 128, 8), ("gl1", 640, 32, 9)]
        SC = [s1([128, 512]) for _ in range(4)]
        praw = [s1([128, 513]) for _ in range(2)]
        lastc = s1([128, 10])
        c.op('dve', lambda: V.memset(lastc[:], 0.0), [lastc], [])
        mixd = {}
        for nm in ("r0", "r1", "k0", "k1", "v0", "v1"):
            mixd[nm] = s1([64, 512])
        mixd["wl"] = SC[0]; mixd["al"] = SC[1]; mixd["gl0"] = SC[2]; mixd["gl1"] = SC[3]
        twb = s1([64, 512], BF16); alb = s1([64, 512], BF16); sg0 = s1([128, 512], BF16); sg1 = s1([32, 512], BF16)
        hd = []
        for h in range(2):
            dct = {}
            for nm in ("dT", "b", "nkk", "kp"):
                dct[nm] = s1([64, 512])
            for nm in ("gT", "bv"):
                dct[nm] = s1([64, 512], BF16)
            dct["rT"] = s1([64, 512], BF16); dct["o"] = s1([64, 512], BF16)
            dct["tokE"] = [s1([64, 4, 64], BF16) for _ in range(2)]
            dct["tokO"] = [s1([64, 4, 64], BF16) for _ in range(2)]
            dct["BX"] = s1([64, 32, 64], BF16); dct["VX"] = s1([64, 32, 64], BF16)
            dct["tokF"] = [s1([64, 4, 64], BF16) for _ in range(2)]
            dct["A"] = [s1([64, 32, 64], BF16) for _ in range(2)]
            hd.append(dct)
        kvn = s1([128, 256]); kvs = s1([128, 4]); kvnT = s1([128, 2, 128], BF16)
        kss = s1([128, 8]); krs = s1([128, 8]); ktmp = s1([128, 16])
        KTt = [s1([128, 4, 128], BF16) for _ in range(2)]; V1t = [s1([128, 8, 65], BF16) for _ in range(2)]
        for v1 in V1t:
            c.op('dve', lambda: V.memset(v1[:], 1.0), [v1], [])

        maskE = s1([64, 1]); maskO = s1([64, 1])
        c.op('dve', lambda: V.tensor_reduce(out=maskE[:], in_=ident[0:64, 0:32], axis=AX.X, op=ALU.add), [maskE], [ident])
        c.op('dve', lambda: V.tensor_reduce(out=maskO[:], in_=ident[0:64, 32:64], axis=AX.X, op=ALU.add), [maskO], [ident])
        import os as _os
        acnt = [0]
        for g in range(int(_os.environ.get('START_G', '0')), NG):
            xg = xnT
            for tt in range(4):
                tile = 4 * g + tt
                norm_T(xbs[0 if _os.environ.get('XB0') else tile // 32][(tile % 32) * 128:(tile % 32 + 1) * 128, :], gmod1, sh1, xg, lambda k: xg[:, k, tt * 128:(tt + 1) * 128])
            if g == 0:
                dbg_dump("d_xnT", xg, xg[:], [128, 8, 512], BF16)
            if stop == 10 and g == int(_os.environ.get('STOP_G', '0')):
                c.barrier()
                return nc, c
            pm = PB[2]
            for k in range(0 if _os.environ.get('SKIP_KI') else 8):
                c.op('pe', lambda: PE.matmul(pm[:, :], lhsT=Wki[:, k, :], rhs=xg[:, k, :], start=(k == 0), stop=(k == 7)), [pm], [Wki, xg])
            gk = 0 if _os.environ.get('KI0') else g
            c.op('act', lambda: A.copy(out=kiT[:, gk * 512:(gk + 1) * 512], in_=pm[:, :]), [kiT], [pm])
            if stop == 11 and g == int(_os.environ.get('STOP_G', '0')):
                c.barrier()
                return nc, c
            for tt in range(0 if _os.environ.get('SKIP_KV') else 4):
                tile = 4 * g + tt
                pm = PB[3]
                for k in range(8):
                    c.op('pe', lambda: PE.matmul(pm[:, 0:256], lhsT=xg[:, k, tt * 128:(tt + 1) * 128], rhs=Wkv[:, k, :], start=(k == 0), stop=(k == 7)),
                         [pm], [Wkv, xg])
                c.op('act', lambda: A.activation(out=kvn[:], in_=pm[:, 0:256], func=AF.Square, accum_out=kvs[:, 0:1]), [kvn, kvs], [pm])
                c.op('dve', lambda: V.tensor_scalar(out=kvs[:, 1:2], in0=kvs[:, 0:1], scalar1=1.0 / 256, scalar2=NORM_EPS, op0=ALU.mult, op1=ALU.add), [kvs], [kvs])
                c.op('act', lambda: A.activation(out=kvs[:, 2:3], in_=kvs[:, 1:2], func=AF.Sqrt), [kvs], [kvs])
                c.op('dve', lambda: V.reciprocal(out=kvs[:, 3:4], in_=kvs[:, 2:3]), [kvs], [kvs])
                c.op('act', lambda: A.activation(out=kvn[:], in_=pm[:, 0:256], func=AF.Copy, scale=kvs[:, 3:4]), [kvn], [pm, kvs])
                pt = PB[4]
                for k in range(2):
                    c.op('pe', lambda: PE.transpose(out=pt[:, k * 128:(k + 1) * 128], in_=kvn[:, k * 128:(k + 1) * 128], identity=ident[:]), [pt], [kvn, ident])
                c.op('dve', lambda: V.tensor_copy(out=kvnT[:].rearrange("p k t -> p (k t)"), in_=pt[:, 0:256]), [kvnT], [pt])
                pk = PB[5]; pv = PB[6]
                for k in range(2):
                    c.op('pe', lambda: PE.matmul(pk[:, :], lhsT=kvnT[:, k, :], rhs=Wup[:, k, 0:512], start=(k == 0), stop=(k == 1)), [pk], [kvnT, Wup])
                for k in range(2):
                    c.op('pe', lambda: PE.matmul(pv[:, :], lhsT=kvnT[:, k, :], rhs=Wup[:, k, 512:1024], start=(k == 0), stop=(k == 1)), [pv], [kvnT, Wup])
                v1 = V1t[tile % 2]
                c.op('act', lambda: A.copy(out=v1[:, :, 0:64], in_=pv[:, :].rearrange("p (h e) -> p h e", h=8)), [v1], [pv])
                c.dma('sp', V1d.ap()[tile], v1[:], [V1dT], [v1])
                ksq = SC[0]; Kn = SC[1]
                c.op('act', lambda: A.activation(out=ksq[:], in_=pk[:, :], func=AF.Square), [ksq], [pk])
                c.op('dve', lambda: V.tensor_reduce(out=kss[:], in_=ksq[:].rearrange("p (h e) -> p h e", h=8), axis=AX.X, op=ALU.add), [kss], [ksq])
                head_rstd(kss, ktmp, krs, 1.0 / 64, NORM_EPS)
                c.op('dve', lambda: V.tensor_tensor(out=Kn[:].rearrange("p (h e) -> p h e", h=8), in0=pk[:, :].rearrange("p (h e) -> p h e", h=8),
                                                    in1=krs[:, :].unsqueeze(2).broadcast_to([128, 8, 64]), op=ALU.mult), [Kn], [pk, krs])
                ptk = PB[7]
                for pr in range(4):
                    c.op('pe', lambda: PE.transpose(out=ptk[:, pr * 128:(pr + 1) * 128], in_=Kn[:, pr * 128:(pr + 1) * 128], identity=ident[:]), [ptk], [Kn, ident])
                kt = KTt[tile % 2]
                c.op('dve', lambda: V.tensor_scalar(out=kt[:].rearrange("p a t -> p (a t)"), in0=ptk[:, :], scalar1=kg_t[:, 0:1], scalar2=None, op0=ALU.mult),
                     [kt], [ptk, kg_t])
                c.dma('sp', KTd.ap()[g, :, :, tt * 128:(tt + 1) * 128], kt[:], [KTdT], [kt])

            if stop == 12:
                c.barrier()
                return nc, c
            if _os.environ.get('SKIP_RW'):
                continue
            for qi, (nm, c0, M, mc) in enumerate(qspec):
                pm = PB[2 + (qi % 2)]
                pr_ = praw[qi % 2]; mx = mixd[nm]
                c.op('dve', lambda: V.tensor_copy(out=pr_[0:M, 0:1], in_=lastc[0:M, qi:qi + 1]), [pr_], [lastc])
                for k in range(8):
                    c.op('pe', lambda: PE.matmul(pm[0:M, :], lhsT=Wrw[:, k, c0:c0 + M], rhs=xg[:, k, :], start=(k == 0), stop=(k == 7)), [pm], [Wrw, xg])
                c.op('act', lambda: A.copy(out=pr_[0:M, 1:513], in_=pm[0:M, :]), [pr_], [pm])
                c.op('dve', lambda: V.tensor_copy(out=lastc[0:M, qi:qi + 1], in_=pr_[0:M, 512:513]), [lastc], [pr_])
                c.op('dve', lambda: V.tensor_tensor(out=mx[0:M, :], in0=pr_[0:M, 0:512], in1=pr_[0:M, 1:513], op=ALU.subtract), [mx], [pr_])
                c.op('dve', lambda: V.scalar_tensor_tensor(out=mx[0:M, :], in0=mx[0:M, :], scalar=mu_t[0:M, mc:mc + 1], in1=pr_[0:M, 1:513],
                                                           op0=ALU.mult, op1=ALU.add), [mx], [mx, pr_, mu_t])
            if stop == 13:
                c.barrier()
                return nc, c
            c.op('act', lambda: A.activation(out=twb[:], in_=mixd["wl"][0:64, :], func=AF.Tanh), [twb], [mixd["wl"]])
            c.op('dve', lambda: V.tensor_copy(out=alb[:], in_=mixd["al"][0:64, :]), [alb], [mixd["al"]])
            c.op('act', lambda: A.activation(out=sg0[:], in_=mixd["gl0"][:, :], func=AF.Sigmoid), [sg0], [mixd["gl0"]])
            c.op('act', lambda: A.activation(out=sg1[:], in_=mixd["gl1"][0:32, :], func=AF.Sigmoid), [sg1], [mixd["gl1"]])
            for h in range(2):
                dct = hd[h]
                rv = lambda j: rwv_t[:, h, j:j + 1]
                mr, mk, mv = mixd["r%d" % h], mixd["k%d" % h], mixd["v%d" % h]
                s0, s1_, s2_, s3_ = [SC[j] for j in range(4)]
                pm = PB[2]
                c.op('pe', lambda: PE.matmul(pm[0:64, :], lhsT=w2b[:, h * 64:(h + 1) * 64], rhs=twb[:], start=True, stop=True), [pm], [w2b, twb])
                c.op('act', lambda: A.activation(out=s0[0:64, :], in_=pm[0:64, :], func=AF.Sigmoid, bias=rv(0), scale=1.0), [s0], [pm, rwv_t])
                c.op('act', lambda: A.activation(out=dct["dT"][:], in_=s0[0:64, :], func=AF.Exp, scale=-0.6065306597126334), [dct["dT"]], [s0])
                pm = PB[3]
                c.op('pe', lambda: PE.matmul(pm[0:64, :], lhsT=a2b[:, h * 64:(h + 1) * 64], rhs=alb[:], start=True, stop=True), [pm], [a2b, alb])
                c.op('act', lambda: A.activation(out=s1_[0:64, :], in_=pm[0:64, :], func=AF.Sigmoid, bias=rv(1), scale=1.0), [s1_], [pm, rwv_t])
                pm = PB[2]
                c.op('pe', lambda: PE.matmul(pm[0:64, :], lhsT=g2b0[:, h * 64:(h + 1) * 64], rhs=sg0[:], start=True, stop=False), [pm], [g2b0, sg0])
                c.op('pe', lambda: PE.matmul(pm[0:64, :], lhsT=g2b1[:, h * 64:(h + 1) * 64], rhs=sg1[:], start=False, stop=True), [pm], [g2b1, sg1])
                c.op('act', lambda: A.copy(out=dct["gT"][:], in_=pm[0:64, :]), [dct["gT"]], [pm])
                c.op('dve', lambda: V.tensor_scalar(out=s2_[0:64, :], in0=mk[:], scalar1=rv(2), scalar2=None, op0=ALU.mult), [s2_], [mk, rwv_t])
                c.op('act', lambda: A.activation(out=s3_[0:64, :], in_=s2_[0:64, :], func=AF.Square), [s3_], [s2_])
                pm = PB[3]
                c.op('pe', lambda: PE.matmul(pm[0:64, :], lhsT=ones64[:], rhs=s3_[0:64, :], start=True, stop=True), [pm], [ones64, s3_])
                c.op('act', lambda: A.activation(out=s3_[0:64, :], in_=pm[0:64, :], func=AF.Sqrt, scale=64.0), [s3_], [pm])
                c.op('dve', lambda: V.tensor_scalar(out=s3_[0:64, :], in0=s3_[0:64, :], scalar1=1e-12, scalar2=None, op0=ALU.max), [s3_], [s3_])
                c.op('dve', lambda: V.reciprocal(out=s0[0:64, :], in_=s3_[0:64, :]), [s0], [s3_])
                c.op('dve', lambda: V.tensor_tensor(out=s2_[0:64, :], in0=s2_[0:64, :], in1=s0[0:64, :], op=ALU.mult), [s2_], [s2_, s0])
                c.op('dve', lambda: V.tensor_tensor(out=dct["b"][:], in0=s2_[0:64, :], in1=s1_[0:64, :], op=ALU.mult), [dct["b"]], [s2_, s1_])
                c.op('dve', lambda: V.tensor_scalar(out=dct["nkk"][:], in0=s2_[0:64, :], scalar1=-1.0, scalar2=None, op0=ALU.mult), [dct["nkk"]], [s2_])
                c.op('dve', lambda: V.tensor_scalar(out=s0[0:64, :], in0=s1_[0:64, :], scalar1=1.0, scalar2=rv(3), op0=ALU.subtract, op1=ALU.mult),
                     [s0], [s1_, rwv_t])
                c.op('dve', lambda: V.scalar_tensor_tensor(out=dct["kp"][:], in0=s0[0:64, :], scalar=1.0, in1=mk[:], op0=ALU.add, op1=ALU.mult),
                     [dct["kp"]], [s0, mk])
                c.op('dve', lambda: V.scalar_tensor_tensor(out=s3_[0:64, :], in0=mr[:], scalar=rv(4), in1=dct["kp"][:], op0=ALU.mult, op1=ALU.mult),
                     [s3_], [mr, dct["kp"], rwv_t])
                pm = PB[2]
                c.op('pe', lambda: PE.matmul(pm[0:64, :], lhsT=ones64[:], rhs=s3_[0:64, :], start=True, stop=True), [pm], [ones64, s3_])
                c.op('dve', lambda: V.scalar_tensor_tensor(out=dct["bv"][:], in0=pm[0:64, :], scalar=64.0, in1=mv[:], op0=ALU.mult, op1=ALU.mult),
                     [dct["bv"]], [pm, mv])
                c.op('act', lambda: A.copy(out=dct["rT"][:], in_=mr[:]), [dct["rT"]], [mr])
                if g == 0 and dbg:
                    for nm in ("dT", "gT", "b", "nkk", "kp", "bv"):
                        dbg_dump("d_%s%d" % (nm, h), dct[nm], dct[nm][:], [64, 512], F32 if nm in ("dT", "b", "nkk", "kp") else BF16)
            if stop == 14:
                c.barrier()
                return nc, c
            PY = [PB[6], PB[7]]
            PS = [PB[4], PB[5]]
            for ht in range(0 if _os.environ.get('SKIP_CHAIN') else 8):
                for h in range(2):
                    dct = hd[h]
                    tokE = dct["tokE"][ht % 2]; tokO = dct["tokO"][ht % 2]
                    pm = PB[2 + h]
                    srcs = [dct["nkk"], dct["b"], dct["kp"], mixd["v%d" % h]]
                    for si, sT in enumerate(srcs):
                        c.op('pe', lambda: PE.transpose(out=pm[0:64, si * 64:(si + 1) * 64], in_=sT[:, ht * 64:(ht + 1) * 64], identity=ident[0:64, 0:64]),
                             [pm], [sT, ident])
                    c.op('act', lambda: A.activation(out=tokE[:].rearrange("p a j -> p (a j)"), in_=pm[0:64, 0:256], func=AF.Copy, scale=maskE[:, 0:1]),
                         [tokE], [pm, maskE])
                    c.op('act', lambda: A.activation(out=tokO[:].rearrange("p a j -> p (a j)"), in_=pm[0:64, 0:256], func=AF.Copy, scale=maskO[:, 0:1]),
                         [tokO], [pm, maskO])
                    tokF = dct["tokF"][ht % 2]
                    c.op('act', lambda: A.copy(out=tokF[:].rearrange("p a j -> p (a j)"), in_=pm[0:64, 0:256]), [tokF], [pm])
                    c.op('pool', lambda: G.tensor_tensor(out=dct["BX"][:], in0=idrep[0:64, :].unsqueeze(2).broadcast_to([64, 32, 64]),
                                                         in1=tokF[:, 1, :].unsqueeze(1).broadcast_to([64, 32, 64]), op=ALU.mult), [dct["BX"]], [idrep, tokF])
                    c.op('pool', lambda: G.tensor_tensor(out=dct["VX"][:], in0=idrep[0:64, :].unsqueeze(2).broadcast_to([64, 32, 64]),
                                                         in1=tokF[:, 3, :].unsqueeze(1).broadcast_to([64, 32, 64]), op=ALU.mult), [dct["VX"]], [idrep, tokF])
                if stop == 15:
                    c.barrier()
                    return nc, c
                for mb in range(2):
                    Ablk = []
                    for h in range(2):
                        dct = hd[h]
                        tk = (dct["tokE"] if mb == 0 else dct["tokO"])[ht % 2]
                        bx = dct["BX"]
                        Ab = dct["A"][acnt[0] % 2]
                        Ablk.append(Ab)
                        for qq in range(4):
                            pm = PB[2 + (qq % 2)]
                            c.op('pe', lambda: PE.matmul(pm[0:64, :], lhsT=tk[:, 0, :], rhs=bx[:, 8 * qq:8 * qq + 8, :].rearrange("p a j -> p (a j)"),
                                                         start=True, stop=True), [pm], [tk, bx])
                            c.op('act', lambda: A.copy(out=Ab[:, 8 * qq:8 * qq + 8, :].rearrange("p a j -> p (a j)"), in_=pm[0:64, :]), [Ab], [pm])
                    acnt[0] += 1
                    if stop == 16:
                        c.barrier()
                        return nc, c
                    for tl in range(32):
                        tcol = ht * 64 + mb * 32 + tl
                        for h in range(2):
                            dct = hd[h]
                            tk = (dct["tokE"] if mb == 0 else dct["tokO"])[ht % 2]
                            vx = dct["VX"]
                            ps = PS[h]
                            src = Sst[h][tcol % 2]; dstS = Sst[h][(tcol + 1) % 2]
                            c.op('pe', lambda: PE.matmul(ps[0:64, 0:64], lhsT=Ablk[h][:, tl, :], rhs=src[:], start=True, stop=False), [ps], [Ablk[h], src])
                            c.op('pe', lambda: PE.matmul(ps[0:64, 0:64], lhsT=tk[:, 2, :], rhs=vx[:, tl, :], start=False, stop=True), [ps], [tk, vx])
                            c.op('dve', lambda: V.scalar_tensor_tensor(out=dstS[:], in0=src[:], scalar=dct["dT"][:, tcol:tcol + 1], in1=ps[0:64, 0:64],
                                                                       op0=ALU.mult, op1=ALU.add), [dstS], [src, ps, dct["dT"]])
                        if pend[0] is not None:
                            pc = pend[0]
                            for h in range(2):
                                dct = hd[h]
                                sy = Sst[h][(pc + 1) % 2]
                                c.op('pe', lambda: PE.matmul(PY[h][0:64, pc:pc + 1], lhsT=sy[:], rhs=dct["rT"][:, pc:pc + 1], start=True, stop=True),
                                     [PY[h]], [sy, dct["rT"]])
                        pend[0] = tcol
                        if stop == 18:
                            c.barrier()
                            return nc, c
            if pend[0] is not None:
                pc = pend[0]
                for h in range(2):
                    dct = hd[h]
                    sy = Sst[h][(pc + 1) % 2]
                    c.op('pe', lambda: PE.matmul(PY[h][0:64, pc:pc + 1], lhsT=sy[:], rhs=dct["rT"][:, pc:pc + 1], start=True, stop=True),
                         [PY[h]], [sy, dct["rT"]])
                pend[0] = None
            if stop == 17:
                c.barrier()
                return nc, c
            for h in range(2):
                dct = hd[h]
                rv = lambda j: rwv_t[:, h, j:j + 1]
                Y, yc, ysq, tmp = [SC[j] for j in range(4)]
                c.op('act', lambda: A.copy(out=Y[0:64, :], in_=PY[h][0:64, :]), [Y], [PY[h]])
                if g == 0:
                    dbg_dump("d_Y%d" % h, Y, Y[0:64, :], [64, 512])
                pm = PB[2]
                c.op('pe', lambda: PE.matmul(pm[0:64, :], lhsT=ones64[:], rhs=Y[0:64, :], start=True, stop=True), [pm], [ones64, Y])
                c.op('dve', lambda: V.tensor_tensor(out=yc[0:64, :], in0=Y[0:64, :], in1=pm[0:64, :], op=ALU.subtract), [yc], [Y, pm])
                c.op('act', lambda: A.activation(out=ysq[0:64, :], in_=yc[0:64, :], func=AF.Square), [ysq], [yc])
                pm = PB[3]
                c.op('pe', lambda: PE.matmul(pm[0:64, :], lhsT=ones64[:], rhs=ysq[0:64, :], start=True, stop=True), [pm], [ones64, ysq])
                c.op('dve', lambda: V.tensor_scalar(out=tmp[0:64, :], in0=pm[0:64, :], scalar1=GN_EPS, scalar2=None, op0=ALU.add), [tmp], [pm])
                c.op('act', lambda: A.activation(out=ysq[0:64, :], in_=tmp[0:64, :], func=AF.Sqrt), [ysq], [tmp])
                c.op('dve', lambda: V.reciprocal(out=tmp[0:64, :], in_=ysq[0:64, :]), [tmp], [ysq])
                c.op('dve', lambda: V.tensor_tensor(out=yc[0:64, :], in0=yc[0:64, :], in1=tmp[0:64, :], op=ALU.mult), [yc], [yc, tmp])
                c.op('dve', lambda: V.tensor_scalar(out=yc[0:64, :], in0=yc[0:64, :], scalar1=rv(5), scalar2=rv(6), op0=ALU.mult, op1=ALU.add),
                     [yc], [yc, rwv_t])
                c.op('dve', lambda: V.tensor_tensor(out=yc[0:64, :], in0=yc[0:64, :], in1=dct["bv"][:], op=ALU.add), [yc], [yc, dct["bv"]])
                c.op('dve', lambda: V.tensor_tensor(out=dct["o"][:], in0=yc[0:64, :], in1=dct["gT"][:], op=ALU.mult), [dct["o"]], [yc, dct["gT"]])
                dst = RSrcs[g // CG].ap().rearrange("(g t c) x -> g c t x", g=CG, t=4, c=128)[g % CG, h * 64:(h + 1) * 64, :, :]
                c.dma('sp', dst, dct["o"][:].rearrange("p (t x) -> p t x", t=4), [RSrcT], [dct["o"]])
        c.barrier()
    if dbg:
        dk = nc.dram_tensor("d_ki", [128, S], BF16, kind="ExternalOutput")
        c.dma('sp', dk.ap(), kiT[:], [], [kiT])
        drs = nc.dram_tensor("d_rsrc", [NG * 4 * 128, 128], BF16, kind="ExternalOutput")
        for k_ in range(NCH):
            c.dma('sp', drs.ap()[k_ * CG * 512:(k_ + 1) * CG * 512, :], RSrcs[k_].ap(), [], [RSrcT])
        dkt = nc.dram_tensor("d_kt", [NG, 128, 4, 512], BF16, kind="ExternalOutput")
        c.dma('sp', dkt.ap(), KTd.ap(), [], [KTdT])
        dv1 = nc.dram_tensor("d_v1", [NT, 128, 8, 65], BF16, kind="ExternalOutput")
        c.dma('sp', dv1.ap(), V1d.ap(), [], [V1dT])

    if stop == 1:
        c.barrier()
        return nc, c
    c._deps('pool', [RDstT], [RSrcT])
    for k_ in range(NCH):
        inst = nc.gpsimd.collective_compute("AllGather", ALU.bypass, replica_groups=[[0, 1, 2, 3], [4, 5, 6, 7]],
                                            ins=[RSrcs[k_].ap()], outs=[RDsts[k_].ap()])
        inst.then_inc(c.sem['cc'], 1)
    c._done(('cc', NCH), [RDstT], [RSrcT])

    if stop == 2:
        c.barrier()
        return nc, c
    with ExitStack() as st2:
        s2 = lambda shape, dt=F32: c.sb(st2, shape, dt)
        Wq = s2([128, 8, 776], BF16)
        load_w(Wq, lambda c0, c1: Wq[:, :, c0:c1], w_q, 8, 776)
        sc = s2([128, S]); Mall = s2([128, S], BF16)
        pen = s2([128, 512])
        iota0 = s2([128, 512]); iota1 = s2([128, 512]); iotai = s2([128, 512], mybir.dt.int32)
        c.op('pool', lambda: G.iota(iotai[:], pattern=[[1, 512]], base=0, channel_multiplier=0), [iotai], [])
        c.op('dve', lambda: V.tensor_copy(out=iota0[:], in_=iotai[:]), [iota0], [iotai])
        c.op('dve', lambda: V.tensor_scalar(out=iota1[:], in0=iota0[:], scalar1=1.0, scalar2=None, op0=ALU.add), [iota1], [iota0])
        c.op('dve', lambda: V.tensor_scalar(out=pen[:], in0=iota0[:], scalar1=qrel_t[:, 0:1], scalar2=-1e30, op0=ALU.is_gt, op1=ALU.mult), [pen], [iota0, qrel_t])
        KPf = s2([3, 512]); KPl = s2([3, 512], BF16); lo_f = s2([1, 512])
        c.op('dve', lambda: V.memset(KPf[:], 1.0), [KPf], [])
        kpi = s2([1, 512], mybir.dt.int32); kpi2 = s2([1, 512], mybir.dt.int32)
        c.op('pool', lambda: G.iota(kpi[:], pattern=[[64, 8], [0, 64]], base=0, channel_multiplier=0), [kpi], [])
        c.op('pool', lambda: G.iota(kpi2[:], pattern=[[0, 8], [1, 64]], base=0, channel_multiplier=0), [kpi2], [])
        c.op('dve', lambda: V.tensor_copy(out=KPf[0:1, :], in_=kpi[:]), [KPf], [kpi, KPf])
        c.op('dve', lambda: V.tensor_copy(out=lo_f[:], in_=kpi2[:]), [lo_f], [kpi2])
        c.dma('sp', KPf[1:2, :], lo_f[:], [KPf], [lo_f])
        c.op('dve', lambda: V.tensor_copy(out=KPl[:], in_=KPf[:]), [KPl], [KPf])
        sl3 = s2([3, 8])
        for h in range(8):
            c.op('dve', lambda: V.memset(sl3[:, h:h + 1], 8.0 * SLOPES[h]), [sl3], [sl3])
        xq_t = s2([128, 8, 128], BF16)
        qsq = s2([128, 512]); qss = s2([128, 8]); qrs = s2([128, 8]); qtmp = s2([128, 16]); Qn = s2([128, 512])
        QT = s2([128, 4, 128], BF16); qiT = s2([128, 3, 128], BF16); widx = s2([128, 8])
        Rl = [s2([128, 512], BF16) for _ in range(2)]
        bs = s2([128, 8])
        sm = s2([128, 8]); smtmp = s2([128, 512])
        base3 = s2([128, 8]); QPb = s2([3, 128]); QP = s2([3, 8, 128], BF16)
        KTg = [s2([128, 4, 512], BF16) for _ in range(2)]; V1g = [s2([128, 4, 8, 65], BF16) for _ in range(2)]
        PT = [s2([128, 4, 128], BF16) for _ in range(2)]
        acc = s2([128, 8, 65]); rec = s2([128, 8]); oat = s2([128, 512])
        OATb = [s2([128, 4, 128], BF16) for _ in range(2)]
        pcnt = [0]
        for i in range(NO):
            L = 512 * (i + 1)
            norm_T(xo[i * 128:(i + 1) * 128, :], gmod1, sh1, xq_t, lambda k: xq_t[:, k, :])
            xq = lambda k: xq_t[:, k, :]
            pm = PB[2]
            for k in range(8):
                c.op('pe', lambda: PE.matmul(pm[:, :], lhsT=xq(k), rhs=Wq[:, k, 0:512], start=(k == 0), stop=(k == 7)), [pm], [xq_t, Wq])
            c.op('act', lambda: A.activation(out=qsq[:], in_=pm[:, :], func=AF.Square), [qsq], [pm])
            c.op('dve', lambda: V.tensor_reduce(out=qss[:], in_=qsq[:].rearrange("p (h e) -> p h e", h=8), axis=AX.X, op=ALU.add), [qss], [qsq])
            head_rstd(qss, qtmp, qrs, 1.0 / 64, NORM_EPS)
            c.op('dve', lambda: V.tensor_tensor(out=Qn[:].rearrange("p (h e) -> p h e", h=8), in0=pm[:, :].rearrange("p (h e) -> p h e", h=8),
                                                in1=qrs[:, :].unsqueeze(2).broadcast_to([128, 8, 64]), op=ALU.mult), [Qn], [pm, qrs])
            pt = PB[3]
            for pr in range(4):
                c.op('pe', lambda: PE.transpose(out=pt[:, pr * 128:(pr + 1) * 128], in_=Qn[:, pr * 128:(pr + 1) * 128], identity=ident[:]), [pt], [Qn, ident])
            c.op('dve', lambda: V.tensor_scalar(out=QT[:].rearrange("p a t -> p (a t)"), in0=pt[:, :], scalar1=qg_t[:, 0:1], scalar2=None, op0=ALU.mult),
                 [QT], [pt, qg_t])
            pm = PB[2]
            for a in range(3):
                M = 96 if a < 2 else 64
                for k in range(8):
                    c.op('pe', lambda: PE.matmul(pm[0:M, a * 128:(a + 1) * 128], lhsT=Wq[:, k, 512 + a * 96:512 + a * 96 + M], rhs=xq(k),
                                                 start=(k == 0), stop=(k == 7)), [pm], [xq_t, Wq])
                c.op('act', lambda: A.copy(out=qiT[0:M, a, :], in_=pm[0:M, a * 128:(a + 1) * 128]), [qiT], [pm])
            pm = PB[3]
            for k in range(8):
                c.op('pe', lambda: PE.matmul(pm[:, 0:8], lhsT=xq(k), rhs=Wq[:, k, 768:776], start=(k == 0), stop=(k == 7)), [pm], [xq_t, Wq])
            c.op('dve', lambda: V.tensor_copy(out=widx[:], in_=pm[:, 0:8]), [widx], [pm])
            for cg in range(i + 1):
                for h in range(8):
                    a, r = h // 3, h % 3
                    pm = PB[4 + (h % 2)]
                    c.op('pe', lambda: PE.matmul(pm[:, :], lhsT=qiT[32 * r:32 * r + 32, a, :], rhs=kiT[32 * r:32 * r + 32, cg * 512:(cg + 1) * 512],
                                                 start=True, stop=True), [pm], [qiT, kiT])
                    rl = Rl[h % 2]
                    c.op('act', lambda: A.activation(out=rl[:], in_=pm[:, :], func=AF.Relu), [rl], [pm])
                    if h == 0:
                        c.op('dve', lambda: V.tensor_scalar(out=sc[:, cg * 512:(cg + 1) * 512], in0=rl[:], scalar1=widx[:, 0:1], scalar2=None, op0=ALU.mult),
                             [sc], [rl, widx])
                    else:
                        c.op('dve', lambda: V.scalar_tensor_tensor(out=sc[:, cg * 512:(cg + 1) * 512], in0=rl[:], scalar=widx[:, h:h + 1],
                                                                   in1=sc[:, cg * 512:(cg + 1) * 512], op0=ALU.mult, op1=ALU.add), [sc], [rl, widx, sc])
            c.op('dve', lambda: V.tensor_reduce(out=bs[:, 0:1], in_=sc[:, 0:L], axis=AX.X, op=ALU.max, apply_absolute_value=True), [bs], [sc])
            c.op('dve', lambda: V.tensor_tensor(out=sc[:, L - 512:L], in0=sc[:, L - 512:L], in1=pen[:], op=ALU.add), [sc], [sc, pen])
            c.op('dve', lambda: V.tensor_scalar(out=bs[:, 2:3], in0=bs[:, 0:1], scalar1=-1.0, scalar2=-1.0, op0=ALU.mult, op1=ALU.add), [bs], [bs])
            c.op('dve', lambda: V.tensor_scalar(out=bs[:, 1:2], in0=bs[:, 0:1], scalar1=2.0, scalar2=2.0, op0=ALU.mult, op1=ALU.add), [bs], [bs])
            for it in range(NBIS):
                f = 2.0 ** (-(it + 1))
                c.op('dve', lambda: V.scalar_tensor_tensor(out=bs[:, 3:4], in0=bs[:, 1:2], scalar=f, in1=bs[:, 2:3], op0=ALU.mult, op1=ALU.add), [bs], [bs])
                c.op('dve', lambda: V.tensor_scalar(out=Mall[:, 0:L], in0=sc[:, 0:L], scalar1=bs[:, 3:4], scalar2=None, op0=ALU.is_ge, op1=ALU.add,
                                                    accum_out=bs[:, 4:5]), [Mall, bs], [sc, bs])
                c.op('dve', lambda: V.tensor_scalar(out=bs[:, 5:6], in0=bs[:, 4:5], scalar1=255.5, scalar2=f, op0=ALU.is_ge, op1=ALU.mult), [bs], [bs])
                c.op('dve', lambda: V.scalar_tensor_tensor(out=bs[:, 2:3], in0=bs[:, 5:6], scalar=bs[:, 1:2], in1=bs[:, 2:3], op0=ALU.mult, op1=ALU.add), [bs], [bs])
            c.op('dve', lambda: V.memset(bs[:, 7:8], 0.0), [bs], [bs])
            for cg in range(i + 1):
                c.op('dve', lambda: V.tensor_scalar(out=Mall[:, cg * 512:(cg + 1) * 512], in0=sc[:, cg * 512:(cg + 1) * 512], scalar1=bs[:, 2:3], scalar2=None,
                                                    op0=ALU.is_ge), [Mall], [sc, bs])
                c.op('dve', lambda: V.tensor_tensor(out=smtmp[:], in0=Mall[:, cg * 512:(cg + 1) * 512], in1=iota1[:], op=ALU.mult), [smtmp], [Mall, iota1])
                c.op('dve', lambda: V.tensor_reduce(out=sm[:, 0:1], in_=smtmp[:], axis=AX.X, op=ALU.max), [sm], [smtmp])
                c.op('dve', lambda: V.tensor_scalar(out=sm[:, 1:2], in0=sm[:, 0:1], scalar1=1.0, scalar2=512.0 * cg, op0=ALU.min, op1=ALU.mult), [sm], [sm])
                c.op('dve', lambda: V.tensor_tensor(out=sm[:, 2:3], in0=sm[:, 0:1], in1=sm[:, 1:2], op=ALU.add), [sm], [sm])
                c.op('dve', lambda: V.tensor_tensor(out=bs[:, 7:8], in0=bs[:, 7:8], in1=sm[:, 2:3], op=ALU.max), [bs], [bs, sm])
            if i == min(1, NO - 1):
                dbg_dump("d_bs", bs, bs[:], [128, 8]); dbg_dump("d_mall", Mall, Mall[:, 0:L], [128, L], BF16)
                dbg_dump("d_sc", sc, sc[:, 0:L], [128, L])
            c.op('dve', lambda: V.memset(base3[:], 1.0), [base3], [])
            c.op('dve', lambda: V.tensor_scalar(out=base3[:, 2:3], in0=bs[:, 7:8], scalar1=-1.0, scalar2=1.0, op0=ALU.mult, op1=ALU.add), [base3], [bs, base3])
            pm = PB[2]
            c.op('pe', lambda: PE.transpose(out=pm[0:8, 0:128], in_=base3[:, 0:8], identity=ident[:]), [pm], [base3, ident])
            c.op('dve', lambda: V.tensor_copy(out=QPb[:], in_=pm[0:3, 0:128]), [QPb], [pm])
            c.op('dve', lambda: V.tensor_tensor(out=QP[:], in0=QPb[:, :].unsqueeze(1).broadcast_to([3, 8, 128]),
                                                in1=sl3[:, :].unsqueeze(2).broadcast_to([3, 8, 128]), op=ALU.mult), [QP], [QPb, sl3])
            for cg in range(i + 1):
                ktg = KTg[pcnt[0] % 2]; v1g = V1g[pcnt[0] % 2]
                c.dma('sp', ktg[:], KTd.ap()[cg], [ktg], [KTdT])
                c.dma('sp', v1g[:], V1d.ap()[4 * cg:4 * cg + 4].rearrange("t p h e -> p t h e"), [v1g], [V1dT])
                for h in range(8):
                    pr_, hh = h // 2, h % 2
                    pq = PB[4 + (h % 2)]
                    for sb_ in range(4):
                        o_ = pq[:, sb_ * 128:(sb_ + 1) * 128]
                        c.op('pe', lambda: PE.matmul(o_, lhsT=ktg[hh * 64:(hh + 1) * 64, pr_, sb_ * 128:(sb_ + 1) * 128], rhs=QT[hh * 64:(hh + 1) * 64, pr_, :],
                                                     start=True, stop=False), [pq], [ktg, QT])
                        c.op('pe', lambda: PE.matmul(o_, lhsT=KPl[0:3, sb_ * 128:(sb_ + 1) * 128], rhs=QP[0:3, h, :],
                                                     start=False, stop=False), [pq], [KPl, QP])
                        c.op('pe', lambda: PE.matmul(o_, lhsT=Mall[:, cg * 512 + sb_ * 128:cg * 512 + (sb_ + 1) * 128], rhs=ibig[:],
                                                     start=False, stop=True), [pq], [Mall, ibig])
                    ptt = PT[h % 2]
                    bias_h = SLOPES[h] * 512.0 * cg - BIG / 8.0
                    c.op('act', lambda: A.activation(out=ptt[:].rearrange("p a t -> p (a t)"), in_=pq[:, :], func=AF.Exp, scale=0.125, bias=bias_h),
                         [ptt], [pq])
                    po = PB[6 + (h // 4)]
                    for sb_ in range(4):
                        c.op('pe', lambda: PE.matmul(po[:, (h % 4) * 65:(h % 4) * 65 + 65], lhsT=ptt[:, sb_, :], rhs=v1g[:, sb_, h, :],
                                                     start=(sb_ == 0), stop=(sb_ == 3)), [po], [ptt, v1g])
                    if h % 4 == 3:
                        av = acc[:, (h // 4) * 4:(h // 4) * 4 + 4, :].rearrange("p a e -> p (a e)")
                        if cg == 0:
                            c.op('dve', lambda: V.tensor_copy(out=av, in_=po[:, 0:260]), [acc], [po])
                        else:
                            c.op('dve', lambda: V.tensor_tensor(out=av, in0=av, in1=po[:, 0:260], op=ALU.add), [acc], [acc, po])
                pcnt[0] += 1
            c.op('dve', lambda: V.reciprocal(out=rec[:], in_=acc[:, :, 64]), [rec], [acc])
            c.op('dve', lambda: V.tensor_tensor(out=oat[:].rearrange("p (h e) -> p h e", h=8), in0=acc[:, :, 0:64],
                                                in1=rec[:, :].unsqueeze(2).broadcast_to([128, 8, 64]), op=ALU.mult), [oat], [acc, rec])
            if i == min(1, NO - 1):
                dbg_dump("d_oat", oat, oat[:], [128, 512])
            pt = PB[2]
            for k in range(4):
                c.op('pe', lambda: PE.transpose(out=pt[:, k * 128:(k + 1) * 128], in_=oat[:, k * 128:(k + 1) * 128], identity=ident[:]), [pt], [oat, ident])
            oatb = OATb[i % 2]
            c.op('act', lambda: A.copy(out=oatb[:].rearrange("p k t -> p (k t)"), in_=pt[:, :]), [oatb], [pt])
            c.dma('sp', OATd.ap()[i], oatb[:], [OATdT], [oatb])
        c.barrier()
    stK.close()
    if stop == 3:
        c.barrier()
        return nc, c
    with ExitStack() as st3:
        s3 = lambda shape, dt=F32: c.sb(st3, shape, dt)
        gt1_bc = s3([128, D]); gt2_bc = s3([128, D])
        with ExitStack() as stg_:
            silu_bc = c.sb(stg_, [128, 8, 128])
            c.op('dve', lambda: V.tensor_copy(out=silu_bc[:], in_=silu_c[:, :].unsqueeze(2).broadcast_to([128, 8, 128])), [silu_bc], [silu_c])
            ada_phase((2, 5), stg_, gts=(gt1_bc, gt2_bc), silu_bc=silu_bc)
            c.barrier()
        dbg_dump("d_gt1", gt1_bc, gt1_bc[:], [128, D]); dbg_dump("d_gt2", gt2_bc, gt2_bc[:], [128, D])
        xnTo = s3([128, 8, NO * 128], BF16)
        cw = s3([128, NO, 32])
        stM = ExitStack()
        mergedT = c.sb(stM, [128, 8, NO * 128], BF16)
        with ExitStack() as st3a:
            s3a = lambda shape, dt=F32: c.sb(st3a, shape, dt)
            for i in range(NO):
                norm_T(xo[i * 128:(i + 1) * 128, :], gmod1, sh1, xnTo, lambda k: xnTo[:, k, i * 128:(i + 1) * 128])
            OAT = s3a([128, 4, NO * 128], BF16)
            for i in range(NO):
                c.dma('sp', OAT[:, :, i * 128:(i + 1) * 128], OATd.ap()[i], [OAT], [OATdT])
            ORT = s3a([128, 4, NO * 128], BF16)
            GH = max(1, NG // 2)
            Gsb = s3a([128, GH, 4, 128], BF16)
            rds = [RDsts[k_].ap().rearrange("(p g t c) x -> p c g t x", p=4, g=CG, t=4, c=128) for k_ in range(NCH)]
            for p in range(4):
                for g0 in range(0, NG, GH):
                    for gq in range(g0, g0 + GH, 2):
                        g1 = min(gq + 2, g0 + GH)
                        c.dma('sp', Gsb[:, gq - g0:g1 - g0, :, :], rds[gq // CG][p, :, gq % CG:gq % CG + (g1 - gq), :, :], [Gsb], [RDstT])
                    dstv = ORT[:, p, g0 * 128:(g0 + GH) * 128].rearrange("c (g x) -> c g x", g=GH)
                    c.op('dve', lambda: V.tensor_scalar(out=dstv, in0=Gsb[:, :, 0, :], scalar1=sel_t[:, 0:1], scalar2=None, op0=ALU.mult), [ORT], [Gsb, sel_t])
                    for t in range(1, 4):
                        c.op('dve', lambda: V.scalar_tensor_tensor(out=dstv, in0=Gsb[:, :, t, :], scalar=sel_t[:, t:t + 1], in1=dstv, op0=ALU.mult, op1=ALU.add),
                             [ORT], [Gsb, sel_t, ORT])
            if stop == 30:
                c.barrier()
                return nc, c
            Wba = s3a([128, 4, D], BF16); Wbr = s3a([128, 4, D], BF16)
            load_w(Wba, lambda c0, c1: Wba[:, :, c0:c1], w_ba, 4, D)
            load_w(Wbr, lambda c0, c1: Wbr[:, :, c0:c1], w_br, 4, D)
            Wga = s3a([128, 8, 128], BF16); Wgr = s3a([128, 8, 128], BF16)
            sga = s3a([128, 512], BF16); sgr = s3a([128, 512], BF16); t1 = s3a([128, 512]); t2 = s3a([128, 512])
            NTG = (NO * 128 + 511) // 512
            for m in range(8):
                load_w(Wga, lambda c0, c1: Wga[:, :, c0:c1], w_ga[:, m * 128:(m + 1) * 128], 8, 128)
                load_w(Wgr, lambda c0, c1: Wgr[:, :, c0:c1], w_gr[:, m * 128:(m + 1) * 128], 8, 128)
                for tg in range(NTG):
                    t0 = tg * 512; t1e = min(NO * 128, t0 + 512); n = t1e - t0
                    pa, pr, pba, pbr = PB[2], PB[3], PB[4], PB[5]
                    for k in range(8):
                        c.op('pe', lambda: PE.matmul(pa[:, 0:n], lhsT=Wga[:, k, :], rhs=xnTo[:, k, t0:t1e], start=(k == 0), stop=(k == 7)), [pa], [Wga, xnTo])
                    for k in range(8):
                        c.op('pe', lambda: PE.matmul(pr[:, 0:n], lhsT=Wgr[:, k, :], rhs=xnTo[:, k, t0:t1e], start=(k == 0), stop=(k == 7)), [pr], [Wgr, xnTo])
                    for k in range(4):
                        c.op('pe', lambda: PE.matmul(pba[:, 0:n], lhsT=Wba[:, k, m * 128:(m + 1) * 128], rhs=OAT[:, k, t0:t1e], start=(k == 0), stop=(k == 3)),
                             [pba], [Wba, OAT])
                    for k in range(4):
                        c.op('pe', lambda: PE.matmul(pbr[:, 0:n], lhsT=Wbr[:, k, m * 128:(m + 1) * 128], rhs=ORT[:, k, t0:t1e], start=(k == 0), stop=(k == 3)),
                             [pbr], [Wbr, ORT])
                    c.op('act', lambda: A.activation(out=sga[:, 0:n], in_=pa[:, 0:n], func=AF.Sigmoid), [sga], [pa])
                    c.op('act', lambda: A.activation(out=sgr[:, 0:n], in_=pr[:, 0:n], func=AF.Sigmoid), [sgr], [pr])
                    c.op('dve', lambda: V.tensor_tensor(out=t1[:, 0:n], in0=sga[:, 0:n], in1=pba[:, 0:n], op=ALU.mult), [t1], [sga, pba])
                    c.op('dve', lambda: V.tensor_tensor(out=t2[:, 0:n], in0=sgr[:, 0:n], in1=pbr[:, 0:n], op=ALU.mult), [t2], [sgr, pbr])
                    c.op('pool', lambda: G.tensor_tensor(out=mergedT[:, m, t0:t1e], in0=t1[:, 0:n], in1=t2[:, 0:n], op=ALU.add), [mergedT], [t1, t2])
            c.barrier()
        if stop == 31:
            c.barrier()
            return nc, c
        dbg_dump("d_merged", mergedT, mergedT[:], [128, 8, NO * 128], BF16)
        with ExitStack() as st3b:
            s3b = lambda shape, dt=F32: c.sb(st3b, shape, dt)
            Wout = s3b([128, 8, D], BF16); Wrt = s3b([128, 8, 36], BF16)
            load_w(Wout, lambda c0, c1: Wout[:, :, c0:c1], w_out, 8, D)
            load_w(Wrt, lambda c0, c1: Wrt[:, :, c0:c1], w_rt, 8, 36)
            h1t = s3b([128, D]); xot = s3b([128, D]); tmpo = s3b([128, 512])
            lg = s3b([128, 36]); r8 = s3b([128, 16]); ohg = s3b([128, 4]); peng = s3b([128, 4]); el = s3b([128, 32]); el2 = s3b([128, 32])
            oh1 = s3b([128, 32]); oh2 = s3b([128, 32]); ge4 = s3b([128, 4])
            for i in range(NO):
                c.dma('sp', xot[:], xo[i * 128:(i + 1) * 128, :], [xot], [])
                for half in range(2):
                    po = PB[6 + half]
                    for k in range(8):
                        c.op('pe', lambda: PE.matmul(po[:, :], lhsT=mergedT[:, k, i * 128:(i + 1) * 128], rhs=Wout[:, k, half * 512:(half + 1) * 512],
                                                     start=(k == 0), stop=(k == 7)), [po], [mergedT, Wout])
                    c.op('dve', lambda: V.tensor_tensor(out=tmpo[:], in0=po[:, :], in1=gt1_bc[:, half * 512:(half + 1) * 512], op=ALU.mult), [tmpo], [po, gt1_bc])
                    c.op('dve', lambda: V.tensor_tensor(out=h1t[:, half * 512:(half + 1) * 512], in0=tmpo[:], in1=xot[:, half * 512:(half + 1) * 512], op=ALU.add),
                         [h1t], [tmpo, xot])
                c.dma('sp', out.ap()[i * 128:(i + 1) * 128, :], h1t[:], [outT], [h1t])
                norm_T(None, gmod2, sh2, xnTo, lambda k: xnTo[:, k, i * 128:(i + 1) * 128], keep_x=h1t)
                pm = PB[2]
                for k in range(8):
                    c.op('pe', lambda: PE.matmul(pm[:, 0:36], lhsT=xnTo[:, k, i * 128:(i + 1) * 128], rhs=Wrt[:, k, :], start=(k == 0), stop=(k == 7)), [pm], [xnTo, Wrt])
                c.op('dve', lambda: V.tensor_copy(out=lg[:], in_=pm[:, 0:36]), [lg], [pm])
                c.op('dve', lambda: V.tensor_reduce(out=r8[:, 0:1], in_=lg[:, 0:4], axis=AX.X, op=ALU.max), [r8], [lg])
                c.op('dve', lambda: V.tensor_scalar(out=r8[:, 1:2], in0=r8[:, 0:1], scalar1=-1.0, scalar2=None, op0=ALU.mult), [r8], [r8])
                c.op('act', lambda: A.activation(out=ge4[:], in_=lg[:, 0:4], func=AF.Exp, bias=r8[:, 1:2], scale=1.0, accum_out=r8[:, 2:3]), [ge4, r8], [lg, r8])
                c.op('dve', lambda: V.reciprocal(out=r8[:, 3:4], in_=r8[:, 2:3]), [r8], [r8])
                c.op('dve', lambda: V.tensor_scalar(out=ohg[:], in0=lg[:, 0:4], scalar1=r8[:, 0:1], scalar2=None, op0=ALU.is_equal), [ohg], [lg, r8])
                c.op('dve', lambda: V.tensor_scalar(out=peng[:], in0=ohg[:], scalar1=1.0, scalar2=1e30, op0=ALU.subtract, op1=ALU.mult), [peng], [ohg])
                c.op('dve', lambda: V.tensor_tensor(out=el[:], in0=lg[:, 4:36], in1=ebias_t[:], op=ALU.add), [el], [lg, ebias_t])
                c.op('dve', lambda: V.tensor_tensor(out=el[:].rearrange("p (g e) -> p g e", g=4), in0=el[:].rearrange("p (g e) -> p g e", g=4),
                                                    in1=peng[:, :].unsqueeze(2).broadcast_to([128, 4, 8]), op=ALU.add), [el], [el, peng])
                c.op('dve', lambda: V.tensor_reduce(out=r8[:, 4:5], in_=el[:], axis=AX.X, op=ALU.max), [r8], [el])
                c.op('dve', lambda: V.tensor_scalar(out=oh1[:], in0=el[:], scalar1=r8[:, 4:5], scalar2=None, op0=ALU.is_equal), [oh1], [el, r8])
                c.op('dve', lambda: V.scalar_tensor_tensor(out=el2[:], in0=oh1[:], scalar=-1e30, in1=el[:], op0=ALU.mult, op1=ALU.add), [el2], [oh1, el])
                c.op('dve', lambda: V.tensor_reduce(out=r8[:, 5:6], in_=el2[:], axis=AX.X, op=ALU.max), [r8], [el2])
                c.op('dve', lambda: V.tensor_scalar(out=oh2[:], in0=el2[:], scalar1=r8[:, 5:6], scalar2=None, op0=ALU.is_equal), [oh2], [el2, r8])
                c.op('dve', lambda: V.tensor_tensor(out=r8[:, 6:7], in0=r8[:, 4:5], in1=r8[:, 5:6], op=ALU.subtract), [r8], [r8])
                c.op('act', lambda: A.activation(out=r8[:, 7:8], in_=r8[:, 6:7], func=AF.Sigmoid), [r8], [r8])
                c.op('dve', lambda: V.tensor_scalar(out=r8[:, 8:9], in0=r8[:, 7:8], scalar1=-1.0, scalar2=1.0, op0=ALU.mult, op1=ALU.add), [r8], [r8])
                c.op('dve', lambda: V.tensor_tensor(out=r8[:, 9:10], in0=r8[:, 7:8], in1=r8[:, 3:4], op=ALU.mult), [r8], [r8])
                c.op('dve', lambda: V.tensor_tensor(out=r8[:, 10:11], in0=r8[:, 8:9], in1=r8[:, 3:4], op=ALU.mult), [r8], [r8])
                c.op('dve', lambda: V.tensor_scalar(out=cw[:, i, :], in0=oh1[:], scalar1=r8[:, 9:10], scalar2=None, op0=ALU.mult), [cw], [oh1, r8])
                c.op('dve', lambda: V.scalar_tensor_tensor(out=cw[:, i, :], in0=oh2[:], scalar=r8[:, 10:11], in1=cw[:, i, :], op0=ALU.mult, op1=ALU.add),
                     [cw], [oh2, r8, cw])
            c.barrier()
        if stop == 32:
            c.barrier()
            return nc, c
        dbg_dump("d_cw", cw, cw[:], [128, NO, 32])
        stM.close()
        with ExitStack() as st3c:
            s3c = lambda shape, dt=F32: c.sb(st3c, shape, dt)
            accm = s3c([128, NO, D], BF16)
            c.op('pool', lambda: G.memset(accm[:], 0.0), [accm], [])
            Wg = [s3c([128, 8, 512], BF16) for _ in range(2)]; Wu = [s3c([128, 8, 512], BF16) for _ in range(2)]
            Wd = [s3c([128, 4, D], BF16) for _ in range(2)]
            hdn = [s3c([128, 4, 512], BF16) for _ in range(2)]; sgb = [s3c([128, 512], BF16) for _ in range(2)]
            NTG = (NO * 128 + 511) // 512
            cnt = 0
            for e in range(NE):
                wg, wu, wd = Wg[e % 2], Wu[e % 2], Wd[e % 2]
                load_w(wg, lambda c0, c1: wg[:, :, c0:c1], ew_g[e], 8, 512)
                load_w(wu, lambda c0, c1: wu[:, :, c0:c1], ew_u[e], 8, 512)
                load_w(wd, lambda c0, c1: wd[:, :, c0:c1], ew_d[e], 4, D)
                for tg in range(NTG):
                    t0 = tg * 512; t1e = min(NO * 128, t0 + 512); n = t1e - t0
                    hb = hdn[tg % 2]
                    for f in range(4):
                        pg_, pu_ = PB[2 + (cnt % 2)], PB[4 + (cnt % 2)]
                        sg_ = sgb[cnt % 2]
                        cnt += 1
                        for k in range(8):
                            c.op('pe', lambda: PE.matmul(pg_[:, 0:n], lhsT=wg[:, k, f * 128:(f + 1) * 128], rhs=xnTo[:, k, t0:t1e], start=(k == 0), stop=(k == 7)),
                                 [pg_], [wg, xnTo])
                        for k in range(8):
                            c.op('pe', lambda: PE.matmul(pu_[:, 0:n], lhsT=wu[:, k, f * 128:(f + 1) * 128], rhs=xnTo[:, k, t0:t1e], start=(k == 0), stop=(k == 7)),
                                 [pu_], [wu, xnTo])
                        c.op('act', lambda: A.activation(out=sg_[:, 0:n], in_=pg_[:, 0:n], func=AF.Silu), [sg_], [pg_])
                        c.op('dve', lambda: V.tensor_tensor(out=hb[:, f, 0:n], in0=sg_[:, 0:n], in1=pu_[:, 0:n], op=ALU.mult), [hb], [sg_, pu_])
                    for tt in range(n // 128):
                        tile = tg * 4 + tt
                        for half in range(2):
                            pd = PB[6 + half]
                            for f in range(4):
                                c.op('pe', lambda: PE.matmul(pd[:, :], lhsT=hb[:, f, tt * 128:(tt + 1) * 128], rhs=wd[:, f, half * 512:(half + 1) * 512],
                                                             start=(f == 0), stop=(f == 3)), [pd], [hb, wd])
                            av = accm[:, tile, half * 512:(half + 1) * 512]
                            c.op('dve', lambda: V.scalar_tensor_tensor(out=av, in0=pd[:, :], scalar=cw[:, tile, e:e + 1], in1=av, op0=ALU.mult, op1=ALU.add),
                                 [accm], [pd, cw, accm])
            if stop == 33:
                c.barrier()
                return nc, c
            h1b = [s3c([128, D]) for _ in range(2)]; ob = [s3c([128, D]) for _ in range(2)]
            for i in range(NO):
                hb_, o_ = h1b[i % 2], ob[i % 2]
                c.dma('sp', hb_[:], out.ap()[i * 128:(i + 1) * 128, :], [hb_], [outT])
                c.op('dve', lambda: V.tensor_tensor(out=o_[:], in0=accm[:, i, :], in1=gt2_bc[:], op=ALU.mult), [o_], [accm, gt2_bc])
                c.op('pool', lambda: G.tensor_tensor(out=o_[:], in0=o_[:], in1=hb_[:], op=ALU.add), [o_], [o_, hb_])
                c.dma('sp', out.ap()[i * 128:(i + 1) * 128, :], o_[:], [outT], [o_])
            c.barrier()
    import os as _os
    for _ in range(int(_os.environ.get("PAD_PE", "0"))):
        c.op('pe', lambda: PE.matmul(PB[0][:, 0:1], lhsT=ident[:, 0:128], rhs=ident[:, 0:1], start=True, stop=True), [PB[0]], [ident])
    for _ in range(int(_os.environ.get("PAD_ACT", "0"))):
        c.op('act', lambda: A.copy(out=st4[:, 0:1], in_=st4[:, 1:2]), [st4], [])
    c.barrier()
    glob.close()
    return nc, c


_IN_W_OFF = dict(q_a=0, kv=512, q_i=768, k_i=1024, w_i=1056, r=1064, k=1576, v=2088, wl=2600, al=2664, gl=2728, ga=2888, gr=3912)


def _prep_inputs(inp, NG=16, NE=32):
    S = 512 * NG
    f = lambda a: np.ascontiguousarray(np.asarray(a, dtype=np.float32))
    x = f(inp["x"]); cvec = f(inp["c"])
    w_in = f(inp["w_in"])[0]
    O = _IN_W_OFF
    col = lambda a, n: w_in[:, a:a + n]
    r128 = lambda v: f(v.reshape(-1, 128).T)
    mu = f(inp["rwkv_mu"])[0]
    shared = {
        "ada_w": f(inp["ada_w"])[0], "ada_b": f(inp["ada_b"])[0].reshape(1, -1),
        "mixg": r128(f(inp["mix_norm_g"])[0]), "moeg": r128(f(inp["moe_norm_g"])[0]),
        "w_kv": f(col(O["kv"], 256)), "w_ki": f(np.tile(col(O["k_i"], 32), (1, 4))),
        "w_q": f(np.concatenate([col(O["q_a"], 512), col(O["q_i"], 256), col(O["w_i"], 8)], axis=1)),
        "kvg": r128(f(inp["kv_norm_g"])[0]), "w_kvup": f(inp["w_kv_up"])[0],
        "qg": f(np.tile(f(inp["q_norm_g"])[0], 2).reshape(128, 1)), "kg": f(np.tile(f(inp["k_norm_g"])[0], 2).reshape(128, 1)),
        "w_ga": f(col(O["ga"], 1024)), "w_gr": f(col(O["gr"], 1024)),
        "w_ba": f(inp["w_branch_attn"])[0], "w_br": f(inp["w_branch_rwkv"])[0], "w_out": f(inp["w_out"])[0],
        "w_rt": f(np.concatenate([f(inp["router_group_w"])[0], f(inp["router_expert_w"])[0]], axis=1)),
        "e_bias": f(inp["router_expert_bias"])[0].reshape(1, 32),
    }
    for e in range(NE):
        shared["ew_g%d" % e] = f(inp["expert_w_gate"][0, e]); shared["ew_u%d" % e] = f(inp["expert_w_up"][0, e])
        shared["ew_d%d" % e] = f(inp["expert_w_down"][0, e])
    maps = []
    for cid in range(8):
        b, q = cid // 4, cid % 4
        m = dict(shared)
        for i_ in range((4 * NG + 31) // 32):
            m["xb%d" % i_] = f(x[b, i_ * 4096:min(S, (i_ + 1) * 4096)])
        own = [q + 4 * i for i in range(NG)]
        m["xo"] = f(np.concatenate([x[b, t * 128:(t + 1) * 128] for t in own], axis=0))
        m["cb"] = r128(cvec[b])
        m["qrel"] = f((128 * q + np.arange(128)).reshape(128, 1))
        sel = np.zeros((128, 4), np.float32); sel[:, q] = 1.0
        m["sel"] = sel
        h0 = 2 * q
        rcols = [col(O["r"] + (h0 + h) * 64, 64) for h in range(2)]
        kcols = [col(O["k"] + (h0 + h) * 64, 64) for h in range(2)]
        vcols = [col(O["v"] + (h0 + h) * 64, 64) for h in range(2)]
        m["w_rw"] = f(np.concatenate(rcols + kcols + vcols + [col(O["wl"], 64), col(O["al"], 64), col(O["gl"], 160)], axis=1))
        mu_rw = np.zeros((128, 10), np.float32)
        for h in range(2):
            mu_rw[0:64, 0 + h] = mu[(h0 + h) * 64:(h0 + h + 1) * 64]
            mu_rw[0:64, 2 + h] = mu[512 + (h0 + h) * 64:512 + (h0 + h + 1) * 64]
            mu_rw[0:64, 4 + h] = mu[1024 + (h0 + h) * 64:1024 + (h0 + h + 1) * 64]
        mu_rw[0:64, 6] = mu[1536:1600]; mu_rw[0:64, 7] = mu[1600:1664]
        mu_rw[0:128, 8] = mu[1664:1792]; mu_rw[0:32, 9] = mu[1792:1824]
        m["mu_rw"] = mu_rw
        rwv = np.zeros((64, 2, 8), np.float32)
        for h in range(2):
            sl = slice((h0 + h) * 64, (h0 + h + 1) * 64)
            rwv[:, h, 0] = f(inp["rwkv_w0"])[0][sl]; rwv[:, h, 1] = f(inp["rwkv_a0"])[0][sl]
            rwv[:, h, 2] = f(inp["rwkv_k_k"])[0][sl]; rwv[:, h, 3] = f(inp["rwkv_k_a"])[0][sl]
            rwv[:, h, 4] = f(inp["rwkv_r_k"])[0][h0 + h]; rwv[:, h, 5] = f(inp["rwkv_ln_w"])[0][sl]
            rwv[:, h, 6] = f(inp["rwkv_ln_b"])[0][sl]
        m["rwv"] = rwv
        hs = slice(h0 * 64, h0 * 64 + 128)
        m["w2"] = f(f(inp["rwkv_w2"])[0][:, hs]); m["a2"] = f(f(inp["rwkv_a2"])[0][:, hs]); m["g2"] = f(f(inp["rwkv_g2"])[0][:, hs])
        maps.append(m)
    return maps


_CACHE = {}


def kernel(**inputs):
    NG = 16
    if NG not in _CACHE:
        _CACHE[NG] = build_program(NG)[0]
    nc = _CACHE[NG]
    maps = _prep_inputs(inputs, NG)
    res = run_bass_kernel_spmd(nc, maps, core_ids=list(range(8)))
    out = np.zeros((2, 512 * NG, D), np.float32)
    for cid in range(8):
        b, q = cid // 4, cid % 4
        o = np.asarray(res.results[cid]["out"], dtype=np.float32)
        for i in range(NG):
            t = q + 4 * i
            out[b, t * 128:(t + 1) * 128] = o[i * 128:(i + 1) * 128]
    return out
```

```python
from contextlib import ExitStack
import numpy as np
import concourse.bass as bass
import concourse.mybir as mybir
from concourse.bass_utils import run_bass_kernel_spmd

F32 = mybir.dt.float32
BF16 = mybir.dt.bfloat16
AF = mybir.ActivationFunctionType
ALU = mybir.AluOpType
AX = mybir.AxisListType

D = 1024
NBIS = 17
NORM_EPS = 1e-6
GN_EPS = 64e-5
SLOPES = [2.0 ** (-(h + 1)) for h in range(8)]
BIG = 262144.0


class T:
    def __init__(self, h, name):
        self.h = h
        self.name = name
        self.w = None
        self.r = {}

    def __getitem__(self, idx):
        return self.h[idx]


class Ctx:
    NDMA = 32

    def __init__(self, nc):
        self.nc = nc
        self.eng = {'pe': nc.tensor, 'act': nc.scalar, 'dve': nc.vector, 'pool': nc.gpsimd, 'sp': nc.sync}
        self.sem = {}
        self.cnt = {}
        self.known = {}
        for k in self.eng:
            self.sem[k] = nc.alloc_semaphore("s_" + k)
            self.cnt[k] = 0
            self.known[k] = {}
        self.dma_n = 0
        for i in range(self.NDMA):
            self.sem['dma%d' % i] = nc.alloc_semaphore("s_dma%d" % i)
        self.sem['cc'] = nc.alloc_semaphore("s_cc")
        self.uid = 0
        self.ninst = 0
        self.nw = {k: 0 for k in self.eng}
        self.snaps = {}
        self.snapq = []
        self.nd = {k: 0 for k in self.eng}

    def sb(self, stack, shape, dt=F32, name=None):
        self.uid += 1
        name = name or ("t%d" % self.uid)
        h = stack.enter_context(self.nc.sbuf_tensor(name, list(shape), dt))
        return T(h, name)

    def _learn(self, e, key, val):
        kn = self.known[e]
        if kn.get(key, 0) < val:
            kn[key] = val
        sn = self.snaps.get((key, val))
        if sn is not None:
            for k2, v2 in sn.items():
                if kn.get(k2, 0) < v2:
                    kn[k2] = v2

    def _wait(self, e, key, val):
        if key == e and e in ('pe', 'sp'):
            return
        kn = self.known[e]
        if kn.get(key, 0) >= val:
            return
        self.eng[e].wait_ge(self.sem[key], val)
        self._learn(e, key, val)
        self.ninst += 1
        self.nw[e] += 1

    def _snap(self, e, ev):
        self.snaps[ev] = dict(self.known[e])
        self.snapq.append(ev)
        if len(self.snapq) > 20000:
            old = self.snapq.pop(0)
            self.snaps.pop(old, None)

    def _deps(self, e, outs, ins):
        for t in ins:
            if t is not None and t.w is not None:
                self._wait(e, *t.w)
        for t in outs:
            if t.w is not None:
                self._wait(e, *t.w)
            for k, v in t.r.items():
                if k != e:
                    self._wait(e, k, v)

    def _done(self, ev, outs, ins):
        for t in ins:
            if t is not None:
                t.r[ev[0]] = max(ev[1], t.r.get(ev[0], 0))
        for t in outs:
            t.w = ev
            t.r = {}

    def op(self, e, fn, outs, ins):
        self._deps(e, outs, ins)
        inst = fn()
        self.cnt[e] += 1
        inst.then_inc(self.sem[e], 1)
        self.ninst += 1
        self._snap(e, (e, self.cnt[e]))
        self._done((e, self.cnt[e]), outs, ins)

    def dma(self, e, out_ap, in_ap, outs, ins, **kw):
        n = self.dma_n
        self.dma_n += 1
        key = 'dma%d' % (n % self.NDMA)
        rnd = n // self.NDMA
        if rnd > 0:
            self._wait(e, key, 16 * rnd)
        self._deps(e, outs, ins)
        inst = self.eng[e].dma_start(out=out_ap, in_=in_ap, **kw)
        self.nd[e] += 1
        inst.then_inc(self.sem[key], 16)
        self.ninst += 1
        ev = (key, 16 * (rnd + 1))
        self._snap(e, ev)
        self._done(ev, outs, ins)
        return ev

    def barrier(self):
        cur = {k: self.cnt[k] for k in ('pe', 'act', 'dve', 'pool')}
        for i in range(self.NDMA):
            total = (self.dma_n - i + self.NDMA - 1) // self.NDMA
            if total > 0:
                cur['dma%d' % i] = 16 * total
        for e in ('pe', 'act', 'dve', 'pool', 'sp'):
            for k, v in cur.items():
                if v > 0 and k != e:
                    self._wait(e, k, v)
            if e in cur and cur[e] > 0 and e not in ('pe',):
                self._wait(e, e, cur[e])


def build_program(NG=16, dbg=False, stop=99, NE=32):
    NT = 4 * NG; NO = NG; S = 512 * NG
    nc = bass.Bass("TRN2", target_bir_lowering=False)
    c = Ctx(nc)
    V, A, P, G, PE = nc.vector, nc.scalar, nc.gpsimd, nc.gpsimd, nc.tensor

    def din(name, shape, dt=F32):
        return nc.dram_tensor(name, list(shape), dt, kind="ExternalInput").ap()

    NXB = (NT + 31) // 32
    xbs = [din("xb%d" % i, [min(32, NT - 32 * i) * 128, D]) for i in range(NXB)]
    xo = din("xo", [NO * 128, D]); cb = din("cb", [128, 8])
    qrel = din("qrel", [128, 1]); sel = din("sel", [128, 4])
    ada_w = din("ada_w", [D, 6 * D]); ada_b = din("ada_b", [1, 6 * D])
    mixg = din("mixg", [128, 8]); moeg = din("moeg", [128, 8])
    w_rw = din("w_rw", [D, 672]); mu_rw = din("mu_rw", [128, 10])
    w_kv = din("w_kv", [D, 256]); w_ki = din("w_ki", [D, 128]); w_q = din("w_q", [D, 776])
    kvg = din("kvg", [128, 2]); w_kvup = din("w_kvup", [256, 1024])
    qg = din("qg", [128, 1]); kg = din("kg", [128, 1])
    rwv = din("rwv", [64, 2, 8])
    w2 = din("w2", [64, 128]); a2 = din("a2", [64, 128]); g2 = din("g2", [160, 128])
    w_ga = din("w_ga", [D, D]); w_gr = din("w_gr", [D, D])
    w_ba = din("w_ba", [512, D]); w_br = din("w_br", [512, D]); w_out = din("w_out", [D, D])
    w_rt = din("w_rt", [D, 36]); e_bias = din("e_bias", [1, 32])
    ew_g = [din("ew_g%d" % e, [D, 512]) for e in range(NE)]
    ew_u = [din("ew_u%d" % e, [D, 512]) for e in range(NE)]
    ew_d = [din("ew_d%d" % e, [512, D]) for e in range(NE)]
    out = nc.dram_tensor("out", [NO * 128, D], F32, kind="ExternalOutput")
    outT = T(out, "out")
    KTd = nc.dram_tensor("KTd", [NG, 128, 4, 512], BF16, kind="ExternalOutput"); KTdT = T(KTd, "KTd")
    V1d = nc.dram_tensor("V1d", [NT, 128, 8, 65], BF16, kind="ExternalOutput"); V1dT = T(V1d, "V1d")
    CG = min(NG, 4); NCH = NG // CG
    RSrcs = [nc.dram_tensor("RSrc%d" % k, [CG * 4 * 128, 128], BF16, kind="Internal") for k in range(NCH)]
    RSrcT = T(None, "RSrc")
    OATd = nc.dram_tensor("OATd", [NO, 128, 4, 128], BF16, kind="ExternalOutput"); OATdT = T(OATd, "OATd")
    RDsts = [nc.dram_tensor("RDst%d" % k, [4 * CG * 4 * 128, 128], BF16, kind="Internal") for k in range(NCH)]
    RDstT = T(None, "RDst")

    glob = ExitStack()
    PB = []
    for i in range(8):
        h = glob.enter_context(nc.psum_tensor("pb%d" % i, [128, 512], F32))
        PB.append(T(h, "pb%d" % i))

    sb = lambda shape, dt=F32, st=glob: c.sb(st, shape, dt)

    ident = sb([128, 128]); identb = sb([128, 128], BF16); ibig = sb([128, 128], BF16)
    ones64 = sb([64, 64]); onesr = sb([1, 128]); one11 = sb([1, 1])
    idrep = sb([128, 32], BF16)
    c.op('pool', lambda: G.memset(ident[:], 1.0), [ident], [])
    c.op('pool', lambda: G.affine_select(out=ident[:], in_=ident[:], pattern=[[-1, 128]], compare_op=ALU.is_equal,
                                         fill=0.0, base=0, channel_multiplier=1), [ident], [ident])
    c.op('dve', lambda: V.tensor_copy(out=identb[:], in_=ident[:]), [identb], [ident])
    c.op('dve', lambda: V.tensor_scalar(out=ibig[:], in0=ident[:], scalar1=BIG, scalar2=None, op0=ALU.mult), [ibig], [ident])
    c.op('dve', lambda: V.memset(ones64[:], 1.0 / 64.0), [ones64], [])
    c.op('dve', lambda: V.memset(onesr[:], 1.0), [onesr], [])
    c.op('dve', lambda: V.memset(one11[:], 1.0), [one11], [])
    idr32 = sb([128, 32])
    c.op('dve', lambda: V.tensor_tensor(out=idr32[:], in0=ident[:, 0:32], in1=ident[:, 32:64], op=ALU.add), [idr32], [ident])
    c.op('dve', lambda: V.tensor_tensor(out=idr32[:], in0=idr32[:], in1=ident[:, 64:96], op=ALU.add), [idr32], [idr32, ident])
    c.op('dve', lambda: V.tensor_tensor(out=idrep[:], in0=idr32[:], in1=ident[:, 96:128], op=ALU.add), [idrep], [idr32, ident])

    def load_small(ap, shape):
        t = sb(shape)
        c.dma('sp', t[:], ap, [t], [])
        return t
    cbt = load_small(cb, [128, 8]); qrel_t = load_small(qrel, [128, 1]); sel_t = load_small(sel, [128, 4])
    mixg_t = load_small(mixg, [128, 8]); moeg_t = load_small(moeg, [128, 8]); mu_t = load_small(mu_rw, [128, 10])
    kvg_t = load_small(kvg, [128, 2]); qg_t = load_small(qg, [128, 1]); kg_t = load_small(kg, [128, 1])
    rwv_t = load_small(rwv, [64, 2, 8])
    ebias_t = sb([128, 32])
    c.dma('sp', ebias_t[:], e_bias[0:1, :].broadcast_to([128, 32]), [ebias_t], [])

    stg = [sb([128, 1024]) for _ in range(2)]
    stg_i = [0]

    def load_w(dst, dst_ap_fn, src, nk, ncols, eng_cast=('pool', 'act')):
        cbk = max(1, min(ncols, 1024 // nk))
        for c0 in range(0, ncols, cbk):
            c1 = min(ncols, c0 + cbk)
            st = stg[stg_i[0] % 2]; stg_i[0] += 1
            view = st[:, 0:nk * (c1 - c0)].rearrange("p (k f) -> p k f", k=nk)
            c.dma('sp', view, src[:, c0:c1].rearrange("(k p) f -> p k f", p=128), [st], [])
            e = eng_cast[stg_i[0] % len(eng_cast)]
            if e == 'act':
                c.op('act', lambda: A.copy(out=dst_ap_fn(c0, c1), in_=view), [dst], [st])
            else:
                c.op('pool', lambda: G.tensor_copy(out=dst_ap_fn(c0, c1), in_=view), [dst], [st])

    silu_c = sb([128, 8])
    c.op('act', lambda: A.activation(out=silu_c[:], in_=cbt[:], func=AF.Silu), [silu_c], [cbt])
    gmod1 = sb([128, 8]); sh1 = sb([128, 8]); gmod2 = sb([128, 8]); sh2 = sb([128, 8])

    def ada_phase(js, stk, gts=None, silu_bc=None):
        blk = [c.sb(stk, [128, 8, 1024]) for _ in range(2)]
        adab_t = c.sb(stk, [1, 6 * D])
        c.dma('sp', adab_t[:], ada_b, [adab_t], [])
        for jj, j in enumerate(js):
            bt = blk[jj % 2]
            for kk in range(2):
                c.dma('sp', bt[:, 4 * kk:4 * kk + 4, :],
                      ada_w[512 * kk:512 * kk + 512, j * 1024:(j + 1) * 1024].rearrange("(k p) f -> p k f", p=128), [bt], [])
            if j in (0, 1, 3, 4):
                pm = PB[j % 2]
                for m in range(8):
                    for k in range(8):
                        c.op('pe', lambda: PE.matmul(pm[:, m:m + 1], lhsT=bt[:, k, m * 128:(m + 1) * 128], rhs=silu_c[:, k:k + 1],
                                                     start=(k == 0), stop=False), [pm], [bt, silu_c])
                    c.op('pe', lambda: PE.matmul(pm[:, m:m + 1], lhsT=adab_t[0:1, j * 1024 + m * 128:j * 1024 + (m + 1) * 128],
                                                 rhs=one11[0:1, 0:1], start=False, stop=True), [pm], [adab_t, one11])
                if j == 0:
                    c.op('dve', lambda: V.tensor_copy(out=sh1[:], in_=pm[:, 0:8]), [sh1], [pm])
                elif j == 3:
                    c.op('dve', lambda: V.tensor_copy(out=sh2[:], in_=pm[:, 0:8]), [sh2], [pm])
                else:
                    gm, gg = (gmod1, mixg_t) if j == 1 else (gmod2, moeg_t)
                    c.op('dve', lambda: V.scalar_tensor_tensor(out=gm[:], in0=pm[:, 0:8], scalar=1.0, in1=gg[:], op0=ALU.add, op1=ALU.mult),
                         [gm], [pm, gg])
            else:
                gt = gts[0] if j == 2 else gts[1]
                for half in range(2):
                    pm = PB[2 + half]
                    for k in range(8):
                        c.op('pe', lambda: PE.matmul(pm[:, :], lhsT=silu_bc[:, k, :], rhs=bt[:, k, half * 512:(half + 1) * 512],
                                                     start=(k == 0), stop=False), [pm], [bt, silu_bc])
                    c.op('pe', lambda: PE.matmul(pm[:, :], lhsT=onesr[0:1, :], rhs=adab_t[0:1, j * 1024 + half * 512:j * 1024 + (half + 1) * 512],
                                                 start=False, stop=True), [pm], [adab_t, onesr])
                    c.op('act', lambda: A.copy(out=gt[:, half * 512:(half + 1) * 512], in_=pm[:, :]), [gt], [pm])

    with ExitStack() as st0:
        ada_phase((0, 1, 3, 4), st0)
        c.barrier()
    if stop == 0:
        return nc, c

    xt_buf = [sb([128, D]) for _ in range(2)]
    xs_buf = sb([128, D]); st4 = sb([128, 4])
    xcnt = [0]
    dbg_outs = {}

    def dbg_dump(name, t, ap, shape, dt=F32):
        if not dbg:
            return
        d = nc.dram_tensor(name, list(shape), dt, kind="ExternalOutput")
        c.dma('sp', d.ap(), ap, [], [t])

    def norm_T(src_dram_ap, gm, sh, dst, dst_ap_fn, keep_x=None):
        xt = keep_x if keep_x is not None else xt_buf[xcnt[0] % 2]
        xcnt[0] += 1
        if src_dram_ap is not None:
            c.dma('sp', xt[:], src_dram_ap, [xt], [])
        c.op('act', lambda: A.activation(out=xs_buf[:], in_=xt[:], func=AF.Square, accum_out=st4[:, 0:1]), [xs_buf, st4], [xt])
        c.op('dve', lambda: V.tensor_scalar(out=st4[:, 1:2], in0=st4[:, 0:1], scalar1=1.0 / D, scalar2=NORM_EPS, op0=ALU.mult, op1=ALU.add), [st4], [st4])
        c.op('act', lambda: A.activation(out=st4[:, 2:3], in_=st4[:, 1:2], func=AF.Sqrt), [st4], [st4])
        c.op('dve', lambda: V.reciprocal(out=st4[:, 3:4], in_=st4[:, 2:3]), [st4], [st4])
        c.op('act', lambda: A.activation(out=xs_buf[:], in_=xt[:], func=AF.Copy, scale=st4[:, 3:4]), [xs_buf], [xt, st4])
        for half in range(2):
            pm = PB[half]
            for kq in range(4):
                k = half * 4 + kq
                c.op('pe', lambda: PE.transpose(out=pm[:, kq * 128:(kq + 1) * 128], in_=xs_buf[:, k * 128:(k + 1) * 128], identity=ident[:]),
                     [pm], [xs_buf, ident])
            for kq in range(4):
                k = half * 4 + kq
                if kq % 2 == 0:
                    c.op('dve', lambda: V.tensor_scalar(out=dst_ap_fn(k), in0=pm[:, kq * 128:(kq + 1) * 128], scalar1=gm[:, k:k + 1],
                                                        scalar2=sh[:, k:k + 1], op0=ALU.mult, op1=ALU.add), [dst], [pm, gm, sh])
                else:
                    c.op('act', lambda: A.activation(out=dst_ap_fn(k), in_=pm[:, kq * 128:(kq + 1) * 128], func=AF.Identity,
                                                     scale=gm[:, k:k + 1], bias=sh[:, k:k + 1]), [dst], [pm, gm, sh])

    def head_rstd(ss, tmp, rs, scale, eps):
        c.op('dve', lambda: V.tensor_scalar(out=tmp[:, 0:8], in0=ss[:], scalar1=scale, scalar2=eps, op0=ALU.mult, op1=ALU.add), [tmp], [ss])
        c.op('act', lambda: A.activation(out=tmp[:, 8:16], in_=tmp[:, 0:8], func=AF.Sqrt), [tmp], [tmp])
        c.op('dve', lambda: V.reciprocal(out=rs[:], in_=tmp[:, 8:16]), [rs], [tmp])

    dbg_dump("d_gmod1", gmod1, gmod1[:], [128, 8]); dbg_dump("d_sh1", sh1, sh1[:], [128, 8])
    dbg_dump("d_gmod2", gmod2, gmod2[:], [128, 8]); dbg_dump("d_sh2", sh2, sh2[:], [128, 8])

    stK = ExitStack()
    kiT = c.sb(stK, [128, S], BF16)
    with ExitStack() as st1:
        s1 = lambda shape, dt=F32: c.sb(st1, shape, dt)
        Wrw = s1([128, 8, 672], BF16); Wkv = s1([128, 8, 256], BF16); Wki = s1([128, 8, 128], BF16)
        Wup = s1([128, 2, 1024], BF16)
        load_w(Wrw, lambda c0, c1: Wrw[:, :, c0:c1], w_rw, 8, 672)
        load_w(Wkv, lambda c0, c1: Wkv[:, :, c0:c1], w_kv, 8, 256)
        load_w(Wki, lambda c0, c1: Wki[:, :, c0:c1], w_ki, 8, 128)
        for k in range(2):
            st = stg[stg_i[0] % 2]; stg_i[0] += 1
            c.dma('sp', st[:, 0:1024], w_kvup[k * 128:(k + 1) * 128, :], [st], [])
            c.op('dve', lambda: V.tensor_scalar(out=Wup[:, k, :], in0=st[:, 0:1024], scalar1=kvg_t[:, k:k + 1], scalar2=None, op0=ALU.mult),
                 [Wup], [st, kvg_t])
        w2b = s1([64, 128], BF16); a2b = s1([64, 128], BF16); g2b0 = s1([128, 128], BF16); g2b1 = s1([32, 128], BF16)
        for (bb, src, np_) in ((w2b, w2, 64), (a2b, a2, 64), (g2b0, g2[0:128, :], 128), (g2b1, g2[128:160, :], 32)):
            st = stg[stg_i[0] % 2]; stg_i[0] += 1
            c.dma('sp', st[0:np_, 0:128], src, [st], [])
            c.op('dve', lambda: V.tensor_copy(out=bb[:], in_=st[0:np_, 0:128]), [bb], [st])

        xnT = s1([128, 8, 512], BF16)
        Sst = [[s1([64, 64], BF16) for _ in range(2)] for _ in range(2)]
        for h in range(2):
            for j_ in range(2):
                c.op('dve', lambda: V.memset(Sst[h][j_][:], 0.0), [Sst[h][j_]], [])
        pend = [None]
        qspec = [("r0", 0, 64, 0), ("r1", 64, 64, 1), ("k0", 128, 64, 2), ("k1", 192, 64, 3), ("v0", 256, 64, 4), ("v1", 320, 64, 5),
                 ("wl", 384, 64, 6), ("al", 448, 64, 7), ("gl0", 512, 128, 8), ("gl1", 640, 32, 9)]
        SC = [s1([128, 512]) for _ in range(4)]
        praw = [s1([128, 513]) for _ in range(2)]
        lastc = s1([128, 10])
        c.op('dve', lambda: V.memset(lastc[:], 0.0), [lastc], [])
        mixd = {}
        for nm in ("r0", "r1", "k0", "k1", "v0", "v1"):
            mixd[nm] = s1([64, 512])
        mixd["wl"] = SC[0]; mixd["al"] = SC[1]; mixd["gl0"] = SC[2]; mixd["gl1"] = SC[3]
        twb = s1([64, 512], BF16); alb = s1([64, 512], BF16); sg0 = s1([128, 512], BF16); sg1 = s1([32, 512], BF16)
        hd = []
        for h in range(2):
            dct = {}
            for nm in ("dT", "b", "nkk", "kp"):
                dct[nm] = s1([64, 512])
            for nm in ("gT", "bv"):
                dct[nm] = s1([64, 512], BF16)
            dct["rT"] = s1([64, 512], BF16); dct["o"] = s1([64, 512], BF16)
            dct["tokE"] = [s1([64, 4, 64], BF16) for _ in range(2)]
            dct["tokO"] = [s1([64, 4, 64], BF16) for _ in range(2)]
            dct["BX"] = s1([64, 32, 64], BF16); dct["VX"] = s1([64, 32, 64], BF16)
            dct["tokF"] = [s1([64, 4, 64], BF16) for _ in range(2)]
            dct["A"] = [s1([64, 32, 64], BF16) for _ in range(2)]
            hd.append(dct)
        kvn = s1([128, 256]); kvs = s1([128, 4]); kvnT = s1([128, 2, 128], BF16)
        kss = s1([128, 8]); krs = s1([128, 8]); ktmp = s1([128, 16])
        KTt = [s1([128, 4, 128], BF16) for _ in range(2)]; V1t = [s1([128, 8, 65], BF16) for _ in range(2)]
        for v1 in V1t:
            c.op('dve', lambda: V.memset(v1[:], 1.0), [v1], [])

        maskE = s1([64, 1]); maskO = s1([64, 1])
        c.op('dve', lambda: V.tensor_reduce(out=maskE[:], in_=ident[0:64, 0:32], axis=AX.X, op=ALU.add), [maskE], [ident])
        c.op('dve', lambda: V.tensor_reduce(out=maskO[:], in_=ident[0:64, 32:64], axis=AX.X, op=ALU.add), [maskO], [ident])
        import os as _os
        acnt = [0]
        for g in range(int(_os.environ.get('START_G', '0')), NG):
            xg = xnT
            for tt in range(4):
                tile = 4 * g + tt
                norm_T(xbs[0 if _os.environ.get('XB0') else tile // 32][(tile % 32) * 128:(tile % 32 + 1) * 128, :], gmod1, sh1, xg, lambda k: xg[:, k, tt * 128:(tt + 1) * 128])
            if g == 0:
                dbg_dump("d_xnT", xg, xg[:], [128, 8, 512], BF16)
            if stop == 10 and g == int(_os.environ.get('STOP_G', '0')):
                c.barrier()
                return nc, c
            pm = PB[2]
            for k in range(0 if _os.environ.get('SKIP_KI') else 8):
                c.op('pe', lambda: PE.matmul(pm[:, :], lhsT=Wki[:, k, :], rhs=xg[:, k, :], start=(k == 0), stop=(k == 7)), [pm], [Wki, xg])
            gk = 0 if _os.environ.get('KI0') else g
            c.op('act', lambda: A.copy(out=kiT[:, gk * 512:(gk + 1) * 512], in_=pm[:, :]), [kiT], [pm])
            if stop == 11 and g == int(_os.environ.get('STOP_G', '0')):
                c.barrier()
                return nc, c
            for tt in range(0 if _os.environ.get('SKIP_KV') else 4):
                tile = 4 * g + tt
                pm = PB[3]
                for k in range(8):
                    c.op('pe', lambda: PE.matmul(pm[:, 0:256], lhsT=xg[:, k, tt * 128:(tt + 1) * 128], rhs=Wkv[:, k, :], start=(k == 0), stop=(k == 7)),
                         [pm], [Wkv, xg])
                c.op('act', lambda: A.activation(out=kvn[:], in_=pm[:, 0:256], func=AF.Square, accum_out=kvs[:, 0:1]), [kvn, kvs], [pm])
                c.op('dve', lambda: V.tensor_scalar(out=kvs[:, 1:2], in0=kvs[:, 0:1], scalar1=1.0 / 256, scalar2=NORM_EPS, op0=ALU.mult, op1=ALU.add), [kvs], [kvs])
                c.op('act', lambda: A.activation(out=kvs[:, 2:3], in_=kvs[:, 1:2], func=AF.Sqrt), [kvs], [kvs])
                c.op('dve', lambda: V.reciprocal(out=kvs[:, 3:4], in_=kvs[:, 2:3]), [kvs], [kvs])
                c.op('act', lambda: A.activation(out=kvn[:], in_=pm[:, 0:256], func=AF.Copy, scale=kvs[:, 3:4]), [kvn], [pm, kvs])
                pt = PB[4]
                for k in range(2):
                    c.op('pe', lambda: PE.transpose(out=pt[:, k * 128:(k + 1) * 128], in_=kvn[:, k * 128:(k + 1) * 128], identity=ident[:]), [pt], [kvn, ident])
                c.op('dve', lambda: V.tensor_copy(out=kvnT[:].rearrange("p k t -> p (k t)"), in_=pt[:, 0:256]), [kvnT], [pt])
                pk = PB[5]; pv = PB[6]
                for k in range(2):
                    c.op('pe', lambda: PE.matmul(pk[:, :], lhsT=kvnT[:, k, :], rhs=Wup[:, k, 0:512], start=(k == 0), stop=(k == 1)), [pk], [kvnT, Wup])
                for k in range(2):
                    c.op('pe', lambda: PE.matmul(pv[:, :], lhsT=kvnT[:, k, :], rhs=Wup[:, k, 512:1024], start=(k == 0), stop=(k == 1)), [pv], [kvnT, Wup])
                v1 = V1t[tile % 2]
                c.op('act', lambda: A.copy(out=v1[:, :, 0:64], in_=pv[:, :].rearrange("p (h e) -> p h e", h=8)), [v1], [pv])
                c.dma('sp', V1d.ap()[tile], v1[:], [V1dT], [v1])
                ksq = SC[0]; Kn = SC[1]
                c.op('act', lambda: A.activation(out=ksq[:], in_=pk[:, :], func=AF.Square), [ksq], [pk])
                c.op('dve', lambda: V.tensor_reduce(out=kss[:], in_=ksq[:].rearrange("p (h e) -> p h e", h=8), axis=AX.X, op=ALU.add), [kss], [ksq])
                head_rstd(kss, ktmp, krs, 1.0 / 64, NORM_EPS)
                c.op('dve', lambda: V.tensor_tensor(out=Kn[:].rearrange("p (h e) -> p h e", h=8), in0=pk[:, :].rearrange("p (h e) -> p h e", h=8),
                                                    in1=krs[:, :].unsqueeze(2).broadcast_to([128, 8, 64]), op=ALU.mult), [Kn], [pk, krs])
                ptk = PB[7]
                for pr in range(4):
                    c.op('pe', lambda: PE.transpose(out=ptk[:, pr * 128:(pr + 1) * 128], in_=Kn[:, pr * 128:(pr + 1) * 128], identity=ident[:]), [ptk], [Kn, ident])
                kt = KTt[tile % 2]
                c.op('dve', lambda: V.tensor_scalar(out=kt[:].rearrange("p a t -> p (a t)"), in0=ptk[:, :], scalar1=kg_t[:, 0:1], scalar2=None, op0=ALU.mult),
                     [kt], [ptk, kg_t])
                c.dma('sp', KTd.ap()[g, :, :, tt * 128:(tt + 1) * 128], kt[:], [KTdT], [kt])

            if stop == 12:
                c.barrier()
                return nc, c
            if _os.environ.get('SKIP_RW'):
                continue
            for qi, (nm, c0, M, mc) in enumerate(qspec):
                pm = PB[2 + (qi % 2)]
                pr_ = praw[qi % 2]; mx = mixd[nm]
                c.op('dve', lambda: V.tensor_copy(out=pr_[0:M, 0:1], in_=lastc[0:M, qi:qi + 1]), [pr_], [lastc])
                for k in range(8):
                    c.op('pe', lambda: PE.matmul(pm[0:M, :], lhsT=Wrw[:, k, c0:c0 + M], rhs=xg[:, k, :], start=(k == 0), stop=(k == 7)), [pm], [Wrw, xg])
                c.op('act', lambda: A.copy(out=pr_[0:M, 1:513], in_=pm[0:M, :]), [pr_], [pm])
                c.op('dve', lambda: V.tensor_copy(out=lastc[0:M, qi:qi + 1], in_=pr_[0:M, 512:513]), [lastc], [pr_])
                c.op('dve', lambda: V.tensor_tensor(out=mx[0:M, :], in0=pr_[0:M, 0:512], in1=pr_[0:M, 1:513], op=ALU.subtract), [mx], [pr_])
                c.op('dve', lambda: V.scalar_tensor_tensor(out=mx[0:M, :], in0=mx[0:M, :], scalar=mu_t[0:M, mc:mc + 1], in1=pr_[0:M, 1:513],
                                                           op0=ALU.mult, op1=ALU.add), [mx], [mx, pr_, mu_t])
            if stop == 13:
                c.barrier()
                return nc, c
            c.op('act', lambda: A.activation(out=twb[:], in_=mixd["wl"][0:64, :], func=AF.Tanh), [twb], [mixd["wl"]])
            c.op('dve', lambda: V.tensor_copy(out=alb[:], in_=mixd["al"][0:64, :]), [alb], [mixd["al"]])
            c.op('act', lambda: A.activation(out=sg0[:], in_=mixd["gl0"][:, :], func=AF.Sigmoid), [sg0], [mixd["gl0"]])
            c.op('act', lambda: A.activation(out=sg1[:], in_=mixd["gl1"][0:32, :], func=AF.Sigmoid), [sg1], [mixd["gl1"]])
            for h in range(2):
                dct = hd[h]
                rv = lambda j: rwv_t[:, h, j:j + 1]
                mr, mk, mv = mixd["r%d" % h], mixd["k%d" % h], mixd["v%d" % h]
                s0, s1_, s2_, s3_ = [SC[j] for j in range(4)]
                pm = PB[2]
                c.op('pe', lambda: PE.matmul(pm[0:64, :], lhsT=w2b[:, h * 64:(h + 1) * 64], rhs=twb[:], start=True, stop=True), [pm], [w2b, twb])
                c.op('act', lambda: A.activation(out=s0[0:64, :], in_=pm[0:64, :], func=AF.Sigmoid, bias=rv(0), scale=1.0), [s0], [pm, rwv_t])
                c.op('act', lambda: A.activation(out=dct["dT"][:], in_=s0[0:64, :], func=AF.Exp, scale=-0.6065306597126334), [dct["dT"]], [s0])
                pm = PB[3]
                c.op('pe', lambda: PE.matmul(pm[0:64, :], lhsT=a2b[:, h * 64:(h + 1) * 64], rhs=alb[:], start=True, stop=True), [pm], [a2b, alb])
                c.op('act', lambda: A.activation(out=s1_[0:64, :], in_=pm[0:64, :], func=AF.Sigmoid, bias=rv(1), scale=1.0), [s1_], [pm, rwv_t])
                pm = PB[2]
                c.op('pe', lambda: PE.matmul(pm[0:64, :], lhsT=g2b0[:, h * 64:(h + 1) * 64], rhs=sg0[:], start=True, stop=False), [pm], [g2b0, sg0])
                c.op('pe', lambda: PE.matmul(pm[0:64, :], lhsT=g2b1[:, h * 64:(h + 1) * 64], rhs=sg1[:], start=False, stop=True), [pm], [g2b1, sg1])
                c.op('act', lambda: A.copy(out=dct["gT"][:], in_=pm[0:64, :]), [dct["gT"]], [pm])
                c.op('dve', lambda: V.tensor_scalar(out=s2_[0:64, :], in0=mk[:], scalar1=rv(2), scalar2=None, op0=ALU.mult), [s2_], [mk, rwv_t])
                c.op('act', lambda: A.activation(out=s3_[0:64, :], in_=s2_[0:64, :], func=AF.Square), [s3_], [s2_])
                pm = PB[3]
                c.op('pe', lambda: PE.matmul(pm[0:64, :], lhsT=ones64[:], rhs=s3_[0:64, :], start=True, stop=True), [pm], [ones64, s3_])
                c.op('act', lambda: A.activation(out=s3_[0:64, :], in_=pm[0:64, :], func=AF.Sqrt, scale=64.0), [s3_], [pm])
                c.op('dve', lambda: V.tensor_scalar(out=s3_[0:64, :], in0=s3_[0:64, :], scalar1=1e-12, scalar2=None, op0=ALU.max), [s3_], [s3_])
                c.op('dve', lambda: V.reciprocal(out=s0[0:64, :], in_=s3_[0:64, :]), [s0], [s3_])
                c.op('dve', lambda: V.tensor_tensor(out=s2_[0:64, :], in0=s2_[0:64, :], in1=s0[0:64, :], op=ALU.mult), [s2_], [s2_, s0])
                c.op('dve', lambda: V.tensor_tensor(out=dct["b"][:], in0=s2_[0:64, :], in1=s1_[0:64, :], op=ALU.mult), [dct["b"]], [s2_, s1_])
                c.op('dve', lambda: V.tensor_scalar(out=dct["nkk"][:], in0=s2_[0:64, :], scalar1=-1.0, scalar2=None, op0=ALU.mult), [dct["nkk"]], [s2_])
                c.op('dve', lambda: V.tensor_scalar(out=s0[0:64, :], in0=s1_[0:64, :], scalar1=1.0, scalar2=rv(3), op0=ALU.subtract, op1=ALU.mult),
                     [s0], [s1_, rwv_t])
                c.op('dve', lambda: V.scalar_tensor_tensor(out=dct["kp"][:], in0=s0[0:64, :], scalar=1.0, in1=mk[:], op0=ALU.add, op1=ALU.mult),
                     [dct["kp"]], [s0, mk])
                c.op('dve', lambda: V.scalar_tensor_tensor(out=s3_[0:64, :], in0=mr[:], scalar=rv(4), in1=dct["kp"][:], op0=ALU.mult, op1=ALU.mult),
                     [s3_], [mr, dct["kp"], rwv_t])
                pm = PB[2]
                c.op('pe', lambda: PE.matmul(pm[0:64, :], lhsT=ones64[:], rhs=s3_[0:64, :], start=True, stop=True), [pm], [ones64, s3_])
                c.op('dve', lambda: V.scalar_tensor_tensor(out=dct["bv"][:], in0=pm[0:64, :], scalar=64.0, in1=mv[:], op0=ALU.mult, op1=ALU.mult),
                     [dct["bv"]], [pm, mv])
                c.op('act', lambda: A.copy(out=dct["rT"][:], in_=mr[:]), [dct["rT"]], [mr])
                if g == 0 and dbg:
                    for nm in ("dT", "gT", "b", "nkk", "kp", "bv"):
                        dbg_dump("d_%s%d" % (nm, h), dct[nm], dct[nm][:], [64, 512], F32 if nm in ("dT", "b", "nkk", "kp") else BF16)
            if stop == 14:
                c.barrier()
                return nc, c
            PY = [PB[6], PB[7]]
            PS = [PB[4], PB[5]]
            for ht in range(0 if _os.environ.get('SKIP_CHAIN') else 8):
                for h in range(2):
                    dct = hd[h]
                    tokE = dct["tokE"][ht % 2]; tokO = dct["tokO"][ht % 2]
                    pm = PB[2 + h]
                    srcs = [dct["nkk"], dct["b"], dct["kp"], mixd["v%d" % h]]
                    for si, sT in enumerate(srcs):
                        c.op('pe', lambda: PE.transpose(out=pm[0:64, si * 64:(si + 1) * 64], in_=sT[:, ht * 64:(ht + 1) * 64], identity=ident[0:64, 0:64]),
                             [pm], [sT, ident])
                    c.op('act', lambda: A.activation(out=tokE[:].rearrange("p a j -> p (a j)"), in_=pm[0:64, 0:256], func=AF.Copy, scale=maskE[:, 0:1]),
                         [tokE], [pm, maskE])
                    c.op('act', lambda: A.activation(out=tokO[:].rearrange("p a j -> p (a j)"), in_=pm[0:64, 0:256], func=AF.Copy, scale=maskO[:, 0:1]),
                         [tokO], [pm, maskO])
                    tokF = dct["tokF"][ht % 2]
                    c.op('act', lambda: A.copy(out=tokF[:].rearrange("p a j -> p (a j)"), in_=pm[0:64, 0:256]), [tokF], [pm])
                    c.op('pool', lambda: G.tensor_tensor(out=dct["BX"][:], in0=idrep[0:64, :].unsqueeze(2).broadcast_to([64, 32, 64]),
                                                         in1=tokF[:, 1, :].unsqueeze(1).broadcast_to([64, 32, 64]), op=ALU.mult), [dct["BX"]], [idrep, tokF])
                    c.op('pool', lambda: G.tensor_tensor(out=dct["VX"][:], in0=idrep[0:64, :].unsqueeze(2).broadcast_to([64, 32, 64]),
                                                         in1=tokF[:, 3, :].unsqueeze(1).broadcast_to([64, 32, 64]), op=ALU.mult), [dct["VX"]], [idrep, tokF])
                if stop == 15:
                    c.barrier()
                    return nc, c
                for mb in range(2):
                    Ablk = []
                    for h in range(2):
                        dct = hd[h]
                        tk = (dct["tokE"] if mb == 0 else dct["tokO"])[ht % 2]
                        bx = dct["BX"]
                        Ab = dct["A"][acnt[0] % 2]
                        Ablk.append(Ab)
                        for qq in range(4):
                            pm = PB[2 + (qq % 2)]
                            c.op('pe', lambda: PE.matmul(pm[0:64, :], lhsT=tk[:, 0, :], rhs=bx[:, 8 * qq:8 * qq + 8, :].rearrange("p a j -> p (a j)"),
                                                         start=True, stop=True), [pm], [tk, bx])
                            c.op('act', lambda: A.copy(out=Ab[:, 8 * qq:8 * qq + 8, :].rearrange("p a j -> p (a j)"), in_=pm[0:64, :]), [Ab], [pm])
                    acnt[0] += 1
                    if stop == 16:
                        c.barrier()
                        return nc, c
                    for tl in range(32):
                        tcol = ht * 64 + mb * 32 + tl
                        for h in range(2):
                            dct = hd[h]
                            tk = (dct["tokE"] if mb == 0 else dct["tokO"])[ht % 2]
                            vx = dct["VX"]
                            ps = PS[h]
                            src = Sst[h][tcol % 2]; dstS = Sst[h][(tcol + 1) % 2]
                            c.op('pe', lambda: PE.matmul(ps[0:64, 0:64], lhsT=Ablk[h][:, tl, :], rhs=src[:], start=True, stop=False), [ps], [Ablk[h], src])
                            c.op('pe', lambda: PE.matmul(ps[0:64, 0:64], lhsT=tk[:, 2, :], rhs=vx[:, tl, :], start=False, stop=True), [ps], [tk, vx])
                            c.op('dve', lambda: V.scalar_tensor_tensor(out=dstS[:], in0=src[:], scalar=dct["dT"][:, tcol:tcol + 1], in1=ps[0:64, 0:64],
                                                                       op0=ALU.mult, op1=ALU.add), [dstS], [src, ps, dct["dT"]])
                        if pend[0] is not None:
                            pc = pend[0]
                            for h in range(2):
                                dct = hd[h]
                                sy = Sst[h][(pc + 1) % 2]
                                c.op('pe', lambda: PE.matmul(PY[h][0:64, pc:pc + 1], lhsT=sy[:], rhs=dct["rT"][:, pc:pc + 1], start=True, stop=True),
                                     [PY[h]], [sy, dct["rT"]])
                        pend[0] = tcol
                        if stop == 18:
                            c.barrier()
                            return nc, c
            if pend[0] is not None:
                pc = pend[0]
                for h in range(2):
                    dct = hd[h]
                    sy = Sst[h][(pc + 1) % 2]
                    c.op('pe', lambda: PE.matmul(PY[h][0:64, pc:pc + 1], lhsT=sy[:], rhs=dct["rT"][:, pc:pc + 1], start=True, stop=True),
                         [PY[h]], [sy, dct["rT"]])
                pend[0] = None
            if stop == 17:
                c.barrier()
                return nc, c
            for h in range(2):
                dct = hd[h]
                rv = lambda j: rwv_t[:, h, j:j + 1]
                Y, yc, ysq, tmp = [SC[j] for j in range(4)]
                c.op('act', lambda: A.copy(out=Y[0:64, :], in_=PY[h][0:64, :]), [Y], [PY[h]])
                if g == 0:
                    dbg_dump("d_Y%d" % h, Y, Y[0:64, :], [64, 512])
                pm = PB[2]
                c.op('pe', lambda: PE.matmul(pm[0:64, :], lhsT=ones64[:], rhs=Y[0:64, :], start=True, stop=True), [pm], [ones64, Y])
                c.op('dve', lambda: V.tensor_tensor(out=yc[0:64, :], in0=Y[0:64, :], in1=pm[0:64, :], op=ALU.subtract), [yc], [Y, pm])
                c.op('act', lambda: A.activation(out=ysq[0:64, :], in_=yc[0:64, :], func=AF.Square), [ysq], [yc])
                pm = PB[3]
                c.op('pe', lambda: PE.matmul(pm[0:64, :], lhsT=ones64[:], rhs=ysq[0:64, :], start=True, stop=True), [pm], [ones64, ysq])
                c.op('dve', lambda: V.tensor_scalar(out=tmp[0:64, :], in0=pm[0:64, :], scalar1=GN_EPS, scalar2=None, op0=ALU.add), [tmp], [pm])
                c.op('act', lambda: A.activation(out=ysq[0:64, :], in_=tmp[0:64, :], func=AF.Sqrt), [ysq], [tmp])
                c.op('dve', lambda: V.reciprocal(out=tmp[0:64, :], in_=ysq[0:64, :]), [tmp], [ysq])
                c.op('dve', lambda: V.tensor_tensor(out=yc[0:64, :], in0=yc[0:64, :], in1=tmp[0:64, :], op=ALU.mult), [yc], [yc, tmp])
                c.op('dve', lambda: V.tensor_scalar(out=yc[0:64, :], in0=yc[0:64, :], scalar1=rv(5), scalar2=rv(6), op0=ALU.mult, op1=ALU.add),
                     [yc], [yc, rwv_t])
                c.op('dve', lambda: V.tensor_tensor(out=yc[0:64, :], in0=yc[0:64, :], in1=dct["bv"][:], op=ALU.add), [yc], [yc, dct["bv"]])
                c.op('dve', lambda: V.tensor_tensor(out=dct["o"][:], in0=yc[0:64, :], in1=dct["gT"][:], op=ALU.mult), [dct["o"]], [yc, dct["gT"]])
                dst = RSrcs[g // CG].ap().rearrange("(g t c) x -> g c t x", g=CG, t=4, c=128)[g % CG, h * 64:(h + 1) * 64, :, :]
                c.dma('sp', dst, dct["o"][:].rearrange("p (t x) -> p t x", t=4), [RSrcT], [dct["o"]])
        c.barrier()
    if dbg:
        dk = nc.dram_tensor("d_ki", [128, S], BF16, kind="ExternalOutput")
        c.dma('sp', dk.ap(), kiT[:], [], [kiT])
        drs = nc.dram_tensor("d_rsrc", [NG * 4 * 128, 128], BF16, kind="ExternalOutput")
        for k_ in range(NCH):
            c.dma('sp', drs.ap()[k_ * CG * 512:(k_ + 1) * CG * 512, :], RSrcs[k_].ap(), [], [RSrcT])
        dkt = nc.dram_tensor("d_kt", [NG, 128, 4, 512], BF16, kind="ExternalOutput")
        c.dma('sp', dkt.ap(), KTd.ap(), [], [KTdT])
        dv1 = nc.dram_tensor("d_v1", [NT, 128, 8, 65], BF16, kind="ExternalOutput")
        c.dma('sp', dv1.ap(), V1d.ap(), [], [V1dT])

    if stop == 1:
        c.barrier()
        return nc, c
    c._deps('pool', [RDstT], [RSrcT])
    for k_ in range(NCH):
        inst = nc.gpsimd.collective_compute("AllGather", ALU.bypass, replica_groups=[[0, 1, 2, 3], [4, 5, 6, 7]],
                                            ins=[RSrcs[k_].ap()], outs=[RDsts[k_].ap()])
        inst.then_inc(c.sem['cc'], 1)
    c._done(('cc', NCH), [RDstT], [RSrcT])

    if stop == 2:
        c.barrier()
        return nc, c
    with ExitStack() as st2:
        s2 = lambda shape, dt=F32: c.sb(st2, shape, dt)
        Wq = s2([128, 8, 776], BF16)
        load_w(Wq, lambda c0, c1: Wq[:, :, c0:c1], w_q, 8, 776)
        sc = s2([128, S]); Mall = s2([128, S], BF16)
        pen = s2([128, 512])
        iota0 = s2([128, 512]); iota1 = s2([128, 512]); iotai = s2([128, 512], mybir.dt.int32)
        c.op('pool', lambda: G.iota(iotai[:], pattern=[[1, 512]], base=0, channel_multiplier=0), [iotai], [])
        c.op('dve', lambda: V.tensor_copy(out=iota0[:], in_=iotai[:]), [iota0], [iotai])
        c.op('dve', lambda: V.tensor_scalar(out=iota1[:], in0=iota0[:], scalar1=1.0, scalar2=None, op0=ALU.add), [iota1], [iota0])
        c.op('dve', lambda: V.tensor_scalar(out=pen[:], in0=iota0[:], scalar1=qrel_t[:, 0:1], scalar2=-1e30, op0=ALU.is_gt, op1=ALU.mult), [pen], [iota0, qrel_t])
        KPf = s2([3, 512]); KPl = s2([3, 512], BF16); lo_f = s2([1, 512])
        c.op('dve', lambda: V.memset(KPf[:], 1.0), [KPf], [])
        kpi = s2([1, 512], mybir.dt.int32); kpi2 = s2([1, 512], mybir.dt.int32)
        c.op('pool', lambda: G.iota(kpi[:], pattern=[[64, 8], [0, 64]], base=0, channel_multiplier=0), [kpi], [])
        c.op('pool', lambda: G.iota(kpi2[:], pattern=[[0, 8], [1, 64]], base=0, channel_multiplier=0), [kpi2], [])
        c.op('dve', lambda: V.tensor_copy(out=KPf[0:1, :], in_=kpi[:]), [KPf], [kpi, KPf])
        c.op('dve', lambda: V.tensor_copy(out=lo_f[:], in_=kpi2[:]), [lo_f], [kpi2])
        c.dma('sp', KPf[1:2, :], lo_f[:], [KPf], [lo_f])
        c.op('dve', lambda: V.tensor_copy(out=KPl[:], in_=KPf[:]), [KPl], [KPf])
        sl3 = s2([3, 8])
        for h in range(8):
            c.op('dve', lambda: V.memset(sl3[:, h:h + 1], 8.0 * SLOPES[h]), [sl3], [sl3])
        xq_t = s2([128, 8, 128], BF16)
        qsq = s2([128, 512]); qss = s2([128, 8]); qrs = s2([128, 8]); qtmp = s2([128, 16]); Qn = s2([128, 512])
        QT = s2([128, 4, 128], BF16); qiT = s2([128, 3, 128], BF16); widx = s2([128, 8])
        Rl = [s2([128, 512], BF16) for _ in range(2)]
        bs = s2([128, 8])
        sm = s2([128, 8]); smtmp = s2([128, 512])
        base3 = s2([128, 8]); QPb = s2([3, 128]); QP = s2([3, 8, 128], BF16)
        KTg = [s2([128, 4, 512], BF16) for _ in range(2)]; V1g = [s2([128, 4, 8, 65], BF16) for _ in range(2)]
        PT = [s2([128, 4, 128], BF16) for _ in range(2)]
        acc = s2([128, 8, 65]); rec = s2([128, 8]); oat = s2([128, 512])
        OATb = [s2([128, 4, 128], BF16) for _ in range(2)]
        pcnt = [0]
        for i in range(NO):
            L = 512 * (i + 1)
            norm_T(xo[i * 128:(i + 1) * 128, :], gmod1, sh1, xq_t, lambda k: xq_t[:, k, :])
            xq = lambda k: xq_t[:, k, :]
            pm = PB[2]
            for k in range(8):
                c.op('pe', lambda: PE.matmul(pm[:, :], lhsT=xq(k), rhs=Wq[:, k, 0:512], start=(k == 0), stop=(k == 7)), [pm], [xq_t, Wq])
            c.op('act', lambda: A.activation(out=qsq[:], in_=pm[:, :], func=AF.Square), [qsq], [pm])
            c.op('dve', lambda: V.tensor_reduce(out=qss[:], in_=qsq[:].rearrange("p (h e) -> p h e", h=8), axis=AX.X, op=ALU.add), [qss], [qsq])
            head_rstd(qss, qtmp, qrs, 1.0 / 64, NORM_EPS)
            c.op('dve', lambda: V.tensor_tensor(out=Qn[:].rearrange("p (h e) -> p h e", h=8), in0=pm[:, :].rearrange("p (h e) -> p h e", h=8),
                                                in1=qrs[:, :].unsqueeze(2).broadcast_to([128, 8, 64]), op=ALU.mult), [Qn], [pm, qrs])
            pt = PB[3]
            for pr in range(4):
                c.op('pe', lambda: PE.transpose(out=pt[:, pr * 128:(pr + 1) * 128], in_=Qn[:, pr * 128:(pr + 1) * 128], identity=ident[:]), [pt], [Qn, ident])
            c.op('dve', lambda: V.tensor_scalar(out=QT[:].rearrange("p a t -> p (a t)"), in0=pt[:, :], scalar1=qg_t[:, 0:1], scalar2=None, op0=ALU.mult),
                 [QT], [pt, qg_t])
            pm = PB[2]
            for a in range(3):
                M = 96 if a < 2 else 64
                for k in range(8):
                    c.op('pe', lambda: PE.matmul(pm[0:M, a * 128:(a + 1) * 128], lhsT=Wq[:, k, 512 + a * 96:512 + a * 96 + M], rhs=xq(k),
                                                 start=(k == 0), stop=(k == 7)), [pm], [xq_t, Wq])
                c.op('act', lambda: A.copy(out=qiT[0:M, a, :], in_=pm[0:M, a * 128:(a + 1) * 128]), [qiT], [pm])
            pm = PB[3]
            for k in range(8):
                c.op('pe', lambda: PE.matmul(pm[:, 0:8], lhsT=xq(k), rhs=Wq[:, k, 768:776], start=(k == 0), stop=(k == 7)), [pm], [xq_t, Wq])
            c.op('dve', lambda: V.tensor_copy(out=widx[:], in_=pm[:, 0:8]), [widx], [pm])
            for cg in range(i + 1):
                for h in range(8):
                    a, r = h // 3, h % 3
                    pm = PB[4 + (h % 2)]
                    c.op('pe', lambda: PE.matmul(pm[:, :], lhsT=qiT[32 * r:32 * r + 32, a, :], rhs=kiT[32 * r:32 * r + 32, cg * 512:(cg + 1) * 512],
                                                 start=True, stop=True), [pm], [qiT, kiT])
                    rl = Rl[h % 2]
                    c.op('act', lambda: A.activation(out=rl[:], in_=pm[:, :], func=AF.Relu), [rl], [pm])
                    if h == 0:
                        c.op('dve', lambda: V.tensor_scalar(out=sc[:, cg * 512:(cg + 1) * 512], in0=rl[:], scalar1=widx[:, 0:1], scalar2=None, op0=ALU.mult),
                             [sc], [rl, widx])
                    else:
                        c.op('dve', lambda: V.scalar_tensor_tensor(out=sc[:, cg * 512:(cg + 1) * 512], in0=rl[:], scalar=widx[:, h:h + 1],
                                                                   in1=sc[:, cg * 512:(cg + 1) * 512], op0=ALU.mult, op1=ALU.add), [sc], [rl, widx, sc])
            c.op('dve', lambda: V.tensor_reduce(out=bs[:, 0:1], in_=sc[:, 0:L], axis=AX.X, op=ALU.max, apply_absolute_value=True), [bs], [sc])
            c.op('dve', lambda: V.tensor_tensor(out=sc[:, L - 512:L], in0=sc[:, L - 512:L], in1=pen[:], op=ALU.add), [sc], [sc, pen])
            c.op('dve', lambda: V.tensor_scalar(out=bs[:, 2:3], in0=bs[:, 0:1], scalar1=-1.0, scalar2=-1.0, op0=ALU.mult, op1=ALU.add), [bs], [bs])
            c.op('dve', lambda: V.tensor_scalar(out=bs[:, 1:2], in0=bs[:, 0:1], scalar1=2.0, scalar2=2.0, op0=ALU.mult, op1=ALU.add), [bs], [bs])
            for it in range(NBIS):
                f = 2.0 ** (-(it + 1))
                c.op('dve', lambda: V.scalar_tensor_tensor(out=bs[:, 3:4], in0=bs[:, 1:2], scalar=f, in1=bs[:, 2:3], op0=ALU.mult, op1=ALU.add), [bs], [bs])
                c.op('dve', lambda: V.tensor_scalar(out=Mall[:, 0:L], in0=sc[:, 0:L], scalar1=bs[:, 3:4], scalar2=None, op0=ALU.is_ge, op1=ALU.add,
                                                    accum_out=bs[:, 4:5]), [Mall, bs], [sc, bs])
                c.op('dve', lambda: V.tensor_scalar(out=bs[:, 5:6], in0=bs[:, 4:5], scalar1=255.5, scalar2=f, op0=ALU.is_ge, op1=ALU.mult), [bs], [bs])
                c.op('dve', lambda: V.scalar_tensor_tensor(out=bs[:, 2:3], in0=bs[:, 5:6], scalar=bs[:, 1:2], in1=bs[:, 2:3], op0=ALU.mult, op1=ALU.add), [bs], [bs])
            c.op('dve', lambda: V.memset(bs[:, 7:8], 0.0), [bs], [bs])
            for cg in range(i + 1):
                c.op('dve', lambda: V.tensor_scalar(out=Mall[:, cg * 512:(cg + 1) * 512], in0=sc[:, cg * 512:(cg + 1) * 512], scalar1=bs[:, 2:3], scalar2=None,
                                                    op0=ALU.is_ge), [Mall], [sc, bs])
                c.op('dve', lambda: V.tensor_tensor(out=smtmp[:], in0=Mall[:, cg * 512:(cg + 1) * 512], in1=iota1[:], op=ALU.mult), [smtmp], [Mall, iota1])
                c.op('dve', lambda: V.tensor_reduce(out=sm[:, 0:1], in_=smtmp[:], axis=AX.X, op=ALU.max), [sm], [smtmp])
                c.op('dve', lambda: V.tensor_scalar(out=sm[:, 1:2], in0=sm[:, 0:1], scalar1=1.0, scalar2=512.0 * cg, op0=ALU.min, op1=ALU.mult), [sm], [sm])
                c.op('dve', lambda: V.tensor_tensor(out=sm[:, 2:3], in0=sm[:, 0:1], in1=sm[:, 1:2], op=ALU.add), [sm], [sm])
                c.op('dve', lambda: V.tensor_tensor(out=bs[:, 7:8], in0=bs[:, 7:8], in1=sm[:, 2:3], op=ALU.max), [bs], [bs, sm])
            if i == min(1, NO - 1):
                dbg_dump("d_bs", bs, bs[:], [128, 8]); dbg_dump("d_mall", Mall, Mall[:, 0:L], [128, L], BF16)
                dbg_dump("d_sc", sc, sc[:, 0:L], [128, L])
            c.op('dve', lambda: V.memset(base3[:], 1.0), [base3], [])
            c.op('dve', lambda: V.tensor_scalar(out=base3[:, 2:3], in0=bs[:, 7:8], scalar1=-1.0, scalar2=1.0, op0=ALU.mult, op1=ALU.add), [base3], [bs, base3])
            pm = PB[2]
            c.op('pe', lambda: PE.transpose(out=pm[0:8, 0:128], in_=base3[:, 0:8], identity=ident[:]), [pm], [base3, ident])
            c.op('dve', lambda: V.tensor_copy(out=QPb[:], in_=pm[0:3, 0:128]), [QPb], [pm])
            c.op('dve', lambda: V.tensor_tensor(out=QP[:], in0=QPb[:, :].unsqueeze(1).broadcast_to([3, 8, 128]),
                                                in1=sl3[:, :].unsqueeze(2).broadcast_to([3, 8, 128]), op=ALU.mult), [QP], [QPb, sl3])
            for cg in range(i + 1):
                ktg = KTg[pcnt[0] % 2]; v1g = V1g[pcnt[0] % 2]
                c.dma('sp', ktg[:], KTd.ap()[cg], [ktg], [KTdT])
                c.dma('sp', v1g[:], V1d.ap()[4 * cg:4 * cg + 4].rearrange("t p h e -> p t h e"), [v1g], [V1dT])
                for h in range(8):
                    pr_, hh = h // 2, h % 2
                    pq = PB[4 + (h % 2)]
                    for sb_ in range(4):
                        o_ = pq[:, sb_ * 128:(sb_ + 1) * 128]
                        c.op('pe', lambda: PE.matmul(o_, lhsT=ktg[hh * 64:(hh + 1) * 64, pr_, sb_ * 128:(sb_ + 1) * 128], rhs=QT[hh * 64:(hh + 1) * 64, pr_, :],
                                                     start=True, stop=False), [pq], [ktg, QT])
                        c.op('pe', lambda: PE.matmul(o_, lhsT=KPl[0:3, sb_ * 128:(sb_ + 1) * 128], rhs=QP[0:3, h, :],
                                                     start=False, stop=False), [pq], [KPl, QP])
                        c.op('pe', lambda: PE.matmul(o_, lhsT=Mall[:, cg * 512 + sb_ * 128:cg * 512 + (sb_ + 1) * 128], rhs=ibig[:],
                                                     start=False, stop=True), [pq], [Mall, ibig])
                    ptt = PT[h % 2]
                    bias_h = SLOPES[h] * 512.0 * cg - BIG / 8.0
                    c.op('act', lambda: A.activation(out=ptt[:].rearrange("p a t -> p (a t)"), in_=pq[:, :], func=AF.Exp, scale=0.125, bias=bias_h),
                         [ptt], [pq])
                    po = PB[6 + (h // 4)]
                    for sb_ in range(4):
                        c.op('pe', lambda: PE.matmul(po[:, (h % 4) * 65:(h % 4) * 65 + 65], lhsT=ptt[:, sb_, :], rhs=v1g[:, sb_, h, :],
                                                     start=(sb_ == 0), stop=(sb_ == 3)), [po], [ptt, v1g])
                    if h % 4 == 3:
                        av = acc[:, (h // 4) * 4:(h // 4) * 4 + 4, :].rearrange("p a e -> p (a e)")
                        if cg == 0:
                            c.op('dve', lambda: V.tensor_copy(out=av, in_=po[:, 0:260]), [acc], [po])
                        else:
                            c.op('dve', lambda: V.tensor_tensor(out=av, in0=av, in1=po[:, 0:260], op=ALU.add), [acc], [acc, po])
                pcnt[0] += 1
            c.op('dve', lambda: V.reciprocal(out=rec[:], in_=acc[:, :, 64]), [rec], [acc])
            c.op('dve', lambda: V.tensor_tensor(out=oat[:].rearrange("p (h e) -> p h e", h=8), in0=acc[:, :, 0:64],
                                                in1=rec[:, :].unsqueeze(2).broadcast_to([128, 8, 64]), op=ALU.mult), [oat], [acc, rec])
            if i == min(1, NO - 1):
                dbg_dump("d_oat", oat, oat[:], [128, 512])
            pt = PB[2]
            for k in range(4):
                c.op('pe', lambda: PE.transpose(out=pt[:, k * 128:(k + 1) * 128], in_=oat[:, k * 128:(k + 1) * 128], identity=ident[:]), [pt], [oat, ident])
            oatb = OATb[i % 2]
            c.op('act', lambda: A.copy(out=oatb[:].rearrange("p k t -> p (k t)"), in_=pt[:, :]), [oatb], [pt])
            c.dma('sp', OATd.ap()[i], oatb[:], [OATdT], [oatb])
        c.barrier()
    stK.close()
    if stop == 3:
        c.barrier()
        return nc, c
    with ExitStack() as st3:
        s3 = lambda shape, dt=F32: c.sb(st3, shape, dt)
        gt1_bc = s3([128, D]); gt2_bc = s3([128, D])
        with ExitStack() as stg_:
            silu_bc = c.sb(stg_, [128, 8, 128])
            c.op('dve', lambda: V.tensor_copy(out=silu_bc[:], in_=silu_c[:, :].unsqueeze(2).broadcast_to([128, 8, 128])), [silu_bc], [silu_c])
            ada_phase((2, 5), stg_, gts=(gt1_bc, gt2_bc), silu_bc=silu_bc)
            c.barrier()
        dbg_dump("d_gt1", gt1_bc, gt1_bc[:], [128, D]); dbg_dump("d_gt2", gt2_bc, gt2_bc[:], [128, D])
        xnTo = s3([128, 8, NO * 128], BF16)
        cw = s3([128, NO, 32])
        stM = ExitStack()
        mergedT = c.sb(stM, [128, 8, NO * 128], BF16)
        with ExitStack() as st3a:
            s3a = lambda shape, dt=F32: c.sb(st3a, shape, dt)
            for i in range(NO):
                norm_T(xo[i * 128:(i + 1) * 128, :], gmod1, sh1, xnTo, lambda k: xnTo[:, k, i * 128:(i + 1) * 128])
            OAT = s3a([128, 4, NO * 128], BF16)
            for i in range(NO):
                c.dma('sp', OAT[:, :, i * 128:(i + 1) * 128], OATd.ap()[i], [OAT], [OATdT])
            ORT = s3a([128, 4, NO * 128], BF16)
            GH = max(1, NG // 2)
            Gsb = s3a([128, GH, 4, 128], BF16)
            rds = [RDsts[k_].ap().rearrange("(p g t c) x -> p c g t x", p=4, g=CG, t=4, c=128) for k_ in range(NCH)]
            for p in range(4):
                for g0 in range(0, NG, GH):
                    for gq in range(g0, g0 + GH, 2):
                        g1 = min(gq + 2, g0 + GH)
                        c.dma('sp', Gsb[:, gq - g0:g1 - g0, :, :], rds[gq // CG][p, :, gq % CG:gq % CG + (g1 - gq), :, :], [Gsb], [RDstT])
                    dstv = ORT[:, p, g0 * 128:(g0 + GH) * 128].rearrange("c (g x) -> c g x", g=GH)
                    c.op('dve', lambda: V.tensor_scalar(out=dstv, in0=Gsb[:, :, 0, :], scalar1=sel_t[:, 0:1], scalar2=None, op0=ALU.mult), [ORT], [Gsb, sel_t])
                    for t in range(1, 4):
                        c.op('dve', lambda: V.scalar_tensor_tensor(out=dstv, in0=Gsb[:, :, t, :], scalar=sel_t[:, t:t + 1], in1=dstv, op0=ALU.mult, op1=ALU.add),
                             [ORT], [Gsb, sel_t, ORT])
            if stop == 30:
                c.barrier()
                return nc, c
            Wba = s3a([128, 4, D], BF16); Wbr = s3a([128, 4, D], BF16)
            load_w(Wba, lambda c0, c1: Wba[:, :, c0:c1], w_ba, 4, D)
            load_w(Wbr, lambda c0, c1: Wbr[:, :, c0:c1], w_br, 4, D)
            Wga = s3a([128, 8, 128], BF16); Wgr = s3a([128, 8, 128], BF16)
            sga = s3a([128, 512], BF16); sgr = s3a([128, 512], BF16); t1 = s3a([128, 512]); t2 = s3a([128, 512])
            NTG = (NO * 128 + 511) // 512
            for m in range(8):
                load_w(Wga, lambda c0, c1: Wga[:, :, c0:c1], w_ga[:, m * 128:(m + 1) * 128], 8, 128)
                load_w(Wgr, lambda c0, c1: Wgr[:, :, c0:c1], w_gr[:, m * 128:(m + 1) * 128], 8, 128)
                for tg in range(NTG):
                    t0 = tg * 512; t1e = min(NO * 128, t0 + 512); n = t1e - t0
                    pa, pr, pba, pbr = PB[2], PB[3], PB[4], PB[5]
                    for k in range(8):
                        c.op('pe', lambda: PE.matmul(pa[:, 0:n], lhsT=Wga[:, k, :], rhs=xnTo[:, k, t0:t1e], start=(k == 0), stop=(k == 7)), [pa], [Wga, xnTo])
                    for k in range(8):
                        c.op('pe', lambda: PE.matmul(pr[:, 0:n], lhsT=Wgr[:, k, :], rhs=xnTo[:, k, t0:t1e], start=(k == 0), stop=(k == 7)), [pr], [Wgr, xnTo])
                    for k in range(4):
                        c.op('pe', lambda: PE.matmul(pba[:, 0:n], lhsT=Wba[:, k, m * 128:(m + 1) * 128], rhs=OAT[:, k, t0:t1e], start=(k == 0), stop=(k == 3)),
                             [pba], [Wba, OAT])
                    for k in range(4):
                        c.op('pe', lambda: PE.matmul(pbr[:, 0:n], lhsT=Wbr[:, k, m * 128:(m + 1) * 128], rhs=ORT[:, k, t0:t1e], start=(k == 0), stop=(k == 3)),
                             [pbr], [Wbr, ORT])
                    c.op('act', lambda: A.activation(out=sga[:, 0:n], in_=pa[:, 0:n], func=AF.Sigmoid), [sga], [pa])
                    c.op('act', lambda: A.activation(out=sgr[:, 0:n], in_=pr[:, 0:n], func=AF.Sigmoid), [sgr], [pr])
                    c.op('dve', lambda: V.tensor_tensor(out=t1[:, 0:n], in0=sga[:, 0:n], in1=pba[:, 0:n], op=ALU.mult), [t1], [sga, pba])
                    c.op('dve', lambda: V.tensor_tensor(out=t2[:, 0:n], in0=sgr[:, 0:n], in1=pbr[:, 0:n], op=ALU.mult), [t2], [sgr, pbr])
                    c.op('pool', lambda: G.tensor_tensor(out=mergedT[:, m, t0:t1e], in0=t1[:, 0:n], in1=t2[:, 0:n], op=ALU.add), [mergedT], [t1, t2])
            c.barrier()
        if stop == 31:
            c.barrier()
            return nc, c
        dbg_dump("d_merged", mergedT, mergedT[:], [128, 8, NO * 128], BF16)
        with ExitStack() as st3b:
            s3b = lambda shape, dt=F32: c.sb(st3b, shape, dt)
            Wout = s3b([128, 8, D], BF16); Wrt = s3b([128, 8, 36], BF16)
            load_w(Wout, lambda c0, c1: Wout[:, :, c0:c1], w_out, 8, D)
            load_w(Wrt, lambda c0, c1: Wrt[:, :, c0:c1], w_rt, 8, 36)
            h1t = s3b([128, D]); xot = s3b([128, D]); tmpo = s3b([128, 512])
            lg = s3b([128, 36]); r8 = s3b([128, 16]); ohg = s3b([128, 4]); peng = s3b([128, 4]); el = s3b([128, 32]); el2 = s3b([128, 32])
            oh1 = s3b([128, 32]); oh2 = s3b([128, 32]); ge4 = s3b([128, 4])
            for i in range(NO):
                c.dma('sp', xot[:], xo[i * 128:(i + 1) * 128, :], [xot], [])
                for half in range(2):
                    po = PB[6 + half]
                    for k in range(8):
                        c.op('pe', lambda: PE.matmul(po[:, :], lhsT=mergedT[:, k, i * 128:(i + 1) * 128], rhs=Wout[:, k, half * 512:(half + 1) * 512],
                                                     start=(k == 0), stop=(k == 7)), [po], [mergedT, Wout])
                    c.op('dve', lambda: V.tensor_tensor(out=tmpo[:], in0=po[:, :], in1=gt1_bc[:, half * 512:(half + 1) * 512], op=ALU.mult), [tmpo], [po, gt1_bc])
                    c.op('dve', lambda: V.tensor_tensor(out=h1t[:, half * 512:(half + 1) * 512], in0=tmpo[:], in1=xot[:, half * 512:(half + 1) * 512], op=ALU.add),
                         [h1t], [tmpo, xot])
                c.dma('sp', out.ap()[i * 128:(i + 1) * 128, :], h1t[:], [outT], [h1t])
                norm_T(None, gmod2, sh2, xnTo, lambda k: xnTo[:, k, i * 128:(i + 1) * 128], keep_x=h1t)
                pm = PB[2]
                for k in range(8):
                    c.op('pe', lambda: PE.matmul(pm[:, 0:36], lhsT=xnTo[:, k, i * 128:(i + 1) * 128], rhs=Wrt[:, k, :], start=(k == 0), stop=(k == 7)), [pm], [xnTo, Wrt])
                c.op('dve', lambda: V.tensor_copy(out=lg[:], in_=pm[:, 0:36]), [lg], [pm])
                c.op('dve', lambda: V.tensor_reduce(out=r8[:, 0:1], in_=lg[:, 0:4], axis=AX.X, op=ALU.max), [r8], [lg])
                c.op('dve', lambda: V.tensor_scalar(out=r8[:, 1:2], in0=r8[:, 0:1], scalar1=-1.0, scalar2=None, op0=ALU.mult), [r8], [r8])
                c.op('act', lambda: A.activation(out=ge4[:], in_=lg[:, 0:4], func=AF.Exp, bias=r8[:, 1:2], scale=1.0, accum_out=r8[:, 2:3]), [ge4, r8], [lg, r8])
                c.op('dve', lambda: V.reciprocal(out=r8[:, 3:4], in_=r8[:, 2:3]), [r8], [r8])
                c.op('dve', lambda: V.tensor_scalar(out=ohg[:], in0=lg[:, 0:4], scalar1=r8[:, 0:1], scalar2=None, op0=ALU.is_equal), [ohg], [lg, r8])
                c.op('dve', lambda: V.tensor_scalar(out=peng[:], in0=ohg[:], scalar1=1.0, scalar2=1e30, op0=ALU.subtract, op1=ALU.mult), [peng], [ohg])
                c.op('dve', lambda: V.tensor_tensor(out=el[:], in0=lg[:, 4:36], in1=ebias_t[:], op=ALU.add), [el], [lg, ebias_t])
                c.op('dve', lambda: V.tensor_tensor(out=el[:].rearrange("p (g e) -> p g e", g=4), in0=el[:].rearrange("p (g e) -> p g e", g=4),
                                                    in1=peng[:, :].unsqueeze(2).broadcast_to([128, 4, 8]), op=ALU.add), [el], [el, peng])
                c.op('dve', lambda: V.tensor_reduce(out=r8[:, 4:5], in_=el[:], axis=AX.X, op=ALU.max), [r8], [el])
                c.op('dve', lambda: V.tensor_scalar(out=oh1[:], in0=el[:], scalar1=r8[:, 4:5], scalar2=None, op0=ALU.is_equal), [oh1], [el, r8])
                c.op('dve', lambda: V.scalar_tensor_tensor(out=el2[:], in0=oh1[:], scalar=-1e30, in1=el[:], op0=ALU.mult, op1=ALU.add), [el2], [oh1, el])
                c.op('dve', lambda: V.tensor_reduce(out=r8[:, 5:6], in_=el2[:], axis=AX.X, op=ALU.max), [r8], [el2])
                c.op('dve', lambda: V.tensor_scalar(out=oh2[:], in0=el2[:], scalar1=r8[:, 5:6], scalar2=None, op0=ALU.is_equal), [oh2], [el2, r8])
                c.op('dve', lambda: V.tensor_tensor(out=r8[:, 6:7], in0=r8[:, 4:5], in1=r8[:, 5:6], op=ALU.subtract), [r8], [r8])
                c.op('act', lambda: A.activation(out=r8[:, 7:8], in_=r8[:, 6:7], func=AF.Sigmoid), [r8], [r8])
                c.op('dve', lambda: V.tensor_scalar(out=r8[:, 8:9], in0=r8[:, 7:8], scalar1=-1.0, scalar2=1.0, op0=ALU.mult, op1=ALU.add), [r8], [r8])
                c.op('dve', lambda: V.tensor_tensor(out=r8[:, 9:10], in0=r8[:, 7:8], in1=r8[:, 3:4], op=ALU.mult), [r8], [r8])
                c.op('dve', lambda: V.tensor_tensor(out=r8[:, 10:11], in0=r8[:, 8:9], in1=r8[:, 3:4], op=ALU.mult), [r8], [r8])
                c.op('dve', lambda: V.tensor_scalar(out=cw[:, i, :], in0=oh1[:], scalar1=r8[:, 9:10], scalar2=None, op0=ALU.mult), [cw], [oh1, r8])
                c.op('dve', lambda: V.scalar_tensor_tensor(out=cw[:, i, :], in0=oh2[:], scalar=r8[:, 10:11], in1=cw[:, i, :], op0=ALU.mult, op1=ALU.add),
                     [cw], [oh2, r8, cw])
            c.barrier()
        if stop == 32:
            c.barrier()
            return nc, c
        dbg_dump("d_cw", cw, cw[:], [128, NO, 32])
        stM.close()
        with ExitStack() as st3c:
            s3c = lambda shape, dt=F32: c.sb(st3c, shape, dt)
            accm = s3c([128, NO, D], BF16)
            c.op('pool', lambda: G.memset(accm[:], 0.0), [accm], [])
            Wg = [s3c([128, 8, 512], BF16) for _ in range(2)]; Wu = [s3c([128, 8, 512], BF16) for _ in range(2)]
            Wd = [s3c([128, 4, D], BF16) for _ in range(2)]
            hdn = [s3c([128, 4, 512], BF16) for _ in range(2)]; sgb = [s3c([128, 512], BF16) for _ in range(2)]
            NTG = (NO * 128 + 511) // 512
            cnt = 0
            for e in range(NE):
                wg, wu, wd = Wg[e % 2], Wu[e % 2], Wd[e % 2]
                load_w(wg, lambda c0, c1: wg[:, :, c0:c1], ew_g[e], 8, 512)
                load_w(wu, lambda c0, c1: wu[:, :, c0:c1], ew_u[e], 8, 512)
                load_w(wd, lambda c0, c1: wd[:, :, c0:c1], ew_d[e], 4, D)
                for tg in range(NTG):
                    t0 = tg * 512; t1e = min(NO * 128, t0 + 512); n = t1e - t0
                    hb = hdn[tg % 2]
                    for f in range(4):
                        pg_, pu_ = PB[2 + (cnt % 2)], PB[4 + (cnt % 2)]
                        sg_ = sgb[cnt % 2]
                        cnt += 1
                        for k in range(8):
                            c.op('pe', lambda: PE.matmul(pg_[:, 0:n], lhsT=wg[:, k, f * 128:(f + 1) * 128], rhs=xnTo[:, k, t0:t1e], start=(k == 0), stop=(k == 7)),
                                 [pg_], [wg, xnTo])
                        for k in range(8):
                            c.op('pe', lambda: PE.matmul(pu_[:, 0:n], lhsT=wu[:, k, f * 128:(f + 1) * 128], rhs=xnTo[:, k, t0:t1e], start=(k == 0), stop=(k == 7)),
                                 [pu_], [wu, xnTo])
                        c.op('act', lambda: A.activation(out=sg_[:, 0:n], in_=pg_[:, 0:n], func=AF.Silu), [sg_], [pg_])
                        c.op('dve', lambda: V.tensor_tensor(out=hb[:, f, 0:n], in0=sg_[:, 0:n], in1=pu_[:, 0:n], op=ALU.mult), [hb], [sg_, pu_])
                    for tt in range(n // 128):
                        tile = tg * 4 + tt
                        for half in range(2):
                            pd = PB[6 + half]
                            for f in range(4):
                                c.op('pe', lambda: PE.matmul(pd[:, :], lhsT=hb[:, f, tt * 128:(tt + 1) * 128], rhs=wd[:, f, half * 512:(half + 1) * 512],
                                                             start=(f == 0), stop=(f == 3)), [pd], [hb, wd])
                            av = accm[:, tile, half * 512:(half + 1) * 512]
                            c.op('dve', lambda: V.scalar_tensor_tensor(out=av, in0=pd[:, :], scalar=cw[:, tile, e:e + 1], in1=av, op0=ALU.mult, op1=ALU.add),
                                 [accm], [pd, cw, accm])
            if stop == 33:
                c.barrier()
                return nc, c
            h1b = [s3c([128, D]) for _ in range(2)]; ob = [s3c([128, D]) for _ in range(2)]
            for i in range(NO):
                hb_, o_ = h1b[i % 2], ob[i % 2]
                c.dma('sp', hb_[:], out.ap()[i * 128:(i + 1) * 128, :], [hb_], [outT])
                c.op('dve', lambda: V.tensor_tensor(out=o_[:], in0=accm[:, i, :], in1=gt2_bc[:], op=ALU.mult), [o_], [accm, gt2_bc])
                c.op('pool', lambda: G.tensor_tensor(out=o_[:], in0=o_[:], in1=hb_[:], op=ALU.add), [o_], [o_, hb_])
                c.dma('sp', out.ap()[i * 128:(i + 1) * 128, :], o_[:], [outT], [o_])
            c.barrier()
    import os as _os
    for _ in range(int(_os.environ.get("PAD_PE", "0"))):
        c.op('pe', lambda: PE.matmul(PB[0][:, 0:1], lhsT=ident[:, 0:128], rhs=ident[:, 0:1], start=True, stop=True), [PB[0]], [ident])
    for _ in range(int(_os.environ.get("PAD_ACT", "0"))):
        c.op('act', lambda: A.copy(out=st4[:, 0:1], in_=st4[:, 1:2]), [st4], [])
    c.barrier()
    glob.close()
    return nc, c


_IN_W_OFF = dict(q_a=0, kv=512, q_i=768, k_i=1024, w_i=1056, r=1064, k=1576, v=2088, wl=2600, al=2664, gl=2728, ga=2888, gr=3912)


def _prep_inputs(inp, NG=16, NE=32):
    S = 512 * NG
    f = lambda a: np.ascontiguousarray(np.asarray(a, dtype=np.float32))
    x = f(inp["x"]); cvec = f(inp["c"])
    w_in = f(inp["w_in"])[0]
    O = _IN_W_OFF
    col = lambda a, n: w_in[:, a:a + n]
    r128 = lambda v: f(v.reshape(-1, 128).T)
    mu = f(inp["rwkv_mu"])[0]
    shared = {
        "ada_w": f(inp["ada_w"])[0], "ada_b": f(inp["ada_b"])[0].reshape(1, -1),
        "mixg": r128(f(inp["mix_norm_g"])[0]), "moeg": r128(f(inp["moe_norm_g"])[0]),
        "w_kv": f(col(O["kv"], 256)), "w_ki": f(np.tile(col(O["k_i"], 32), (1, 4))),
        "w_q": f(np.concatenate([col(O["q_a"], 512), col(O["q_i"], 256), col(O["w_i"], 8)], axis=1)),
        "kvg": r128(f(inp["kv_norm_g"])[0]), "w_kvup": f(inp["w_kv_up"])[0],
        "qg": f(np.tile(f(inp["q_norm_g"])[0], 2).reshape(128, 1)), "kg": f(np.tile(f(inp["k_norm_g"])[0], 2).reshape(128, 1)),
        "w_ga": f(col(O["ga"], 1024)), "w_gr": f(col(O["gr"], 1024)),
        "w_ba": f(inp["w_branch_attn"])[0], "w_br": f(inp["w_branch_rwkv"])[0], "w_out": f(inp["w_out"])[0],
        "w_rt": f(np.concatenate([f(inp["router_group_w"])[0], f(inp["router_expert_w"])[0]], axis=1)),
        "e_bias": f(inp["router_expert_bias"])[0].reshape(1, 32),
    }
    for e in range(NE):
        shared["ew_g%d" % e] = f(inp["expert_w_gate"][0, e]); shared["ew_u%d" % e] = f(inp["expert_w_up"][0, e])
        shared["ew_d%d" % e] = f(inp["expert_w_down"][0, e])
    maps = []
    for cid in range(8):
        b, q = cid // 4, cid % 4
        m = dict(shared)
        for i_ in range((4 * NG + 31) // 32):
            m["xb%d" % i_] = f(x[b, i_ * 4096:min(S, (i_ + 1) * 4096)])
        own = [q + 4 * i for i in range(NG)]
        m["xo"] = f(np.concatenate([x[b, t * 128:(t + 1) * 128] for t in own], axis=0))
        m["cb"] = r128(cvec[b])
        m["qrel"] = f((128 * q + np.arange(128)).reshape(128, 1))
        sel = np.zeros((128, 4), np.float32); sel[:, q] = 1.0
        m["sel"] = sel
        h0 = 2 * q
        rcols = [col(O["r"] + (h0 + h) * 64, 64) for h in range(2)]
        kcols = [col(O["k"] + (h0 + h) * 64, 64) for h in range(2)]
        vcols = [col(O["v"] + (h0 + h) * 64, 64) for h in range(2)]
        m["w_rw"] = f(np.concatenate(rcols + kcols + vcols + [col(O["wl"], 64), col(O["al"], 64), col(O["gl"], 160)], axis=1))
        mu_rw = np.zeros((128, 10), np.float32)
        for h in range(2):
            mu_rw[0:64, 0 + h] = mu[(h0 + h) * 64:(h0 + h + 1) * 64]
            mu_rw[0:64, 2 + h] = mu[512 + (h0 + h) * 64:512 + (h0 + h + 1) * 64]
            mu_rw[0:64, 4 + h] = mu[1024 + (h0 + h) * 64:1024 + (h0 + h + 1) * 64]
        mu_rw[0:64, 6] = mu[1536:1600]; mu_rw[0:64, 7] = mu[1600:1664]
        mu_rw[0:128, 8] = mu[1664:1792]; mu_rw[0:32, 9] = mu[1792:1824]
        m["mu_rw"] = mu_rw
        rwv = np.zeros((64, 2, 8), np.float32)
        for h in range(2):
            sl = slice((h0 + h) * 64, (h0 + h + 1) * 64)
            rwv[:, h, 0] = f(inp["rwkv_w0"])[0][sl]; rwv[:, h, 1] = f(inp["rwkv_a0"])[0][sl]
            rwv[:, h, 2] = f(inp["rwkv_k_k"])[0][sl]; rwv[:, h, 3] = f(inp["rwkv_k_a"])[0][sl]
            rwv[:, h, 4] = f(inp["rwkv_r_k"])[0][h0 + h]; rwv[:, h, 5] = f(inp["rwkv_ln_w"])[0][sl]
            rwv[:, h, 6] = f(inp["rwkv_ln_b"])[0][sl]
        m["rwv"] = rwv
        hs = slice(h0 * 64, h0 * 64 + 128)
        m["w2"] = f(f(inp["rwkv_w2"])[0][:, hs]); m["a2"] = f(f(inp["rwkv_a2"])[0][:, hs]); m["g2"] = f(f(inp["rwkv_g2"])[0][:, hs])
        maps.append(m)
    return maps


_CACHE = {}


def kernel(**inputs):
    NG = 16
    if NG not in _CACHE:
        _CACHE[NG] = build_program(NG)[0]
    nc = _CACHE[NG]
    maps = _prep_inputs(inputs, NG)
    res = run_bass_kernel_spmd(nc, maps, core_ids=list(range(8)))
    out = np.zeros((2, 512 * NG, D), np.float32)
    for cid in range(8):
        b, q = cid // 4, cid % 4
        o = np.asarray(res.results[cid]["out"], dtype=np.float32)
        for i in range(NG):
            t = q + 4 * i
            out[b, t * 128:(t + 1) * 128] = o[i * 128:(i + 1) * 128]
    return out
```
